# Optimizing a Trainium2 kernel written in Bass

```python
import jax, jax.numpy as jnp
from jax import lax
import numpy as np

D_MODEL = 4096
BATCH = 4
SEQ = 4096
DEPTH = 2

N_A = DEPTH // 2
N_B = DEPTH - N_A

RET_HEADS = 16
RET_HEAD_DIM = D_MODEL // RET_HEADS
RET_CHUNK = 128

NSA_HEADS = 32
NSA_HEAD_DIM = D_MODEL // NSA_HEADS
NSA_KV_GROUPS = 4
NSA_HPG = NSA_HEADS // NSA_KV_GROUPS
CMP_BLOCK = 32
CMP_STRIDE = 16
SEL_BLOCK = 64
N_SELECT = 16
WINDOW = 512
NSA_Q_BLOCK = 16
KV_WIDTH = NSA_KV_GROUPS * NSA_HEAD_DIM

D_FF = ((8 * D_MODEL // 3 + 255) // 256) * 256
CONV_WIDTH = 3

NORM_EPS = 1e-6
GN_EPS = 1e-5
NEG_BIG = -1e30
SEL_FORCE = 1e9

kernel_name = "hybrid_retention_nsa_yoco_convffn"

f32 = jnp.float32


def rmsnorm(x, g):
    xf = x.astype(f32)
    y = xf * lax.rsqrt(jnp.mean(xf * xf, axis=-1, keepdims=True) + NORM_EPS)
    return (y * g.astype(f32)).astype(x.dtype)


def masked_softmax(s, mask):
    s = jnp.where(mask, s, NEG_BIG)
    m = jnp.max(s, axis=-1, keepdims=True)
    e = jnp.where(mask, jnp.exp(s - m), 0.0)
    return e / jnp.maximum(jnp.sum(e, axis=-1, keepdims=True), 1e-30)


def alibi_slopes():
    h = jnp.arange(1, NSA_HEADS + 1, dtype=f32)
    return jnp.exp2(-8.0 * h / NSA_HEADS).reshape(NSA_KV_GROUPS, NSA_HPG)


def retention(h, w_in, gn_gain, w_out):
    B, S, _ = h.shape
    nc = S // RET_CHUNK
    proj = h @ w_in
    q, k, v, g = jnp.split(proj, 4, axis=-1)

    def chunks(t):
        return t.reshape(B, nc, RET_CHUNK, RET_HEADS, RET_HEAD_DIM).transpose(1, 0, 3, 2, 4).astype(f32)

    qc, kc, vc = chunks(q), chunks(k) * (RET_HEAD_DIM ** -0.5), chunks(v)
    log_g = jnp.log1p(-jnp.exp2(-5.0 - jnp.arange(RET_HEADS, dtype=f32)))
    i = jnp.arange(RET_CHUNK, dtype=f32)
    diff = i[:, None] - i[None, :]
    decay_mask = jnp.where(diff >= 0, jnp.exp(jnp.maximum(diff, 0.0)[None] * log_g[:, None, None]), 0.0)
    q_decay = jnp.exp((i + 1.0)[None] * log_g[:, None])
    k_decay = jnp.exp((RET_CHUNK - 1.0 - i)[None] * log_g[:, None])
    chunk_decay = jnp.exp(RET_CHUNK * log_g)

    def step(state, inp):
        qi, ki, vi = inp
        qk = jnp.einsum('bhid,bhjd->bhij', qi, ki) * decay_mask
        inner = jnp.einsum('bhij,bhjd->bhid', qk, vi)
        cross = jnp.einsum('bhid,bhde->bhie', qi, state) * q_decay[:, :, None]
        state = state * chunk_decay[:, None, None] + jnp.einsum(
            'bhjd,bhje->bhde', ki * k_decay[:, :, None], vi)
        return state, inner + cross

    state0 = jnp.zeros((B, RET_HEADS, RET_HEAD_DIM, RET_HEAD_DIM), f32)
    _, y = lax.scan(step, state0, (qc, kc, vc))
    y = y.transpose(1, 0, 3, 2, 4).reshape(B, S, RET_HEADS, RET_HEAD_DIM)
    mu = jnp.mean(y, axis=-1, keepdims=True)
    var = jnp.mean(jnp.square(y - mu), axis=-1, keepdims=True)
    y = ((y - mu) * lax.rsqrt(var + GN_EPS)).reshape(B, S, D_MODEL) * gn_gain.astype(f32)
    return (jax.nn.silu(g.astype(f32)) * y).astype(h.dtype) @ w_out


def compress(tok, pos_emb, w1, w2):
    B, S = tok.shape[0], tok.shape[1]
    nc = (S - CMP_BLOCK) // CMP_STRIDE + 1
    idx = jnp.arange(nc)[:, None] * CMP_STRIDE + jnp.arange(CMP_BLOCK)[None, :]
    blk = tok[:, idx] + pos_emb[None, None, :, None, :]
    blk = blk.transpose(0, 1, 3, 2, 4).reshape(B, nc, NSA_KV_GROUPS, CMP_BLOCK * NSA_HEAD_DIM)
    return jax.nn.silu(blk @ w1) @ w2


def shared_kv(hkv, w_kv, cmp_pos, cmp_w1, cmp_w2):
    B, S, _ = hkv.shape
    kv = (hkv @ w_kv).reshape(B, S, 6, NSA_KV_GROUPS, NSA_HEAD_DIM)
    k_cmp_tok, v_cmp_tok = kv[:, :, 0], kv[:, :, 1]
    k_slc, v_slc = kv[:, :, 2], kv[:, :, 3]
    k_win, v_win = kv[:, :, 4], kv[:, :, 5]
    kc = compress(k_cmp_tok, cmp_pos[0], cmp_w1[0], cmp_w2[0])
    vc = compress(v_cmp_tok, cmp_pos[1], cmp_w1[1], cmp_w2[1])
    ns = S // SEL_BLOCK

    def to_blocks(t):
        return t.transpose(0, 2, 1, 3).reshape(B, NSA_KV_GROUPS, ns, SEL_BLOCK, NSA_HEAD_DIM)

    pad = ((0, 0), (WINDOW, 0), (0, 0), (0, 0))
    return (kc, vc, to_blocks(k_slc), to_blocks(v_slc), jnp.pad(k_win, pad), jnp.pad(v_win, pad))


def selection_map(nc, ns):
    start = jnp.arange(nc)[:, None] * CMP_STRIDE
    j = jnp.arange(ns)[None, :]
    ov = jnp.minimum(start + CMP_BLOCK, (j + 1) * SEL_BLOCK) - jnp.maximum(start, j * SEL_BLOCK)
    return jnp.clip(ov, 0).astype(f32) / CMP_STRIDE


def nsa(h, shared, w_q, w_out):
    B, S, _ = h.shape
    kc, vc, ks_blocks, vs_blocks, kw_pad, vw_pad = shared
    nc = kc.shape[1]
    ns = ks_blocks.shape[2]
    n_sel = min(N_SELECT, ns)
    scale = NSA_HEAD_DIM ** -0.5
    slopes = alibi_slopes()
    sel_map = selection_map(nc, ns)
    pos_cmp = jnp.arange(nc) * CMP_STRIDE + CMP_BLOCK - 1
    gather = jax.vmap(jax.vmap(lambda blocks, i: blocks[i]))

    proj = h @ w_q
    q = proj[..., :D_MODEL].reshape(B, S, NSA_KV_GROUPS, NSA_HPG, NSA_HEAD_DIM)
    gate = jax.nn.sigmoid(proj[..., D_MODEL:].astype(f32)).reshape(B, S, NSA_KV_GROUPS, NSA_HPG, 3)
    nqb = S // NSA_Q_BLOCK
    q_blocks = jnp.moveaxis(q.reshape(B, nqb, NSA_Q_BLOCK, NSA_KV_GROUPS, NSA_HPG, NSA_HEAD_DIM), 1, 0)
    g_blocks = jnp.moveaxis(gate.reshape(B, nqb, NSA_Q_BLOCK, NSA_KV_GROUPS, NSA_HPG, 3), 1, 0)
    starts = jnp.arange(nqb, dtype=jnp.int32) * NSA_Q_BLOCK

    def block(args):
        qb, gb, s0 = args
        t = s0 + jnp.arange(NSA_Q_BLOCK, dtype=jnp.int32)
        dist_c = t[:, None] - pos_cmp[None, :]
        s_c = jnp.einsum('bqghd,bngd->bghqn', qb, kc).astype(f32) * scale
        s_c = s_c - slopes[None, :, :, None, None] * dist_c.astype(f32)
        p_c = masked_softmax(s_c, (dist_c >= 0)[None, None, None])
        o_c = jnp.einsum('bghqn,bngd->bqghd', p_c, vc.astype(f32))
        imp = jnp.einsum('bghqn,nj->bgqj', p_c, sel_map)
        j = jnp.arange(ns)
        cur = t // SEL_BLOCK
        valid = (j[None, :] * SEL_BLOCK) <= t[:, None]
        forced = (j[None, :] == 0) | (j[None, :] == cur[:, None]) | (j[None, :] == cur[:, None] - 1)
        score = jnp.where(valid, jnp.where(forced, SEL_FORCE, imp), -SEL_FORCE)
        _, idx = lax.top_k(score, n_sel)
        k_sel = gather(ks_blocks, idx)
        v_sel = gather(vs_blocks, idx)
        pos = idx[..., None] * SEL_BLOCK + jnp.arange(SEL_BLOCK)
        dist_s = (t[None, None, :, None, None] - pos)[:, :, None]
        s_s = jnp.einsum('bqghd,bgqnld->bghqnl', qb, k_sel).astype(f32) * scale
        s_s = s_s - slopes[None, :, :, None, None, None] * dist_s.astype(f32)
        Bq = s_s.shape[0]
        flat = (Bq, NSA_KV_GROUPS, NSA_HPG, NSA_Q_BLOCK, n_sel * SEL_BLOCK)
        mask_s = jnp.broadcast_to(dist_s >= 0, s_s.shape).reshape(flat)
        p_s = masked_softmax(s_s.reshape(flat), mask_s).reshape(s_s.shape)
        o_s = jnp.einsum('bghqnl,bgqnld->bqghd', p_s, v_sel.astype(f32))
        kw = lax.dynamic_slice_in_dim(kw_pad, s0, NSA_Q_BLOCK + WINDOW, axis=1)
        vw = lax.dynamic_slice_in_dim(vw_pad, s0, NSA_Q_BLOCK + WINDOW, axis=1)
        kpos = s0 - WINDOW + jnp.arange(NSA_Q_BLOCK + WINDOW, dtype=jnp.int32)
        dist_w = t[:, None] - kpos[None, :]
        mask_w = (dist_w >= 0) & (dist_w < WINDOW) & (kpos[None, :] >= 0)
        s_w = jnp.einsum('bqghd,bkgd->bghqk', qb, kw).astype(f32) * scale
        s_w = s_w - slopes[None, :, :, None, None] * dist_w.astype(f32)
        p_w = masked_softmax(s_w, mask_w[None, None, None])
        o_w = jnp.einsum('bghqk,bkgd->bqghd', p_w, vw.astype(f32))
        o = gb[..., 0:1] * o_c + gb[..., 1:2] * o_s + gb[..., 2:3] * o_w
        return o.astype(h.dtype)

    out = lax.map(block, (q_blocks, g_blocks, starts))
    out = jnp.moveaxis(out, 0, 1).reshape(B, S, D_MODEL)
    return out @ w_out


def conv_ffn(h, w_in, conv_w, conv_b, w_out):
    a, u = jnp.split(h @ w_in, 2, axis=-1)
    a = lax.conv_general_dilated(
        a, conv_w[:, None, :].astype(a.dtype), window_strides=(1,),
        padding=[(CONV_WIDTH - 1, 0)], dimension_numbers=('NWC', 'WIO', 'NWC'),
        feature_group_count=a.shape[-1]) + conv_b
    return (jax.nn.silu(a) * u) @ w_out


def setup_inputs(seed: int = 0) -> dict:
    key = jax.random.key(seed)
    ks = jax.random.split(key, 20)
    nrm = jax.random.normal
    D, F = D_MODEL, D_FF
    return {
        "x": nrm(ks[0], (BATCH, SEQ, D), f32),
        "attn_norm": 1.0 + 0.02 * nrm(ks[1], (DEPTH, D), f32),
        "ffn_norm": 1.0 + 0.02 * nrm(ks[2], (DEPTH, D), f32),
        "w_ret_in": nrm(ks[3], (N_A, D, 4 * D), f32) * D ** -0.5,
        "ret_gn_gain": 1.0 + 0.02 * nrm(ks[4], (N_A, D), f32),
        "w_ret_out": nrm(ks[5], (N_A, D, D), f32) * D ** -0.5,
        "kv_norm": 1.0 + 0.02 * nrm(ks[6], (D,), f32),
        "w_kv": nrm(ks[7], (D, 6 * KV_WIDTH), f32) * D ** -0.5,
        "cmp_pos": 0.02 * nrm(ks[8], (2, CMP_BLOCK, NSA_HEAD_DIM), f32),
        "cmp_w1": nrm(ks[9], (2, CMP_BLOCK * NSA_HEAD_DIM, NSA_HEAD_DIM), f32) * (CMP_BLOCK * NSA_HEAD_DIM) ** -0.5,
        "cmp_w2": nrm(ks[10], (2, NSA_HEAD_DIM, NSA_HEAD_DIM), f32) * NSA_HEAD_DIM ** -0.5,
        "w_nsa_q": nrm(ks[11], (N_B, D, D + 3 * NSA_HEADS), f32) * D ** -0.5,
        "w_nsa_out": nrm(ks[12], (N_B, D, D), f32) * D ** -0.5,
        "w_ffn_in": nrm(ks[13], (DEPTH, D, 2 * F), f32) * D ** -0.5,
        "conv_w": nrm(ks[14], (DEPTH, CONV_WIDTH, F), f32) * CONV_WIDTH ** -0.5,
        "conv_b": 0.02 * nrm(ks[15], (DEPTH, F), f32),
        "w_ffn_out": nrm(ks[16], (DEPTH, F, D), f32) * F ** -0.5,
        "final_norm": 1.0 + 0.02 * nrm(ks[17], (D,), f32),
    }


def reference(x, attn_norm, ffn_norm, w_ret_in, ret_gn_gain, w_ret_out, kv_norm, w_kv,
              cmp_pos, cmp_w1, cmp_w2, w_nsa_q, w_nsa_out, w_ffn_in, conv_w, conv_b,
              w_ffn_out, final_norm):
    shared = None
    for layer in range(DEPTH):
        if layer < N_A:
            x = x + retention(rmsnorm(x, attn_norm[layer]), w_ret_in[layer],
                              ret_gn_gain[layer], w_ret_out[layer])
        else:
            if layer == N_A:
                shared = shared_kv(rmsnorm(x, kv_norm), w_kv, cmp_pos, cmp_w1, cmp_w2)
            b = layer - N_A
            x = x + nsa(rmsnorm(x, attn_norm[layer]), shared, w_nsa_q[b], w_nsa_out[b])
        x = x + conv_ffn(rmsnorm(x, ffn_norm[layer]), w_ffn_in[layer], conv_w[layer],
                         conv_b[layer], w_ffn_out[layer])
    return rmsnorm(x, final_norm)
```

```python
import numpy as np
import concourse.bass as bass
import concourse.mybir as mybir
from contextlib import ExitStack

F32 = mybir.dt.float32
F32R = mybir.dt.float32r
BF16 = mybir.dt.bfloat16
AF = mybir.ActivationFunctionType
ALU = mybir.AluOpType
AX = mybir.AxisListType

CE = ("pe", "act", "dve", "pool")
NDMA = 48


class Prog:
    def __init__(self, name="k"):
        self.nc = bass.Bass("TRN2", target_bir_lowering=False)
        nc = self.nc
        self.stack = ExitStack()
        self.eng = {"pe": nc.tensor, "act": nc.scalar, "dve": nc.vector,
                    "pool": nc.gpsimd, "sp": nc.sync}
        self.sem = {e: self.stack.enter_context(nc.semaphore("s_" + e)) for e in CE}
        self.cnt = {e: 0 for e in CE}
        self.dsem = [self.stack.enter_context(nc.semaphore("d%d" % i)) for i in range(NDMA)]
        self.dcnt = [0] * NDMA
        self.dnext = 0
        self.know = {e: {} for e in self.eng}
        self.lastw = {}
        self.rd = {}
        self.nops = 0
        self.out_events = []
        self.sw_out = []
        self.sw_budget = 6000

    def dram(self, name, shape, dtype, kind):
        return self.nc.dram_tensor(name, list(shape), dtype, kind=kind).ap()

    def sb(self, name, shape, dtype):
        return self.stack.enter_context(self.nc.sbuf_tensor(name, list(shape), dtype))

    def ps(self, name, shape, dtype=F32):
        return self.stack.enter_context(self.nc.psum_tensor(name, list(shape), dtype))

    def _semh(self, k):
        return self.sem[k] if isinstance(k, str) else self.dsem[k]

    def _need(self, e, reads, writes):
        ev = []
        for k in reads:
            w = self.lastw.get(k)
            if w is not None:
                ev.append(w)
        for k in writes:
            w = self.lastw.get(k)
            if w is not None:
                ev.append(w)
            ev.extend(self.rd.get(k, ()))
        need = {}
        kn = self.know[e]
        for (sk, v, clk) in ev:
            if e == "pe" and sk == "pe":
                continue
            if kn.get(sk, 0) >= v:
                continue
            if need.get(sk, 0) < v:
                need[sk] = v
        return need, ev

    def _wait(self, e, need, ev):
        eng = self.eng[e]
        kn = self.know[e]
        for sk, v in need.items():
            eng.wait_ge(self._semh(sk), v)
            kn[sk] = v
        for (sk, v, clk) in ev:
            if clk:
                for ck, cv in clk.items():
                    if kn.get(ck, 0) < cv:
                        kn[ck] = cv

    def _record(self, event, reads, writes):
        for k in writes:
            self.lastw[k] = event
            self.rd[k] = []
        for k in reads:
            if k in writes:
                continue
            self.rd.setdefault(k, []).append(event)

    def op(self, e, fn, reads=(), writes=()):
        need, ev = self._need(e, reads, writes)
        self._wait(e, need, ev)
        ins = fn(self.eng[e])
        self.cnt[e] += 1
        ins.then_inc(self.sem[e], 1)
        kn = self.know[e]
        clk = {k: v for k, v in kn.items() if isinstance(k, str)}
        clk[e] = self.cnt[e]
        if e != "pe":
            pass
        event = (e, self.cnt[e], clk)
        self._record(event, reads, writes)
        self.nops += 1
        return event

    def dma(self, q, out, in_, reads=(), writes=(), is_output=False):
        need, ev = self._need(q, reads, writes)
        nd = 0
        if q == "pool":
            nd = 1
            for d_ in list(out.shape)[:-1]:
                nd *= int(d_)
            tot = sum(n for (_, n) in self.sw_out) + nd
            while self.sw_out and tot > self.sw_budget:
                (osk, ov, _), on = self.sw_out.pop(0)
                tot -= on
                if self.know[q].get(osk, 0) < ov:
                    need[osk] = max(need.get(osk, 0), ov)
        s = self.dnext
        self.dnext = (self.dnext + 1) % NDMA
        prev = self.dcnt[s]
        if prev > 0 and self.know[q].get(s, 0) < prev:
            need[s] = max(need.get(s, 0), prev)
        self._wait(q, need, ev)
        ins = self.eng[q].dma_start(out=out, in_=in_)
        self.dcnt[s] = prev + 16
        ins.then_inc(self.dsem[s], 16)
        event = (s, self.dcnt[s], None)
        if q == "pool":
            self.sw_out.append((event, nd))
        self._record(event, reads, writes)
        if is_output:
            self.out_events.append(event)
        self.nops += 1
        return event

    def finish(self):
        e = "sp"
        for (sk, v, _) in self.out_events:
            if self.know[e].get(sk, 0) < v:
                self.eng[e].wait_ge(self._semh(sk), v)
                self.know[e][sk] = v
        self.stack.close()
        return self.nc

NORM_EPS = 1e-6


def build_gemm(K, N, T, TT, pro, epi, NPAIR=False, WSPLIT=4, conv=False):
    p = Prog()
    nc = p.nc
    KC = K // 128
    NCH = (N + 127) // 128
    w = p.dram("w", [K, N], F32, "ExternalInput")
    xT = p.dram("xT", [K, T], F32, "ExternalInput")
    if pro in ("norm", "gate"):
        g = p.dram("g", [128, KC], F32, "ExternalInput")
        gs = p.sb("gs", [128, KC], F32)
        p.dma("sp", gs[:], g, writes=["gs"])
    if pro == "gate":
        gT = p.dram("gT", [K, T], F32, "ExternalInput")
    if epi == "resid":
        rT = p.dram("rT", [N, T], F32, "ExternalInput")
    oT = p.dram("oT", [N, T], F32, "ExternalOutput")

    hT = p.sb("hT", [128, KC, TT], BF16)
    NW = 2
    wb = [p.sb("wb%d" % i, [128, KC, 128], BF16) for i in range(NW)]
    NO = 3
    ob = [p.sb("ob%d" % i, [128, 512], F32) for i in range(NO)]
    NP = 3
    pt = [p.ps("pt%d" % i, [128, 512]) for i in range(NP)]
    NTS = TT // 512
    if pro == "norm":
        ones = p.sb("ones", [128, 128], BF16)
        p.op("dve", lambda e: e.memset(ones[:], 1.0), writes=["ones"])
        epsb = p.sb("epsb", [128, 1], F32)
        p.op("dve", lambda e: e.memset(epsb[:], NORM_EPS), writes=["epsb"])
        rstd = p.sb("rstd", [128, TT], F32)
        ss = [p.ps("ss%d" % i, [128, 512]) for i in range(NTS)]
        xs = [p.sb("xs%d" % i, [128, 512], F32) for i in range(3)]
        sq = [p.sb("sq%d" % i, [128, 512], BF16) for i in range(2)]
    if pro == "gate":
        xs = [p.sb("xs%d" % i, [128, 512], F32) for i in range(2)]
        gx = [p.sb("gx%d" % i, [128, 512], F32) for i in range(2)]
    if epi == "resid":
        rb = [p.sb("rb%d" % i, [128, 512], F32) for i in range(NO)]

    wv = w.rearrange("(c p) n -> p c n", p=128)
    xv = xT.rearrange("(c p) t -> p c t", p=128)
    ksplit = [(i * KC // WSPLIT, (i + 1) * KC // WSPLIT) for i in range(WSPLIT)]
    ctr = {"x": 0, "o": 0, "p": 0, "w": 0}

    for tp in range(T // TT):
        t0 = tp * TT
        if pro == "plain":
            for (k0, k1) in ksplit:
                p.dma("pool", hT[:, k0:k1, :], xv[:, k0:k1, t0:t0 + TT],
                      writes=[("hT", k) for k in range(k0, k1)])
        elif pro == "norm":
            for kc in range(KC):
                for ts in range(NTS):
                    i = ctr["x"]; ctr["x"] += 1
                    xb = xs[i % 3]; sb_ = sq[i % 2]
                    c0 = ts * 512
                    p.dma("sp", xb[:], xv[:, kc, t0 + c0:t0 + c0 + 512], writes=[("xs", i % 3)])
                    p.op("act", lambda e, xb=xb, sb_=sb_: e.activation(out=sb_[:], in_=xb[:], func=AF.Square),
                         reads=[("xs", i % 3)], writes=[("sq", i % 2)])
                    p.op("pe", lambda e, sb_=sb_, ts=ts, kc=kc: e.matmul(ss[ts][:], lhsT=ones[:], rhs=sb_[:], start=(kc == 0), stop=(kc == KC - 1)),
                         reads=[("sq", i % 2), "ones"], writes=[("ss", ts)])
                    p.op("dve", lambda e, xb=xb, kc=kc, c0=c0: e.tensor_scalar(out=hT[:, kc, c0:c0 + 512], in0=xb[:], scalar1=gs[:, kc:kc + 1], scalar2=None, op0=ALU.mult),
                         reads=[("xs", i % 3), "gs"], writes=[("hT", kc)])
            for ts in range(NTS):
                c0 = ts * 512
                p.op("act", lambda e, ts=ts, c0=c0: e.activation(out=rstd[:, c0:c0 + 512], in_=ss[ts][:], func=AF.Sqrt, scale=1.0 / K, bias=epsb[:, 0:1]),
                     reads=[("ss", ts), "epsb"], writes=[("rstd", ts)])
                p.op("dve", lambda e, c0=c0: e.reciprocal(out=rstd[:, c0:c0 + 512], in_=rstd[:, c0:c0 + 512]),
                     reads=[("rstd", ts)], writes=[("rstd", ts)])
        elif pro == "gate":
            gv = gT.rearrange("(c p) t -> p c t", p=128)
            for kc in range(KC):
                for ts in range(NTS):
                    i = ctr["x"]; ctr["x"] += 1
                    xb = xs[i % 2]; gb = gx[i % 2]
                    c0 = ts * 512
                    p.dma("sp", xb[:], xv[:, kc, t0 + c0:t0 + c0 + 512], writes=[("xs", i % 2)])
                    p.dma("sp", gb[:], gv[:, kc, t0 + c0:t0 + c0 + 512], writes=[("gx", i % 2)])
                    p.op("act", lambda e, gb=gb: e.activation(out=gb[:], in_=gb[:], func=AF.Silu),
                         reads=[("gx", i % 2)], writes=[("gx", i % 2)])
                    p.op("dve", lambda e, xb=xb, gb=gb, kc=kc, c0=c0: e.scalar_tensor_tensor(out=hT[:, kc, c0:c0 + 512], in0=xb[:], scalar=gs[:, kc:kc + 1], in1=gb[:], op0=ALU.mult, op1=ALU.mult),
                         reads=[("xs", i % 2), ("gx", i % 2), "gs"], writes=[("hT", kc)])
        for n in range(NCH):
            nw = min(128, N - n * 128)
            wi = ctr["w"]; ctr["w"] += 1
            ws = wi % NW
            for si, (k0, k1) in enumerate(ksplit):
                p.dma("pool", wb[ws][:, k0:k1, :nw], wv[:, k0:k1, n * 128:n * 128 + nw],
                      writes=[("wb", ws, si)])
            for ts in range(NTS):
                c0 = ts * 512
                pi = ctr["p"]; ctr["p"] += 1
                pb = pt[pi % NP]
                for si, (k0, k1) in enumerate(ksplit):
                    for kc in range(k0, k1):
                        p.op("pe", lambda e, pb=pb, ws=ws, kc=kc, nw=nw, c0=c0: e.matmul(pb[:nw, :], lhsT=wb[ws][:, kc, :nw], rhs=hT[:, kc, c0:c0 + 512], start=(kc == 0), stop=(kc == KC - 1)),
                             reads=[("wb", ws, si), ("hT", kc)], writes=[("pt", pi % NP)])
                oi = ctr["o"]; ctr["o"] += 1
                o_ = ob[oi % NO]
                if epi == "rstd":
                    p.op("dve", lambda e, o_=o_, pb=pb, nw=nw, c0=c0: e.tensor_tensor(out=o_[:nw, :], in0=pb[:nw, :], in1=rstd[:nw, c0:c0 + 512], op=ALU.mult),
                         reads=[("pt", pi % NP), ("rstd", ts)], writes=[("ob", oi % NO)])
                elif epi == "resid":
                    r_ = rb[oi % NO]
                    p.dma("sp", r_[:nw, :], rT[n * 128:n * 128 + nw, t0 + c0:t0 + c0 + 512], writes=[("rb", oi % NO)])
                    p.op("dve", lambda e, o_=o_, pb=pb, nw=nw, r_=r_: e.tensor_tensor(out=o_[:nw, :], in0=pb[:nw, :], in1=r_[:nw, :], op=ALU.add),
                         reads=[("pt", pi % NP), ("rb", oi % NO)], writes=[("ob", oi % NO)])
                else:
                    p.op("act", lambda e, o_=o_, pb=pb, nw=nw: e.copy(out=o_[:nw, :], in_=pb[:nw, :]),
                         reads=[("pt", pi % NP)], writes=[("ob", oi % NO)])
                p.dma("sp", oT[n * 128:n * 128 + nw, t0 + c0:t0 + c0 + 512], o_[:nw, :],
                      reads=[("ob", oi % NO)], is_output=True)
    return p.finish()

GN_EPS = 1e-5


def build_ffn_in(K, F, T, TT, WSPLIT=4):
    p = Prog()
    KC = K // 128
    FC = F // 128
    NPASS = T // TT
    NTS = TT // 512
    TW = TT + 2
    w = p.dram("w", [K, 2 * F], F32, "ExternalInput")
    xTh = p.dram("xTh", [K, NPASS, TW], F32, "ExternalInput")
    g = p.dram("g", [128, KC], F32, "ExternalInput")
    cw = p.dram("cw", [128, FC, 4], F32, "ExternalInput")
    oT = p.dram("oT", [F, T], F32, "ExternalOutput")

    gs = p.sb("gs", [128, KC], F32)
    p.dma("sp", gs[:], g, writes=["gs"])
    cws = p.sb("cws", [128, FC, 4], F32)
    p.dma("sp", cws[:], cw, writes=["cws"])
    ones = p.sb("ones", [128, 128], BF16)
    p.op("dve", lambda e: e.memset(ones[:], 1.0), writes=["ones"])
    epsb = p.sb("epsb", [128, 1], F32)
    p.op("dve", lambda e: e.memset(epsb[:], NORM_EPS), writes=["epsb"])

    hT = p.sb("hT", [128, KC, TW], BF16)
    rstd = p.sb("rstd", [128, TW], F32)
    xs = [p.sb("xs%d" % i, [128, 512], F32) for i in range(3)]
    sq = [p.sb("sq%d" % i, [128, 512], BF16) for i in range(2)]
    NW = 2
    wb = [p.sb("wb%d" % i, [128, KC, 256], BF16) for i in range(NW)]
    NB = 2
    Ab = [p.sb("A%d" % i, [128, TW], F32) for i in range(NB)]
    Ub = [p.sb("U%d" % i, [128, TT], F32) for i in range(NB)]
    Cb = [p.sb("C%d" % i, [128, TT], F32) for i in range(NB)]
    Ob = [p.sb("O%d" % i, [128, TT], F32) for i in range(NB)]
    NP = 4
    pt = [p.ps("pt%d" % i, [128, 512]) for i in range(NP)]
    ss = [p.ps("ss%d" % i, [128, 512]) for i in range(NTS + 1)]
    pieces = [(0, 2)] + [(2 + 512 * ts, 512) for ts in range(NTS)]
    xv = xTh.rearrange("(c p) n t -> p c n t", p=128)
    wv = w.rearrange("(c p) n -> p c n", p=128)
    ksplit = [(i * KC // WSPLIT, (i + 1) * KC // WSPLIT) for i in range(WSPLIT)]
    ctr = {"x": 0, "p": 0, "w": 0}

    for tp in range(NPASS):
        for kc in range(KC):
            for pi_, (c0, cn) in enumerate(pieces):
                i = ctr["x"]; ctr["x"] += 1
                xb = xs[i % 3]; sb_ = sq[i % 2]
                p.dma("sp", xb[:, :cn], xv[:, kc, tp, c0:c0 + cn], writes=[("xs", i % 3)])
                p.op("act", lambda e, xb=xb, sb_=sb_, cn=cn: e.activation(out=sb_[:, :cn], in_=xb[:, :cn], func=AF.Square),
                     reads=[("xs", i % 3)], writes=[("sq", i % 2)])
                p.op("pe", lambda e, sb_=sb_, pi_=pi_, kc=kc, cn=cn: e.matmul(ss[pi_][:, :cn], lhsT=ones[:], rhs=sb_[:, :cn], start=(kc == 0), stop=(kc == KC - 1)),
                     reads=[("sq", i % 2), "ones"], writes=[("ss", pi_)])
                p.op("dve", lambda e, xb=xb, kc=kc, c0=c0, cn=cn: e.tensor_scalar(out=hT[:, kc, c0:c0 + cn], in0=xb[:, :cn], scalar1=gs[:, kc:kc + 1], scalar2=None, op0=ALU.mult),
                     reads=[("xs", i % 3), "gs"], writes=[("hT", kc)])
        for pi_, (c0, cn) in enumerate(pieces):
            p.op("act", lambda e, pi_=pi_, c0=c0, cn=cn: e.activation(out=rstd[:, c0:c0 + cn], in_=ss[pi_][:, :cn], func=AF.Sqrt, scale=1.0 / K, bias=epsb[:, 0:1]),
                 reads=[("ss", pi_), "epsb"], writes=[("rstd", pi_)])
            p.op("dve", lambda e, c0=c0, cn=cn: e.reciprocal(out=rstd[:, c0:c0 + cn], in_=rstd[:, c0:c0 + cn]),
                 reads=[("rstd", pi_)], writes=[("rstd", pi_)])
        for fc in range(FC):
            wi = ctr["w"]; ctr["w"] += 1
            ws = wi % NW
            bi = wi % NB
            A, U, C, O = Ab[bi], Ub[bi], Cb[bi], Ob[bi]
            for half, col0 in ((0, fc * 128), (1, F + fc * 128)):
                for si, (k0, k1) in enumerate(ksplit):
                    p.dma("pool", wb[ws][:, k0:k1, half * 128:(half + 1) * 128], wv[:, k0:k1, col0:col0 + 128],
                          writes=[("wb", ws, half, si)])
            for pi_, (c0, cn) in enumerate(pieces):
                halves = (0,) if pi_ == 0 else (0, 1)
                for half in halves:
                    qi = ctr["p"]; ctr["p"] += 1
                    pb = pt[qi % NP]
                    for si, (k0, k1) in enumerate(ksplit):
                        for kc in range(k0, k1):
                            p.op("pe", lambda e, pb=pb, ws=ws, kc=kc, half=half, c0=c0, cn=cn: e.matmul(pb[:, :cn], lhsT=wb[ws][:, kc, half * 128:(half + 1) * 128], rhs=hT[:, kc, c0:c0 + cn], start=(kc == 0), stop=(kc == KC - 1)),
                                 reads=[("wb", ws, half, si), ("hT", kc)], writes=[("pt", qi % NP)])
                    if half == 0:
                        p.op("dve", lambda e, A=A, pb=pb, c0=c0, cn=cn: e.tensor_tensor(out=A[:, c0:c0 + cn], in0=pb[:, :cn], in1=rstd[:, c0:c0 + cn], op=ALU.mult),
                             reads=[("pt", qi % NP), ("rstd", pi_)], writes=[("A", bi)])
                    else:
                        p.op("dve", lambda e, U=U, pb=pb, c0=c0, cn=cn: e.tensor_tensor(out=U[:, c0 - 2:c0 - 2 + cn], in0=pb[:, :cn], in1=rstd[:, c0:c0 + cn], op=ALU.mult),
                             reads=[("pt", qi % NP), ("rstd", pi_)], writes=[("U", bi)])
            p.op("dve", lambda e, A=A, C=C, fc=fc: e.tensor_scalar(out=C[:], in0=A[:, 0:TT], scalar1=cws[:, fc, 0:1], scalar2=cws[:, fc, 3:4], op0=ALU.mult, op1=ALU.add),
                 reads=[("A", bi), "cws"], writes=[("C", bi)])
            p.op("dve", lambda e, A=A, C=C, fc=fc: e.scalar_tensor_tensor(out=C[:], in0=A[:, 1:TT + 1], scalar=cws[:, fc, 1:2], in1=C[:], op0=ALU.mult, op1=ALU.add),
                 reads=[("A", bi), "cws", ("C", bi)], writes=[("C", bi)])
            p.op("dve", lambda e, A=A, C=C, fc=fc: e.scalar_tensor_tensor(out=C[:], in0=A[:, 2:TT + 2], scalar=cws[:, fc, 2:3], in1=C[:], op0=ALU.mult, op1=ALU.add),
                 reads=[("A", bi), "cws", ("C", bi)], writes=[("C", bi)])
            p.op("act", lambda e, C=C: e.activation(out=C[:], in_=C[:], func=AF.Silu),
                 reads=[("C", bi)], writes=[("C", bi)])
            p.op("pool", lambda e, C=C, U=U, O=O: e.tensor_tensor(out=O[:], in0=C[:], in1=U[:], op=ALU.mult),
                 reads=[("C", bi), ("U", bi)], writes=[("O", bi)])
            p.dma("sp", oT[fc * 128:(fc + 1) * 128, tp * TT:(tp + 1) * TT], O[:], reads=[("O", bi)], is_output=True)
    return p.finish()


def build_final_norm(K, T):
    p = Prog()
    KC = K // 128
    xT = p.dram("xT", [K, T], F32, "ExternalInput")
    g = p.dram("g", [128, KC], F32, "ExternalInput")
    oT = p.dram("oT", [K, T], F32, "ExternalOutput")
    gs = p.sb("gs", [128, KC], F32)
    p.dma("sp", gs[:], g, writes=["gs"])
    ones = p.sb("ones", [128, 128], BF16)
    p.op("dve", lambda e: e.memset(ones[:], 1.0), writes=["ones"])
    epsb = p.sb("epsb", [128, 1], F32)
    p.op("dve", lambda e: e.memset(epsb[:], NORM_EPS), writes=["epsb"])
    xa = [p.sb("xa%d" % i, [128, KC, 512], F32) for i in range(2)]
    sq = [p.sb("sq%d" % i, [128, 512], BF16) for i in range(2)]
    ob = [p.sb("ob%d" % i, [128, 512], F32) for i in range(3)]
    rstd = [p.sb("rstd%d" % i, [128, 512], F32) for i in range(2)]
    ss = [p.ps("ss%d" % i, [128, 512]) for i in range(2)]
    xv = xT.rearrange("(c p) t -> p c t", p=128)
    i = 0
    oi = 0
    for ts in range(T // 512):
        b = ts % 2
        c0 = ts * 512
        for kq in range(4):
            k0, k1 = kq * KC // 4, (kq + 1) * KC // 4
            p.dma("sp", xa[b][:, k0:k1, :], xv[:, k0:k1, c0:c0 + 512], writes=[("xa", b, kq)])
        for kc in range(KC):
            kq = kc * 4 // KC
            sb_ = sq[i % 2]
            p.op("act", lambda e, sb_=sb_, kc=kc, b=b: e.activation(out=sb_[:], in_=xa[b][:, kc, :], func=AF.Square),
                 reads=[("xa", b, kq)], writes=[("sq", i % 2)])
            p.op("pe", lambda e, sb_=sb_, kc=kc, b=b: e.matmul(ss[b][:], lhsT=ones[:], rhs=sb_[:], start=(kc == 0), stop=(kc == KC - 1)),
                 reads=[("sq", i % 2), "ones"], writes=[("ss", b)])
            i += 1
        p.op("act", lambda e, b=b: e.activation(out=rstd[b][:], in_=ss[b][:], func=AF.Sqrt, scale=1.0 / K, bias=epsb[:, 0:1]),
             reads=[("ss", b), "epsb"], writes=[("rstd", b)])
        p.op("dve", lambda e, b=b: e.reciprocal(out=rstd[b][:], in_=rstd[b][:]), reads=[("rstd", b)], writes=[("rstd", b)])
        for kc in range(KC):
            kq = kc * 4 // KC
            o_ = ob[oi % 3]
            p.op("dve", lambda e, o_=o_, kc=kc, b=b: e.scalar_tensor_tensor(out=o_[:], in0=xa[b][:, kc, :], scalar=gs[:, kc:kc + 1], in1=rstd[b][:], op0=ALU.mult, op1=ALU.mult),
                 reads=[("xa", b, kq), "gs", ("rstd", b)], writes=[("ob", oi % 3)])
            p.dma("sp", oT[kc * 128:(kc + 1) * 128, c0:c0 + 512], o_[:], reads=[("ob", oi % 3)], is_output=True)
            oi += 1
    return p.finish()


def build_retention(H, S, CG=4):
    p = Prog()
    NCK = S // 128
    D = 256
    qT = p.dram("qT", [H * D, S], F32, "ExternalInput")
    kT = p.dram("kT", [H * D, S], F32, "ExternalInput")
    ktok = p.dram("ktok", [S, H * D], F32, "ExternalInput")
    vtok = p.dram("vtok", [S, H * D], F32, "ExternalInput")
    dmt = p.dram("dmt", [128, H, 128], F32, "ExternalInput")
    qd = p.dram("qd", [128, H, 128], F32, "ExternalInput")
    kd = p.dram("kd", [128, H], F32, "ExternalInput")
    cd = p.dram("cd", [128, H], F32, "ExternalInput")
    yn = p.dram("yn", [S, H * D], F32, "ExternalOutput")

    dmts = p.sb("dmts", [128, H, 128], F32)
    qds = p.sb("qds", [128, H, 128], F32)
    kds = p.sb("kds", [128, H], F32)
    cds = p.sb("cds", [128, H], F32)
    p.dma("sp", dmts[:], dmt, writes=["dmts"])
    p.dma("sp", qds[:], qd, writes=["qds"])
    p.dma("sp", kds[:], kd, writes=["kds"])
    p.dma("sp", cds[:], cd, writes=["cds"])
    epsb = p.sb("epsb", [128, 1], F32)
    p.op("dve", lambda e: e.memset(epsb[:], GN_EPS), writes=["epsb"])

    W = CG * 128
    qg = [p.sb("qg%d" % i, [128, 2 * H, W], BF16) for i in range(2)]
    kg = [p.sb("kg%d" % i, [128, 2 * H, W], BF16) for i in range(2)]
    kt = [p.sb("kt%d" % i, [128, H * D], BF16) for i in range(2)]
    vt = [p.sb("vt%d" % i, [128, H * D], BF16) for i in range(2)]
    Y = [p.sb("Y%d" % i, [128, H * D], F32) for i in range(2)]
    St = [p.sb("S%d" % h, [128, 2, D], F32) for h in range(H)]
    Sb = [p.sb("Sb%d" % h, [128, 2, D], BF16) for h in range(H)]
    PT = [p.sb("PT%d" % i, [128, 128], BF16) for i in range(2)]
    QD = [p.sb("QD%d" % i, [128, 2, 128], BF16) for i in range(2)]
    KDb = [p.sb("KD%d" % i, [128, D], BF16) for i in range(2)]
    st6 = [p.sb("st6_%d" % i, [128, 6], F32) for i in range(2)]
    mv = [p.sb("mv%d" % i, [128, 2], F32) for i in range(2)]
    rs = [p.sb("rs%d" % i, [128, 1], F32) for i in range(2)]
    pA = [p.ps("pA%d" % i, [128, 128]) for i in range(2)]
    pY = [p.ps("pY%d" % i, [128, D]) for i in range(2)]
    pS = [p.ps("pS%d" % i, [128, 2, D]) for i in range(2)]
    qv = qT.rearrange("(x p) s -> p x s", p=128)
    kv = kT.rearrange("(x p) s -> p x s", p=128)
    u = 0
    for c in range(NCK):
        cgi = c // CG
        gb = cgi % 2
        cc = c % CG
        if cc == 0:
            for hh in range(0, 2 * H, 4):
                p.dma("pool", qg[gb][:, hh:hh + 4, :], qv[:, hh:hh + 4, cgi * W:(cgi + 1) * W], writes=[("qg", gb, hh)])
                p.dma("pool", kg[gb][:, hh:hh + 4, :], kv[:, hh:hh + 4, cgi * W:(cgi + 1) * W], writes=[("kg", gb, hh)])
        cb = c % 2
        p.dma("pool", kt[cb][:], ktok[c * 128:(c + 1) * 128, :], writes=[("kt", cb)])
        p.dma("pool", vt[cb][:], vtok[c * 128:(c + 1) * 128, :], writes=[("vt", cb)])
        cs = slice(cc * 128, (cc + 1) * 128)
        for h in range(H):
            b = u % 2
            u += 1
            hk = (2 * h) // 4 * 4
            for dc in range(2):
                p.op("pe", lambda e, b=b, h=h, dc=dc, gb=gb, cs=cs: e.matmul(pA[b][:], lhsT=kg[gb][:, 2 * h + dc, cs], rhs=qg[gb][:, 2 * h + dc, cs], start=(dc == 0), stop=(dc == 1)),
                     reads=[("kg", gb, hk), ("qg", gb, hk)], writes=[("pA", b)])
            p.op("dve", lambda e, b=b, h=h: e.tensor_tensor(out=PT[b][:], in0=pA[b][:], in1=dmts[:, h, :], op=ALU.mult),
                 reads=[("pA", b), "dmts"], writes=[("PT", b)])
            if c > 0:
                for dc in range(2):
                    p.op("dve", lambda e, b=b, h=h, dc=dc, gb=gb, cs=cs: e.tensor_tensor(out=QD[b][:, dc, :], in0=qg[gb][:, 2 * h + dc, cs], in1=qds[:, h, :], op=ALU.mult),
                         reads=[("qg", gb, hk), "qds"], writes=[("QD", b, dc)])
            p.op("pe", lambda e, b=b, h=h, cb=cb: e.matmul(pY[b][:], lhsT=PT[b][:], rhs=vt[cb][:, h * D:(h + 1) * D], start=True, stop=(c == 0)),
                 reads=[("PT", b), ("vt", cb)], writes=[("pY", b)])
            if c > 0:
                for dc in range(2):
                    p.op("pe", lambda e, b=b, h=h, dc=dc: e.matmul(pY[b][:], lhsT=QD[b][:, dc, :], rhs=Sb[h][:, dc, :], start=False, stop=(dc == 1)),
                         reads=[("QD", b, dc), ("Sb", h)], writes=[("pY", b)])
            p.op("dve", lambda e, b=b: e.bn_stats(out=st6[b][:], in_=pY[b][:]), reads=[("pY", b)], writes=[("st6", b)])
            p.op("dve", lambda e, b=b: e.bn_aggr(out=mv[b][:], in_=st6[b][:]), reads=[("st6", b)], writes=[("mv", b)])
            p.op("act", lambda e, b=b: e.activation(out=rs[b][:], in_=mv[b][:, 1:2], func=AF.Sqrt, bias=epsb[:, 0:1]),
                 reads=[("mv", b), "epsb"], writes=[("rs", b)])
            p.op("dve", lambda e, b=b: e.reciprocal(out=rs[b][:], in_=rs[b][:]), reads=[("rs", b)], writes=[("rs", b)])
            p.op("dve", lambda e, b=b, h=h, cb=cb: e.tensor_scalar(out=Y[cb][:, h * D:(h + 1) * D], in0=pY[b][:], scalar1=mv[b][:, 0:1], scalar2=rs[b][:, 0:1], op0=ALU.subtract, op1=ALU.mult),
                 reads=[("pY", b), ("mv", b), ("rs", b)], writes=[("Y", cb, h)])
            if c < NCK - 1:
                p.op("pool", lambda e, b=b, h=h, cb=cb: e.tensor_scalar(out=KDb[b][:], in0=kt[cb][:, h * D:(h + 1) * D], scalar1=kds[:, h:h + 1], scalar2=None, op0=ALU.mult),
                     reads=[("kt", cb), "kds"], writes=[("KD", b)])
                for dc in range(2):
                    p.op("pe", lambda e, b=b, h=h, dc=dc, cb=cb: e.matmul(pS[b][:, dc, :], lhsT=KDb[b][:, dc * 128:(dc + 1) * 128], rhs=vt[cb][:, h * D:(h + 1) * D], start=True, stop=True),
                         reads=[("KD", b), ("vt", cb)], writes=[("pS", b)])
                if c == 0:
                    p.op("dve", lambda e, b=b, h=h: e.tensor_copy(out=St[h][:], in_=pS[b][:]), reads=[("pS", b)], writes=[("S", h)])
                else:
                    p.op("dve", lambda e, b=b, h=h: e.scalar_tensor_tensor(out=St[h][:], in0=St[h][:], scalar=cds[:, h:h + 1], in1=pS[b][:], op0=ALU.mult, op1=ALU.add),
                         reads=[("pS", b), ("S", h), "cds"], writes=[("S", h)])
                p.op("act", lambda e, h=h: e.copy(out=Sb[h][:], in_=St[h][:]), reads=[("S", h)], writes=[("Sb", h)])
        p.dma("sp", yn[c * 128:(c + 1) * 128, :], Y[cb][:], reads=[("Y", cb, h) for h in range(H)], is_output=True)
    return p.finish()

SCALE = 128 ** -0.5
NEG = -1.0e9


def nsa_consts(slopes):
    NH = len(slopes)
    pp = np.arange(128, dtype=np.float64)[:, None]
    c = np.arange(512, dtype=np.float64)[None, :]
    dt = np.zeros((128, 6, 512), np.float32)
    dt[:, 0, :] = -(pp - c)
    for k in range(4):
        dist = pp - c + 128 * k
        dt[:, 1 + k, :] = np.where(dist >= 0, -dist, NEG)
    dist = pp - c + 512
    dt[:, 5, :] = np.where(dist < 512, -dist, NEG)
    u = np.arange(255, dtype=np.float64)[None, :]
    dist = pp - 16 * (u - 248) - 31
    dc = np.where(dist >= 0, -dist, NEG).astype(np.float32)
    jp = np.arange(126)[None, :] - 62
    hi = (np.arange(128)[:, None] >= 64).astype(np.int64)
    valid = jp <= hi
    forced = (jp == hi) | (jp == hi - 1)
    mm = (valid & ~forced).astype(np.float32)
    ma = np.where(forced, 1e9, np.where(valid, 0.0, -1e9)).astype(np.float32)
    slsc = np.tile((slopes / SCALE)[None, :], (128, 1)).astype(np.float32)
    deltas = 512 + 128 * np.arange(28)
    tab = np.tile((-slopes[:, None] * deltas[None, :])[None], (128, 1, 1)).astype(np.float32)
    ident = np.eye(128, dtype=np.float32)
    return dict(dt=dt, dc=dc, mm=mm, ma=ma, slsc=slsc, tab=tab, ident=ident)


def build_nsa(G, HG, S):
    p = Prog()
    NH = G * HG
    NT = S // 128
    NCMP = (S - 32) // 16 + 1
    qT = p.dram("qT", [NH * 128, S], F32, "ExternalInput")
    gate = p.dram("gate", [S, NH * 3], F32, "ExternalInput")
    kvT = p.dram("kvT", [4, G, 128, S], F32, "ExternalInput")
    kvtok = p.dram("kvtok", [2, G, S, 128], F32, "ExternalInput")
    w1 = p.dram("w1", [2, 4096, 128], F32, "ExternalInput")
    w2 = p.dram("w2", [2, 128, 128], F32, "ExternalInput")
    posT = p.dram("posT", [2, 128, 32], F32, "ExternalInput")
    dt_d = p.dram("dt", [128, 6, 512], F32, "ExternalInput")
    dc_d = p.dram("dc", [128, 255], F32, "ExternalInput")
    mm_d = p.dram("mm", [128, 126], F32, "ExternalInput")
    ma_d = p.dram("ma", [128, 126], F32, "ExternalInput")
    slsc_d = p.dram("slsc", [128, NH], F32, "ExternalInput")
    tab_d = p.dram("tab", [128, NH, 28], F32, "ExternalInput")
    ident_d = p.dram("ident", [128, 128], F32, "ExternalInput")
    out = p.dram("attn", [S, NH * 128], F32, "ExternalOutput")

    def const(name, shape, src, dtype=F32, q="sp"):
        t = p.sb(name + "_sb", shape, dtype)
        p.dma(q, t[:], src, writes=[name])
        return t
    Dt = const("Dt", [128, 6, 512], dt_d)
    Dc = const("Dc", [128, 255], dc_d)
    MM = const("MM", [128, 126], mm_d)
    MA = const("MA", [128, 126], ma_d)
    slsc = const("slsc", [128, NH], slsc_d)
    tab = const("tab", [128, NH, 28], tab_d)
    ident = const("ident", [128, 128], ident_d, BF16, "pool")
    w1s = const("w1s", [128, 2, 32, 128], w1.rearrange("y (l d) m -> d y l m", d=128), BF16, "pool")
    w2s = const("w2s", [128, 2, 128], w2.rearrange("y m n -> m y n"), BF16, "pool")
    poss = const("poss", [128, 2, 32], posT.rearrange("y d l -> d y l"), BF16, "pool")
    gt = p.sb("gt", [128, NT, NH * 3], F32)
    p.dma("sp", gt[:], gate.rearrange("(c p) n -> p c n", p=128), writes=["gt"])
    p.op("act", lambda e: e.activation(out=gt[:], in_=gt[:], func=AF.Sigmoid), reads=["gt"], writes=["gt"])

    qg = p.sb("qg", [128, HG, S], BF16)
    ksT = p.sb("ksT", [128, S], BF16)
    kwT = p.sb("kwT", [128, S], BF16)
    vs = p.sb("vs", [128, NT, 132], BF16)
    vw = p.sb("vw", [128, NT, 132], BF16)
    tokT = p.sb("tokT", [128, S], BF16)
    kcT = p.sb("kcT", [128, 256], BF16)
    vc = p.sb("vc", [128, 2, 128], BF16)
    s1T = p.sb("s1T", [128, 256], BF16)
    bc = p.sb("bc", [128, 1], F32)
    tmp = [p.sb("tmp%d" % i, [128, 512], F32) for i in range(2)]
    eb = [p.sb("eb%d" % i, [128, 512], BF16) for i in range(2)]
    eT = [p.sb("eT%d" % i, [128, 4, 128], BF16) for i in range(2)]
    ef = p.sb("ef", [128, 256], F32)
    pnb = p.sb("pnb", [128, 256], BF16)
    Pacc = p.sb("Pacc", [128, 264], F32)
    imp = p.sb("imp", [128, 64], F32)
    sc2 = p.sb("sc2", [128, 64], F32)
    t8 = p.sb("t8", [128, 8], F32)
    t8b = p.sb("t8b", [128, 8], F32)
    selm = p.sb("selm", [128, 64], BF16)
    sm = [p.sb("sm%d" % i, [128, 4], F32) for i in range(2)]
    acc = [p.sb("acc%d" % i, [128, HG, 128], F32) for i in range(2)]
    pS = [p.ps("pS%d" % i, [128, 512]) for i in range(2)]
    pT = [p.ps("pT%d" % i, [128, 4, 128], BF16) for i in range(2)]
    pO = [p.ps("pO%d" % i, [128, 132]) for i in range(2)]
    ctr = {"u": 0, "o": 0, "s": 0}

    p.op("dve", lambda e: e.memset(vs[:], 1.0), writes=["ksv"])
    p.op("dve", lambda e: e.memset(vw[:], 1.0), writes=["kwv"])

    def unit(h, hh, ti, kT, k0, c_lo, ncol, Dap, bias, mask, Vt, vch0, po, first, last, rk):
        u = ctr["u"]; ctr["u"] += 1
        b = u % 2
        ts = slice(ti * 128, (ti + 1) * 128)
        cw = ncol - c_lo
        p.op("pe", lambda e: e.matmul(pS[b][:, c_lo:ncol], lhsT=qg[:, h, ts], rhs=kT[:, k0 + c_lo:k0 + ncol], start=True, stop=True),
             reads=["qg", rk], writes=[("pS", b)])
        p.op("dve", lambda e: e.scalar_tensor_tensor(out=tmp[b][:, c_lo:ncol], in0=Dap[:, c_lo:ncol], scalar=slsc[:, hh:hh + 1], in1=pS[b][:, c_lo:ncol], op0=ALU.mult, op1=ALU.add),
             reads=[("pS", b), "Dt", "slsc"], writes=[("tmp", b)])
        if bias is None:
            p.op("act", lambda e: e.activation(out=eb[b][:, c_lo:ncol], in_=tmp[b][:, c_lo:ncol], func=AF.Exp, scale=SCALE),
                 reads=[("tmp", b)], writes=[("eb", b)])
        else:
            p.op("act", lambda e: e.activation(out=eb[b][:, c_lo:ncol], in_=tmp[b][:, c_lo:ncol], func=AF.Exp, scale=SCALE, bias=bias),
                 reads=[("tmp", b), "tab"], writes=[("eb", b)])
        if mask is not None:
            p.op("pool", lambda e: e.tensor_tensor(out=eb[b][:].rearrange("p (j l) -> p j l", l=64), in0=eb[b][:].rearrange("p (j l) -> p j l", l=64),
                                                   in1=mask.unsqueeze(2).to_broadcast([128, 8, 64]), op=ALU.mult),
                 reads=[("eb", b), "selm"], writes=[("eb", b)])
        kc0 = c_lo // 128
        nkc = ncol // 128
        for kc in range(kc0, nkc):
            p.op("pe", lambda e, kc=kc: e.transpose(pT[b][:, kc, :], eb[b][:, kc * 128:(kc + 1) * 128], ident[:]),
                 reads=[("eb", b), "ident"], writes=[("pT", b)])
        p.op("act", lambda e: e.copy(out=eT[b][:, kc0:nkc, :], in_=pT[b][:, kc0:nkc, :]), reads=[("pT", b)], writes=[("eT", b)])
        for kc in range(kc0, nkc):
            p.op("pe", lambda e, kc=kc: e.matmul(po[0][:, 0:130], lhsT=eT[b][:, kc, :], rhs=Vt[:, vch0 + kc, 0:130], start=(first and kc == kc0), stop=(last and kc == nkc - 1)),
                 reads=[("eT", b), rk + "v"], writes=[po[1]])

    for g in range(G):
        for hq in range(0, HG, 2):
            p.dma("pool", qg[:, hq:hq + 2, :], qT.rearrange("(x p) s -> p x s", p=128)[:, g * HG + hq:g * HG + hq + 2, :], writes=["qg"])
        p.dma("pool", ksT[:], kvT[2, g], writes=["ks"])
        p.dma("pool", kwT[:], kvT[3, g], writes=["kw"])
        p.dma("pool", vs[:, :, 0:128], kvtok[0, g].rearrange("(c p) d -> p c d", p=128), writes=["ksv"])
        p.dma("pool", vw[:, :, 0:128], kvtok[1, g].rearrange("(c p) d -> p c d", p=128), writes=["kwv"])
        for ty in range(2):
            p.dma("pool", tokT[:], kvT[ty, g], writes=["tokT"])
            for l in range(32):
                p.op("pe", lambda e, l=l: e.matmul(pS[0][:, 0:1], lhsT=w1s[:, ty, l, :], rhs=poss[:, ty, l:l + 1], start=(l == 0), stop=(l == 31)),
                     reads=["w1s", "poss"], writes=[("pS", 0)])
            p.op("dve", lambda e: e.tensor_copy(out=bc[:], in_=pS[0][:, 0:1]), reads=[("pS", 0)], writes=["bc"])
            for l in range(32):
                p.op("pe", lambda e, l=l: e.matmul(pS[1][:, 0:NCMP], lhsT=w1s[:, ty, l, :], rhs=tokT[:, l:l + 16 * (NCMP - 1) + 1:16], start=(l == 0), stop=(l == 31)),
                     reads=["w1s", "tokT"], writes=[("pS", 1)])
            p.op("act", lambda e: e.activation(out=s1T[:, 0:NCMP], in_=pS[1][:, 0:NCMP], func=AF.Silu, bias=bc[:, 0:1]),
                 reads=[("pS", 1), "bc"], writes=["s1T"])
            if ty == 0:
                p.op("pe", lambda e: e.matmul(pS[0][:, 0:NCMP], lhsT=w2s[:, 0, :], rhs=s1T[:, 0:NCMP], start=True, stop=True),
                     reads=["w2s", "s1T"], writes=[("pS", 0)])
                p.op("dve", lambda e: e.tensor_copy(out=kcT[:, 0:NCMP], in_=pS[0][:, 0:NCMP]), reads=[("pS", 0)], writes=["kcT"])
            else:
                p.op("dve", lambda e: e.memset(vc[:], 0.0), writes=["vc"])
                for ch in range((NCMP + 127) // 128):
                    n0 = ch * 128
                    nn = min(128, NCMP - n0)
                    p.op("pe", lambda e, n0=n0, nn=nn: e.matmul(pS[0][:nn, 0:128], lhsT=s1T[:, n0:n0 + nn], rhs=w2s[:, 1, :], start=True, stop=True),
                         reads=["w2s", "s1T"], writes=[("pS", 0)])
                    p.op("dve", lambda e, ch=ch, nn=nn: e.tensor_copy(out=vc[:nn, ch, :], in_=pS[0][:nn, 0:128]), reads=[("pS", 0)], writes=["vc"])
        for ti in range(NT):
            ab = ctr["o"] % 2; ctr["o"] += 1
            A = acc[ab]
            ts = slice(ti * 128, (ti + 1) * 128)
            n_hi = min(NCMP, 8 * ti + 7)
            u0 = 248 - 8 * ti
            p.op("dve", lambda e: e.memset(Pacc[:], 0.0), writes=["Pacc"])
            for h in range(HG):
                hh = g * HG + h
                si = ctr["s"] % 2; ctr["s"] += 1
                s_ = sm[si]
                p.op("pe", lambda e, h=h: e.matmul(pS[0][:, 0:n_hi], lhsT=qg[:, h, ts], rhs=kcT[:, 0:n_hi], start=True, stop=True),
                     reads=["qg", "kcT"], writes=[("pS", 0)])
                p.op("dve", lambda e, hh=hh: e.scalar_tensor_tensor(out=tmp[0][:, 0:n_hi], in0=Dc[:, u0:u0 + n_hi], scalar=slsc[:, hh:hh + 1], in1=pS[0][:, 0:n_hi], op0=ALU.mult, op1=ALU.add),
                     reads=[("pS", 0), "Dc", "slsc"], writes=[("tmp", 0)])
                p.op("dve", lambda e: e.reduce_max(out=s_[:, 0:1], in_=tmp[0][:, 0:n_hi], axis=AX.X), reads=[("tmp", 0)], writes=[("sm", si)])
                p.op("dve", lambda e: e.tensor_scalar(out=s_[:, 0:1], in0=s_[:, 0:1], scalar1=-1.0e6, scalar2=-SCALE, op0=ALU.max, op1=ALU.mult),
                     reads=[("sm", si)], writes=[("sm", si)])
                p.op("act", lambda e: e.activation(out=ef[:, 0:n_hi], in_=tmp[0][:, 0:n_hi], func=AF.Exp, scale=SCALE, bias=s_[:, 0:1], accum_out=s_[:, 1:2]),
                     reads=[("tmp", 0), ("sm", si)], writes=["ef", ("sm", si)])
                p.op("dve", lambda e: e.tensor_scalar(out=s_[:, 1:2], in0=s_[:, 1:2], scalar1=1.0e-30, scalar2=None, op0=ALU.max),
                     reads=[("sm", si)], writes=[("sm", si)])
                p.op("dve", lambda e: e.reciprocal(out=s_[:, 1:2], in_=s_[:, 1:2]), reads=[("sm", si)], writes=[("sm", si)])
                p.op("dve", lambda e: e.scalar_tensor_tensor(out=Pacc[:, 1:1 + n_hi], in0=ef[:, 0:n_hi], scalar=s_[:, 1:2], in1=Pacc[:, 1:1 + n_hi], op0=ALU.mult, op1=ALU.add),
                     reads=["ef", ("sm", si), "Pacc"], writes=["Pacc"])
                p.op("dve", lambda e: e.memset(pnb[:], 0.0), writes=["pnb"])
                p.op("dve", lambda e: e.tensor_scalar(out=pnb[:, 0:n_hi], in0=ef[:, 0:n_hi], scalar1=s_[:, 1:2], scalar2=None, op0=ALU.mult),
                     reads=["ef", ("sm", si)], writes=["pnb"])
                nch = (n_hi + 127) // 128
                for ch in range(nch):
                    p.op("pe", lambda e, ch=ch: e.transpose(pT[0][:, ch, :], pnb[:, ch * 128:(ch + 1) * 128], ident[:]),
                         reads=["pnb", "ident"], writes=[("pT", 0)])
                p.op("act", lambda e: e.copy(out=eT[0][:, 0:nch, :], in_=pT[0][:, 0:nch, :]), reads=[("pT", 0)], writes=[("eT", 0)])
                for ch in range(nch):
                    p.op("pe", lambda e, ch=ch: e.matmul(pO[0][:, 0:128], lhsT=eT[0][:, ch, :], rhs=vc[:, ch, :], start=(ch == 0), stop=(ch == nch - 1)),
                         reads=[("eT", 0), "vc"], writes=[("pO", 0)])
                p.op("dve", lambda e, h=h, hh=hh: e.tensor_scalar(out=A[:, h, :], in0=pO[0][:, 0:128], scalar1=gt[:, ti, hh * 3:hh * 3 + 1], scalar2=None, op0=ALU.mult),
                     reads=[("pO", 0), "gt"], writes=[("acc", ab, h)])
            Pv = Pacc[:, 0:256].rearrange("p (j k) -> p j k", k=4)
            Pv4 = Pacc[:, 4:260].rearrange("p (j k) -> p j k", k=4)
            p.op("dve", lambda e: e.scalar_tensor_tensor(out=imp[:], in0=Pv[:, :, 1], scalar=2.0, in1=Pv[:, :, 0], op0=ALU.mult, op1=ALU.add), reads=["Pacc"], writes=["imp"])
            p.op("dve", lambda e: e.scalar_tensor_tensor(out=imp[:], in0=Pv[:, :, 2], scalar=2.0, in1=imp[:], op0=ALU.mult, op1=ALU.add), reads=["Pacc", "imp"], writes=["imp"])
            p.op("dve", lambda e: e.scalar_tensor_tensor(out=imp[:], in0=Pv[:, :, 3], scalar=2.0, in1=imp[:], op0=ALU.mult, op1=ALU.add), reads=["Pacc", "imp"], writes=["imp"])
            p.op("dve", lambda e: e.tensor_tensor(out=imp[:], in0=imp[:], in1=Pv4[:, :, 0], op=ALU.add), reads=["Pacc", "imp"], writes=["imp"])
            m0 = 62 - 2 * ti
            p.op("dve", lambda e: e.tensor_tensor(out=imp[:], in0=imp[:], in1=MM[:, m0:m0 + 64], op=ALU.mult), reads=["imp", "MM"], writes=["imp"])
            p.op("dve", lambda e: e.tensor_tensor(out=imp[:], in0=imp[:], in1=MA[:, m0:m0 + 64], op=ALU.add), reads=["imp", "MA"], writes=["imp"])
            p.op("dve", lambda e: e.memset(imp[:, 0:1], 1.0e9), reads=["imp"], writes=["imp"])
            p.op("dve", lambda e: e.max(out=t8[:], in_=imp[:]), reads=["imp"], writes=["t8"])
            p.op("dve", lambda e: e.match_replace(out=sc2[:], in_to_replace=t8[:], in_values=imp[:], imm_value=-3.0e9), reads=["imp", "t8"], writes=["sc2"])
            p.op("dve", lambda e: e.max(out=t8b[:], in_=sc2[:]), reads=["sc2"], writes=["t8b"])
            p.op("dve", lambda e: e.tensor_scalar(out=selm[:], in0=imp[:], scalar1=t8b[:, 7:8], scalar2=None, op0=ALU.is_ge), reads=["imp", "t8b"], writes=["selm"])
            for h in range(HG):
                hh = g * HG + h
                for br in (1, 2):
                    ob = ctr["s"] % 2; ctr["s"] += 1
                    po = (pO[ob], ("pO", ob))
                    units = []
                    if br == 1:
                        nk = ti // 4
                        for kt in range(nk + 1):
                            if kt == nk:
                                units.append(dict(kT=ksT, k0=512 * kt, c_lo=0, ncol=128 * (ti % 4) + 128, Dap=Dt[:, 1 + ti % 4, :], bias=None,
                                                  mask=selm[:, 8 * kt:8 * kt + 8], Vt=vs, vch0=4 * kt, rk="ks"))
                            else:
                                di = (128 * ti - 512 * kt - 512) // 128
                                units.append(dict(kT=ksT, k0=512 * kt, c_lo=0, ncol=512, Dap=Dt[:, 0, :], bias=tab[:, hh, di:di + 1],
                                                  mask=selm[:, 8 * kt:8 * kt + 8], Vt=vs, vch0=4 * kt, rk="ks"))
                    else:
                        if ti >= 1:
                            c_lo = max(0, 512 - 128 * ti)
                            units.append(dict(kT=kwT, k0=128 * ti - 512, c_lo=c_lo, ncol=512, Dap=Dt[:, 5, :], bias=None, mask=None, Vt=vw, vch0=ti - 4, rk="kw"))
                        units.append(dict(kT=kwT, k0=128 * ti, c_lo=0, ncol=128, Dap=Dt[:, 1, :], bias=None, mask=None, Vt=vw, vch0=ti, rk="kw"))
                    for ui, ud in enumerate(units):
                        unit(h, hh, ti, po=po, first=(ui == 0), last=(ui == len(units) - 1), **ud)
                    s_ = sm[ob]
                    p.op("dve", lambda e, ob=ob, s_=s_: e.reciprocal(out=s_[:, 2:3], in_=pO[ob][:, 128:129]), reads=[("pO", ob)], writes=[("sm2", ob)])
                    p.op("dve", lambda e, s_=s_, hh=hh, br=br: e.tensor_tensor(out=s_[:, 3:4], in0=s_[:, 2:3], in1=gt[:, ti, hh * 3 + br:hh * 3 + br + 1], op=ALU.mult),
                         reads=[("sm2", ob), "gt"], writes=[("sm3", ob)])
                    p.op("dve", lambda e, ob=ob, s_=s_, h=h: e.scalar_tensor_tensor(out=A[:, h, :], in0=pO[ob][:, 0:128], scalar=s_[:, 3:4], in1=A[:, h, :], op0=ALU.mult, op1=ALU.add),
                         reads=[("pO", ob), ("sm3", ob), ("acc", ab, h)], writes=[("acc", ab, h)])
            p.dma("sp", out[ti * 128:(ti + 1) * 128, g * HG * 128:(g + 1) * HG * 128], A[:].rearrange("p h d -> p (h d)"),
                  reads=[("acc", ab, h) for h in range(HG)], is_output=True)
    return p.finish()


from concourse.bass_utils import run_bass_kernel_spmd

D_MODEL = 4096
SEQ = 4096
BATCH = 4
D_FF = 11008
NCORES = 8
HALF = SEQ // 2


def _gl(v):
    v = np.asarray(v, np.float32)
    return np.ascontiguousarray(v.reshape(-1, 128).T)


def _run(nc, ins, key):
    res = run_bass_kernel_spmd(nc, ins, core_ids=list(range(NCORES)))
    return [np.asarray(r[key]) for r in res.results]


def _ret_consts(heads):
    D = 256
    lg = np.log1p(-np.exp2(-5.0 - np.asarray(heads, dtype=np.float64)))
    i = np.arange(128, dtype=np.float64)
    scale = D ** -0.5
    Hn = len(heads)
    dmt = np.zeros((128, Hn, 128), np.float32)
    qd = np.zeros((128, Hn, 128), np.float32)
    kd = np.zeros((128, Hn), np.float32)
    cd = np.zeros((128, Hn), np.float32)
    for a, l in enumerate(lg):
        diff = i[None, :] - i[:, None]
        dmt[:, a, :] = np.where(diff >= 0, np.exp(np.maximum(diff, 0) * l), 0.0) * scale
        qd[:, a, :] = np.exp((i + 1.0) * l)[None, :]
        kd[:, a] = np.exp((127.0 - i) * l) * scale
        cd[:, a] = np.exp(128 * l)
    return dict(dmt=dmt, qd=qd, kd=kd, cd=cd)


def _ffn(xT_list, l, ffn_norm, w_ffn_in, conv_w, conv_b, w_ffn_out):
    TT = 1024
    cw = np.concatenate([conv_w[l], conv_b[l][None]], 0)
    cw = np.ascontiguousarray(cw.reshape(4, D_FF // 128, 128).transpose(2, 1, 0))
    g = _gl(ffn_norm[l])
    ins = []
    for c in range(NCORES):
        if c % 2 == 0:
            halo = np.zeros((D_MODEL, 2), np.float32)
        else:
            halo = xT_list[c - 1][:, -2:]
        xe = np.concatenate([halo, xT_list[c]], axis=1)
        xTh = np.stack([xe[:, pp * TT:pp * TT + TT + 2] for pp in range(HALF // TT)], axis=1)
        ins.append(dict(w=w_ffn_in[l], xTh=np.ascontiguousarray(xTh), g=g, cw=cw))
    gated = _run(build_ffn_in(D_MODEL, D_FF, HALF, TT), ins, "oT")
    ins = [dict(w=w_ffn_out[l], xT=gated[c], rT=xT_list[c]) for c in range(NCORES)]
    return _run(build_gemm(D_FF, D_MODEL, HALF, 512, "plain", "resid", WSPLIT=8), ins, "oT")


def kernel(x, attn_norm, ffn_norm, w_ret_in, ret_gn_gain, w_ret_out, kv_norm, w_kv,
           cmp_pos, cmp_w1, cmp_w2, w_nsa_q, w_nsa_out, w_ffn_in, conv_w, conv_b,
           w_ffn_out, final_norm):
    f32 = lambda a: np.asarray(a, np.float32)
    x = f32(x)
    (attn_norm, ffn_norm, w_ret_in, ret_gn_gain, w_ret_out, kv_norm, w_kv, cmp_pos, cmp_w1, cmp_w2,
     w_nsa_q, w_nsa_out, w_ffn_in, conv_w, conv_b, w_ffn_out, final_norm) = map(
        f32, (attn_norm, ffn_norm, w_ret_in, ret_gn_gain, w_ret_out, kv_norm, w_kv, cmp_pos, cmp_w1, cmp_w2,
              w_nsa_q, w_nsa_out, w_ffn_in, conv_w, conv_b, w_ffn_out, final_norm))
    cores = [(c // 2, c % 2) for c in range(NCORES)]
    xT = [np.ascontiguousarray(x[b, h * HALF:(h + 1) * HALF].T) for (b, h) in cores]

    ins = [dict(w=w_ret_in[0], xT=xT[c], g=_gl(attn_norm[0])) for c in range(NCORES)]
    proj = _run(build_gemm(D_MODEL, 4 * D_MODEL, HALF, HALF, "norm", "rstd"), ins, "oT")
    ins = []
    for c, (b, h) in enumerate(cores):
        pb = np.concatenate([proj[2 * b], proj[2 * b + 1]], axis=1)
        r0 = h * 2048
        qT_ = np.ascontiguousarray(pb[r0:r0 + 2048])
        kT_ = np.ascontiguousarray(pb[4096 + r0:4096 + r0 + 2048])
        d = dict(qT=qT_, kT=kT_, ktok=np.ascontiguousarray(kT_.T), vtok=np.ascontiguousarray(pb[8192 + r0:8192 + r0 + 2048].T))
        d.update(_ret_consts(list(range(h * 8, h * 8 + 8))))
        ins.append(d)
    yn = _run(build_retention(8, SEQ), ins, "yn")
    ins = []
    for c, (b, h) in enumerate(cores):
        yb = np.concatenate([yn[2 * b], yn[2 * b + 1]], axis=1)
        ins.append(dict(w=w_ret_out[0], xT=np.ascontiguousarray(yb[h * HALF:(h + 1) * HALF].T),
                        gT=np.ascontiguousarray(proj[c][12288:16384]), g=_gl(ret_gn_gain[0]), rT=xT[c]))
    x1 = _run(build_gemm(D_MODEL, D_MODEL, HALF, HALF, "gate", "resid"), ins, "oT")
    del proj, yn
    x2 = _ffn(x1, 0, ffn_norm, w_ffn_in, conv_w, conv_b, w_ffn_out)
    del x1

    ins = [dict(w=w_kv, xT=x2[c], g=_gl(kv_norm)) for c in range(NCORES)]
    kvp = _run(build_gemm(D_MODEL, 3072, HALF, HALF, "norm", "rstd"), ins, "oT")
    ins = [dict(w=w_nsa_q[0], xT=x2[c], g=_gl(attn_norm[1])) for c in range(NCORES)]
    qp = _run(build_gemm(D_MODEL, D_MODEL + 96, HALF, HALF, "norm", "rstd"), ins, "oT")
    ins = []
    for c, (b, h) in enumerate(cores):
        kb = np.concatenate([kvp[2 * b], kvp[2 * b + 1]], axis=1)
        qb = np.concatenate([qp[2 * b], qp[2 * b + 1]], axis=1)
        gs_ = [2 * h, 2 * h + 1]
        kvT = np.stack([np.stack([kb[j * 512 + g * 128:j * 512 + g * 128 + 128] for g in gs_]) for j in (0, 1, 2, 4)])
        kvtok = np.stack([np.stack([kb[j * 512 + g * 128:j * 512 + g * 128 + 128].T for g in gs_]) for j in (3, 5)])
        heads = np.arange(h * 16, h * 16 + 16, dtype=np.float64) + 1.0
        slopes = np.exp2(-8.0 * heads / 32.0)
        d = dict(qT=np.ascontiguousarray(qb[h * 2048:(h + 1) * 2048]),
                 gate=np.ascontiguousarray(qb[4096 + h * 48:4096 + (h + 1) * 48].T),
                 kvT=np.ascontiguousarray(kvT), kvtok=np.ascontiguousarray(kvtok),
                 w1=cmp_w1, w2=cmp_w2, posT=np.ascontiguousarray(cmp_pos.transpose(0, 2, 1)))
        d.update(nsa_consts(slopes))
        ins.append(d)
    attn = _run(build_nsa(2, 8, SEQ), ins, "attn")
    del kvp, qp
    ins = []
    for c, (b, h) in enumerate(cores):
        ab = np.concatenate([attn[2 * b], attn[2 * b + 1]], axis=1)
        ins.append(dict(w=w_nsa_out[0], xT=np.ascontiguousarray(ab[h * HALF:(h + 1) * HALF].T), rT=x2[c]))
    x3 = _run(build_gemm(D_MODEL, D_MODEL, HALF, HALF, "plain", "resid"), ins, "oT")
    del attn, x2
    x4 = _ffn(x3, 1, ffn_norm, w_ffn_in, conv_w, conv_b, w_ffn_out)
    del x3
    ins = [dict(xT=x4[c], g=_gl(final_norm)) for c in range(NCORES)]
    oT = _run(build_final_norm(D_MODEL, HALF), ins, "oT")
    out = np.empty((BATCH, SEQ, D_MODEL), np.float32)
    for c, (b, h) in enumerate(cores):
        out[b, h * HALF:(h + 1) * HALF] = oT[c].T
    return out
```

```python
import numpy as np
import concourse.bass as bass
import concourse.mybir as mybir
from contextlib import ExitStack

F32 = mybir.dt.float32
F32R = mybir.dt.float32r
BF16 = mybir.dt.bfloat16
AF = mybir.ActivationFunctionType
ALU = mybir.AluOpType
AX = mybir.AxisListType

CE = ("pe", "act", "dve", "pool")
NDMA = 48


class Prog:
    def __init__(self, name="k"):
        self.nc = bass.Bass("TRN2", target_bir_lowering=False)
        nc = self.nc
        self.stack = ExitStack()
        self.eng = {"pe": nc.tensor, "act": nc.scalar, "dve": nc.vector,
                    "pool": nc.gpsimd, "sp": nc.sync}
        self.sem = {e: self.stack.enter_context(nc.semaphore("s_" + e)) for e in CE}
        self.cnt = {e: 0 for e in CE}
        self.dsem = [self.stack.enter_context(nc.semaphore("d%d" % i)) for i in range(NDMA)]
        self.dcnt = [0] * NDMA
        self.dnext = 0
        self.know = {e: {} for e in self.eng}
        self.lastw = {}
        self.rd = {}
        self.nops = 0
        self.out_events = []
        self.pstack = None
        self.pid = 0
        self.sw_out = []
        self.sw_budget = 6000

    def dram(self, name, shape, dtype, kind):
        return self.nc.dram_tensor(name, list(shape), dtype, kind=kind).ap()

    def sb(self, name, shape, dtype):
        st = self.pstack if self.pstack is not None else self.stack
        return st.enter_context(self.nc.sbuf_tensor("p%d_%s" % (self.pid, name), list(shape), dtype))

    def ps(self, name, shape, dtype=F32):
        st = self.pstack if self.pstack is not None else self.stack
        return st.enter_context(self.nc.psum_tensor("p%d_%s" % (self.pid, name), list(shape), dtype))

    def begin_phase(self):
        self.pid += 1
        self.pstack = ExitStack()

    def end_phase(self):
        self.barrier()
        self.pstack.close()
        self.pstack = None

    def barrier(self):
        targets = [(e, self.cnt[e]) for e in CE if self.cnt[e] > 0]
        targets += [(i, self.dcnt[i]) for i in range(NDMA) if self.dcnt[i] > 0]
        for e in self.eng:
            kn = self.know[e]
            for sk, v in targets:
                if kn.get(sk, 0) < v:
                    self.eng[e].wait_ge(self._semh(sk), v)
                    kn[sk] = v
        self.lastw = {}
        self.rd = {}
        self.sw_out = []

    def _semh(self, k):
        return self.sem[k] if isinstance(k, str) else self.dsem[k]

    def _need(self, e, reads, writes):
        ev = []
        for k in reads:
            w = self.lastw.get(k)
            if w is not None:
                ev.append(w)
        for k in writes:
            w = self.lastw.get(k)
            if w is not None:
                ev.append(w)
            ev.extend(self.rd.get(k, ()))
        need = {}
        kn = self.know[e]
        for (sk, v, clk) in ev:
            if e == "pe" and sk == "pe":
                continue
            if kn.get(sk, 0) >= v:
                continue
            if need.get(sk, 0) < v:
                need[sk] = v
        return need, ev

    def _wait(self, e, need, ev):
        eng = self.eng[e]
        kn = self.know[e]
        for sk, v in need.items():
            eng.wait_ge(self._semh(sk), v)
            kn[sk] = v
        for (sk, v, clk) in ev:
            if clk:
                for ck, cv in clk.items():
                    if kn.get(ck, 0) < cv:
                        kn[ck] = cv

    def _record(self, event, reads, writes):
        for k in writes:
            self.lastw[k] = event
            self.rd[k] = []
        for k in reads:
            if k in writes:
                continue
            self.rd.setdefault(k, []).append(event)

    def op(self, e, fn, reads=(), writes=()):
        need, ev = self._need(e, reads, writes)
        self._wait(e, need, ev)
        ins = fn(self.eng[e])
        self.cnt[e] += 1
        ins.then_inc(self.sem[e], 1)
        kn = self.know[e]
        clk = {k: v for k, v in kn.items() if isinstance(k, str)}
        clk[e] = self.cnt[e]
        if e != "pe":
            pass
        event = (e, self.cnt[e], clk)
        self._record(event, reads, writes)
        self.nops += 1
        return event

    def dma(self, q, out, in_, reads=(), writes=(), is_output=False):
        need, ev = self._need(q, reads, writes)
        nd = 0
        if q == "pool":
            nd = 1
            for d_ in list(out.shape)[:-1]:
                nd *= int(d_)
            tot = sum(n for (_, n) in self.sw_out) + nd
            while self.sw_out and tot > self.sw_budget:
                (osk, ov, _), on = self.sw_out.pop(0)
                tot -= on
                if self.know[q].get(osk, 0) < ov:
                    need[osk] = max(need.get(osk, 0), ov)
        s = self.dnext
        self.dnext = (self.dnext + 1) % NDMA
        prev = self.dcnt[s]
        if prev > 0 and self.know[q].get(s, 0) < prev:
            need[s] = max(need.get(s, 0), prev)
        self._wait(q, need, ev)
        ins = self.eng[q].dma_start(out=out, in_=in_)
        self.dcnt[s] = prev + 16
        ins.then_inc(self.dsem[s], 16)
        event = (s, self.dcnt[s], None)
        if q == "pool":
            self.sw_out.append((event, nd))
        self._record(event, reads, writes)
        if is_output:
            self.out_events.append(event)
        self.nops += 1
        return event

    def finish(self):
        e = "sp"
        for (sk, v, _) in self.out_events:
            if self.know[e].get(sk, 0) < v:
                self.eng[e].wait_ge(self._semh(sk), v)
                self.know[e][sk] = v
        self.stack.close()
        return self.nc

NORM_EPS = 1e-6
GN_EPS = 1e-5
SCALE = 128 ** -0.5
NEG = -1.0e9


def ph_gemm(p, w, src, out, K, N, T, TT, pro, epi, g=None, gT=None, rT=None, src_dt=F32, WSPLIT=4, out_is_output=False):
    p.begin_phase()
    KC = K // 128
    NCH = (N + 127) // 128
    if pro in ("norm", "gate"):
        gs = p.sb("gs", [128, KC], F32)
        p.dma("sp", gs[:], g, writes=["gs"])
    hT = p.sb("hT", [128, KC, TT], BF16)
    NW = 2
    wb = [p.sb("wb%d" % i, [128, KC, 128], BF16) for i in range(NW)]
    NO = 3
    ob = [p.sb("ob%d" % i, [128, 512], F32) for i in range(NO)]
    NP = 3
    pt = [p.ps("pt%d" % i, [128, 512]) for i in range(NP)]
    NTS = TT // 512
    if pro == "norm":
        ones = p.sb("ones", [128, 128], BF16)
        p.op("dve", lambda e: e.memset(ones[:], 1.0), writes=["ones"])
        epsb = p.sb("epsb", [128, 1], F32)
        p.op("dve", lambda e: e.memset(epsb[:], NORM_EPS), writes=["epsb"])
        rstd = p.sb("rstd", [128, TT], F32)
        ss = [p.ps("ss%d" % i, [128, 512]) for i in range(NTS)]
        xs = [p.sb("xs%d" % i, [128, 512], F32) for i in range(3)]
        sq = [p.sb("sq%d" % i, [128, 512], BF16) for i in range(2)]
    if pro == "gate":
        xs = [p.sb("xs%d" % i, [128, 512], src_dt) for i in range(2)]
        gx = [p.sb("gx%d" % i, [128, 512], F32) for i in range(2)]
    if epi == "resid":
        rb = [p.sb("rb%d" % i, [128, 512], F32) for i in range(NO)]
    wv = w.rearrange("(c p) n -> p c n", p=128)
    xv = src.rearrange("(c p) t -> p c t", p=128)
    ksplit = [(i * KC // WSPLIT, (i + 1) * KC // WSPLIT) for i in range(WSPLIT)]
    ctr = {"x": 0, "o": 0, "p": 0, "w": 0}
    for tp in range(T // TT):
        t0 = tp * TT
        if pro == "plain":
            for (k0, k1) in ksplit:
                p.dma("pool", hT[:, k0:k1, :], xv[:, k0:k1, t0:t0 + TT], writes=[("hT", k) for k in range(k0, k1)])
        elif pro == "norm":
            for kc in range(KC):
                for ts in range(NTS):
                    i = ctr["x"]; ctr["x"] += 1
                    xb = xs[i % 3]; sb_ = sq[i % 2]
                    c0 = ts * 512
                    p.dma("sp", xb[:], xv[:, kc, t0 + c0:t0 + c0 + 512], writes=[("xs", i % 3)])
                    p.op("act", lambda e: e.activation(out=sb_[:], in_=xb[:], func=AF.Square), reads=[("xs", i % 3)], writes=[("sq", i % 2)])
                    p.op("pe", lambda e: e.matmul(ss[ts][:], lhsT=ones[:], rhs=sb_[:], start=(kc == 0), stop=(kc == KC - 1)),
                         reads=[("sq", i % 2), "ones"], writes=[("ss", ts)])
                    p.op("dve", lambda e: e.tensor_scalar(out=hT[:, kc, c0:c0 + 512], in0=xb[:], scalar1=gs[:, kc:kc + 1], scalar2=None, op0=ALU.mult),
                         reads=[("xs", i % 3), "gs"], writes=[("hT", kc)])
            for ts in range(NTS):
                c0 = ts * 512
                p.op("act", lambda e: e.activation(out=rstd[:, c0:c0 + 512], in_=ss[ts][:], func=AF.Sqrt, scale=1.0 / K, bias=epsb[:, 0:1]),
                     reads=[("ss", ts), "epsb"], writes=[("rstd", ts)])
                p.op("dve", lambda e: e.reciprocal(out=rstd[:, c0:c0 + 512], in_=rstd[:, c0:c0 + 512]), reads=[("rstd", ts)], writes=[("rstd", ts)])
        elif pro == "gate":
            gv = gT.rearrange("(c p) t -> p c t", p=128)
            for kc in range(KC):
                for ts in range(NTS):
                    i = ctr["x"]; ctr["x"] += 1
                    xb = xs[i % 2]; gb = gx[i % 2]
                    c0 = ts * 512
                    p.dma("sp", xb[:], xv[:, kc, t0 + c0:t0 + c0 + 512], writes=[("xs", i % 2)])
                    p.dma("sp", gb[:], gv[:, kc, t0 + c0:t0 + c0 + 512], writes=[("gx", i % 2)])
                    p.op("act", lambda e: e.activation(out=gb[:], in_=gb[:], func=AF.Silu), reads=[("gx", i % 2)], writes=[("gx", i % 2)])
                    p.op("dve", lambda e: e.scalar_tensor_tensor(out=hT[:, kc, c0:c0 + 512], in0=xb[:], scalar=gs[:, kc:kc + 1], in1=gb[:], op0=ALU.mult, op1=ALU.mult),
                         reads=[("xs", i % 2), ("gx", i % 2), "gs"], writes=[("hT", kc)])
        for n in range(NCH):
            nw = min(128, N - n * 128)
            wi = ctr["w"]; ctr["w"] += 1
            ws = wi % NW
            for si, (k0, k1) in enumerate(ksplit):
                p.dma("pool", wb[ws][:, k0:k1, :nw], wv[:, k0:k1, n * 128:n * 128 + nw], writes=[("wb", ws, si)])
            for ts in range(NTS):
                c0 = ts * 512
                pi = ctr["p"]; ctr["p"] += 1
                pb = pt[pi % NP]
                for si, (k0, k1) in enumerate(ksplit):
                    for kc in range(k0, k1):
                        p.op("pe", lambda e: e.matmul(pb[:nw, :], lhsT=wb[ws][:, kc, :nw], rhs=hT[:, kc, c0:c0 + 512], start=(kc == 0), stop=(kc == KC - 1)),
                             reads=[("wb", ws, si), ("hT", kc)], writes=[("pt", pi % NP)])
                oi = ctr["o"]; ctr["o"] += 1
                o_ = ob[oi % NO]
                if epi == "rstd":
                    p.op("dve", lambda e: e.tensor_tensor(out=o_[:nw, :], in0=pb[:nw, :], in1=rstd[:nw, c0:c0 + 512], op=ALU.mult),
                         reads=[("pt", pi % NP), ("rstd", ts)], writes=[("ob", oi % NO)])
                elif epi == "resid":
                    r_ = rb[oi % NO]
                    p.dma("sp", r_[:nw, :], rT[n * 128:n * 128 + nw, t0 + c0:t0 + c0 + 512], writes=[("rb", oi % NO)])
                    p.op("dve", lambda e: e.tensor_tensor(out=o_[:nw, :], in0=pb[:nw, :], in1=r_[:nw, :], op=ALU.add),
                         reads=[("pt", pi % NP), ("rb", oi % NO)], writes=[("ob", oi % NO)])
                p.dma("sp", out[n * 128:n * 128 + nw, t0 + c0:t0 + c0 + 512], o_[:nw, :], reads=[("ob", oi % NO)], is_output=out_is_output)
    p.end_phase()


def ph_ffn_in(p, w, src, out, g, cw, K, F, T, TT, WSPLIT=4):
    p.begin_phase()
    KC = K // 128
    FC = F // 128
    NPASS = T // TT
    NTS = TT // 512
    TW = TT + 2
    gs = p.sb("gs", [128, KC], F32)
    p.dma("sp", gs[:], g, writes=["gs"])
    cws = p.sb("cws", [128, FC, 4], F32)
    p.dma("sp", cws[:], cw, writes=["cws"])
    ones = p.sb("ones", [128, 128], BF16)
    p.op("dve", lambda e: e.memset(ones[:], 1.0), writes=["ones"])
    epsb = p.sb("epsb", [128, 1], F32)
    p.op("dve", lambda e: e.memset(epsb[:], NORM_EPS), writes=["epsb"])
    hT = p.sb("hT", [128, KC, TW], BF16)
    rstd = p.sb("rstd", [128, TW], F32)
    xs = [p.sb("xs%d" % i, [128, 512], F32) for i in range(3)]
    sq = [p.sb("sq%d" % i, [128, 512], BF16) for i in range(2)]
    NW = 2
    wb = [p.sb("wb%d" % i, [128, KC, 256], BF16) for i in range(NW)]
    NB = 2
    Ab = [p.sb("A%d" % i, [128, TW], F32) for i in range(NB)]
    Ub = [p.sb("U%d" % i, [128, TT], F32) for i in range(NB)]
    Cb = [p.sb("C%d" % i, [128, TT], F32) for i in range(NB)]
    Ob = [p.sb("O%d" % i, [128, TT], F32) for i in range(NB)]
    NP = 4
    pt = [p.ps("pt%d" % i, [128, 512]) for i in range(NP)]
    ss = [p.ps("ss%d" % i, [128, 512]) for i in range(NTS + 1)]
    xv = src.rearrange("(c p) t -> p c t", p=128)
    wv = w.rearrange("(c p) n -> p c n", p=128)
    ksplit = [(i * KC // WSPLIT, (i + 1) * KC // WSPLIT) for i in range(WSPLIT)]
    ctr = {"x": 0, "p": 0, "w": 0}
    for tp in range(NPASS):
        pieces = [(2 + 512 * ts, 512, tp * TT + 512 * ts) for ts in range(NTS)]
        if tp > 0:
            pieces = [(0, 2, tp * TT - 2)] + pieces
        for kc in range(KC):
            for (c0, cn, d0) in pieces:
                pi_ = 0 if cn == 2 else 1 + (c0 - 2) // 512
                i = ctr["x"]; ctr["x"] += 1
                xb = xs[i % 3]; sb_ = sq[i % 2]
                p.dma("sp", xb[:, :cn], xv[:, kc, d0:d0 + cn], writes=[("xs", i % 3)])
                p.op("act", lambda e: e.activation(out=sb_[:, :cn], in_=xb[:, :cn], func=AF.Square), reads=[("xs", i % 3)], writes=[("sq", i % 2)])
                p.op("pe", lambda e: e.matmul(ss[pi_][:, :cn], lhsT=ones[:], rhs=sb_[:, :cn], start=(kc == 0), stop=(kc == KC - 1)),
                     reads=[("sq", i % 2), "ones"], writes=[("ss", pi_)])
                p.op("dve", lambda e: e.tensor_scalar(out=hT[:, kc, c0:c0 + cn], in0=xb[:, :cn], scalar1=gs[:, kc:kc + 1], scalar2=None, op0=ALU.mult),
                     reads=[("xs", i % 3), "gs"], writes=[("hT", kc)])
        for (c0, cn, d0) in pieces:
            pi_ = 0 if cn == 2 else 1 + (c0 - 2) // 512
            p.op("act", lambda e: e.activation(out=rstd[:, c0:c0 + cn], in_=ss[pi_][:, :cn], func=AF.Sqrt, scale=1.0 / K, bias=epsb[:, 0:1]),
                 reads=[("ss", pi_), "epsb"], writes=[("rstd", pi_)])
            p.op("dve", lambda e: e.reciprocal(out=rstd[:, c0:c0 + cn], in_=rstd[:, c0:c0 + cn]), reads=[("rstd", pi_)], writes=[("rstd", pi_)])
        for fc in range(FC):
            wi = ctr["w"]; ctr["w"] += 1
            ws = wi % NW
            bi = wi % NB
            A, U, C, O = Ab[bi], Ub[bi], Cb[bi], Ob[bi]
            for half, col0 in ((0, fc * 128), (1, F + fc * 128)):
                for si, (k0, k1) in enumerate(ksplit):
                    p.dma("pool", wb[ws][:, k0:k1, half * 128:(half + 1) * 128], wv[:, k0:k1, col0:col0 + 128], writes=[("wb", ws, half, si)])
            if tp == 0:
                p.op("dve", lambda e: e.memset(A[:, 0:2], 0.0), writes=[("A", bi)])
            for (c0, cn, d0) in pieces:
                pi_ = 0 if cn == 2 else 1 + (c0 - 2) // 512
                halves = (0,) if cn == 2 else (0, 1)
                for half in halves:
                    qi = ctr["p"]; ctr["p"] += 1
                    pb = pt[qi % NP]
                    for si, (k0, k1) in enumerate(ksplit):
                        for kc in range(k0, k1):
                            p.op("pe", lambda e: e.matmul(pb[:, :cn], lhsT=wb[ws][:, kc, half * 128:(half + 1) * 128], rhs=hT[:, kc, c0:c0 + cn], start=(kc == 0), stop=(kc == KC - 1)),
                                 reads=[("wb", ws, half, si), ("hT", kc)], writes=[("pt", qi % NP)])
                    if half == 0:
                        p.op("dve", lambda e: e.tensor_tensor(out=A[:, c0:c0 + cn], in0=pb[:, :cn], in1=rstd[:, c0:c0 + cn], op=ALU.mult),
                             reads=[("pt", qi % NP), ("rstd", pi_), ("A", bi)], writes=[("A", bi)])
                    else:
                        p.op("dve", lambda e: e.tensor_tensor(out=U[:, c0 - 2:c0 - 2 + cn], in0=pb[:, :cn], in1=rstd[:, c0:c0 + cn], op=ALU.mult),
                             reads=[("pt", qi % NP), ("rstd", pi_)], writes=[("U", bi)])
            p.op("dve", lambda e: e.tensor_scalar(out=C[:], in0=A[:, 0:TT], scalar1=cws[:, fc, 0:1], scalar2=cws[:, fc, 3:4], op0=ALU.mult, op1=ALU.add),
                 reads=[("A", bi), "cws"], writes=[("C", bi)])
            p.op("dve", lambda e: e.scalar_tensor_tensor(out=C[:], in0=A[:, 1:TT + 1], scalar=cws[:, fc, 1:2], in1=C[:], op0=ALU.mult, op1=ALU.add),
                 reads=[("A", bi), "cws", ("C", bi)], writes=[("C", bi)])
            p.op("dve", lambda e: e.scalar_tensor_tensor(out=C[:], in0=A[:, 2:TT + 2], scalar=cws[:, fc, 2:3], in1=C[:], op0=ALU.mult, op1=ALU.add),
                 reads=[("A", bi), "cws", ("C", bi)], writes=[("C", bi)])
            p.op("act", lambda e: e.activation(out=C[:], in_=C[:], func=AF.Silu), reads=[("C", bi)], writes=[("C", bi)])
            p.op("pool", lambda e: e.tensor_tensor(out=O[:], in0=C[:], in1=U[:], op=ALU.mult), reads=[("C", bi), ("U", bi)], writes=[("O", bi)])
            p.dma("sp", out[fc * 128:(fc + 1) * 128, tp * TT:(tp + 1) * TT], O[:], reads=[("O", bi)])
    p.end_phase()


def ph_final_norm(p, src, out, g, K, T):
    p.begin_phase()
    KC = K // 128
    gs = p.sb("gs", [128, KC], F32)
    p.dma("sp", gs[:], g, writes=["gs"])
    ones = p.sb("ones", [128, 128], BF16)
    p.op("dve", lambda e: e.memset(ones[:], 1.0), writes=["ones"])
    epsb = p.sb("epsb", [128, 1], F32)
    p.op("dve", lambda e: e.memset(epsb[:], NORM_EPS), writes=["epsb"])
    xa = [p.sb("xa%d" % i, [128, KC, 512], F32) for i in range(2)]
    sq = [p.sb("sq%d" % i, [128, 512], BF16) for i in range(2)]
    ob = [p.sb("ob%d" % i, [128, 512], F32) for i in range(3)]
    rstd = [p.sb("rstd%d" % i, [128, 512], F32) for i in range(2)]
    ss = [p.ps("ss%d" % i, [128, 512]) for i in range(2)]
    xv = src.rearrange("(c p) t -> p c t", p=128)
    i = 0
    oi = 0
    for ts in range(T // 512):
        b = ts % 2
        c0 = ts * 512
        for kq in range(4):
            k0, k1 = kq * KC // 4, (kq + 1) * KC // 4
            p.dma("sp", xa[b][:, k0:k1, :], xv[:, k0:k1, c0:c0 + 512], writes=[("xa", b, kq)])
        for kc in range(KC):
            kq = kc * 4 // KC
            sb_ = sq[i % 2]
            p.op("act", lambda e: e.activation(out=sb_[:], in_=xa[b][:, kc, :], func=AF.Square), reads=[("xa", b, kq)], writes=[("sq", i % 2)])
            p.op("pe", lambda e: e.matmul(ss[b][:], lhsT=ones[:], rhs=sb_[:], start=(kc == 0), stop=(kc == KC - 1)),
                 reads=[("sq", i % 2), "ones"], writes=[("ss", b)])
            i += 1
        p.op("act", lambda e: e.activation(out=rstd[b][:], in_=ss[b][:], func=AF.Sqrt, scale=1.0 / K, bias=epsb[:, 0:1]),
             reads=[("ss", b), "epsb"], writes=[("rstd", b)])
        p.op("dve", lambda e: e.reciprocal(out=rstd[b][:], in_=rstd[b][:]), reads=[("rstd", b)], writes=[("rstd", b)])
        for kc in range(KC):
            kq = kc * 4 // KC
            o_ = ob[oi % 3]
            p.op("dve", lambda e: e.scalar_tensor_tensor(out=o_[:], in0=xa[b][:, kc, :], scalar=gs[:, kc:kc + 1], in1=rstd[b][:], op0=ALU.mult, op1=ALU.mult),
                 reads=[("xa", b, kq), "gs", ("rstd", b)], writes=[("ob", oi % 3)])
            p.dma("sp", out[kc * 128:(kc + 1) * 128, c0:c0 + 512], o_[:], reads=[("ob", oi % 3)], is_output=True)
            oi += 1
    p.end_phase()


def ph_retention(p, qT, kT, vT, ynT, dmt, qd, kd, cd, ident_d, H, S, CG=4):
    p.begin_phase()
    NCK = S // 128
    D = 256
    dmts = p.sb("dmts", [128, H, 128], F32)
    qds = p.sb("qds", [128, H, 128], F32)
    kds = p.sb("kds", [128, H], F32)
    cds = p.sb("cds", [128, H], F32)
    ident = p.sb("ident", [128, 128], BF16)
    p.dma("sp", dmts[:], dmt, writes=["dmts"])
    p.dma("sp", qds[:], qd, writes=["qds"])
    p.dma("sp", kds[:], kd, writes=["kds"])
    p.dma("sp", cds[:], cd, writes=["cds"])
    p.dma("pool", ident[:], ident_d, writes=["ident"])
    epsb = p.sb("epsb", [128, 1], F32)
    p.op("dve", lambda e: e.memset(epsb[:], GN_EPS), writes=["epsb"])
    W = CG * 128
    qg = [p.sb("qg%d" % i, [128, 2 * H, W], BF16) for i in range(2)]
    kg = [p.sb("kg%d" % i, [128, 2 * H, W], BF16) for i in range(2)]
    vg = [p.sb("vg%d" % i, [128, 2 * H, W], BF16) for i in range(2)]
    YT = [p.sb("YT%d" % i, [128, 2 * H, W], F32) for i in range(2)]
    St = [p.sb("S%d" % h, [128, 2, D], F32) for h in range(H)]
    Sb = [p.sb("Sb%d" % h, [128, 2, D], BF16) for h in range(H)]
    PT = [p.sb("PT%d" % i, [128, 128], BF16) for i in range(2)]
    QD = [p.sb("QD%d" % i, [128, 2, 128], BF16) for i in range(2)]
    KDb = [p.sb("KD%d" % i, [128, D], BF16) for i in range(2)]
    VTk = [p.sb("VT%d" % i, [128, 2, 128], BF16) for i in range(2)]
    KTk = [p.sb("KT%d" % i, [128, 2, 128], BF16) for i in range(2)]
    Yb = [p.sb("Yb%d" % i, [128, D], BF16) for i in range(2)]
    st6 = [p.sb("st6_%d" % i, [128, 6], F32) for i in range(2)]
    mv = [p.sb("mv%d" % i, [128, 2], F32) for i in range(2)]
    rs = [p.sb("rs%d" % i, [128, 1], F32) for i in range(2)]
    pA = [p.ps("pA%d" % i, [128, 128]) for i in range(2)]
    pY = [p.ps("pY%d" % i, [128, D]) for i in range(2)]
    pS = [p.ps("pS%d" % i, [128, 2, D]) for i in range(2)]
    pT = [p.ps("pT%d" % i, [128, 4, 128], BF16) for i in range(2)]
    qv = qT.rearrange("(x p) s -> p x s", p=128)
    kv = kT.rearrange("(x p) s -> p x s", p=128)
    vv = vT.rearrange("(x p) s -> p x s", p=128)
    yv = ynT.rearrange("(x p) s -> p x s", p=128)
    u = 0
    for c in range(NCK):
        cgi = c // CG
        gb = cgi % 2
        cc = c % CG
        if cc == 0:
            for hh in range(0, 2 * H, 4):
                p.dma("pool", qg[gb][:, hh:hh + 4, :], qv[:, hh:hh + 4, cgi * W:(cgi + 1) * W], writes=[("qg", gb, hh)])
                p.dma("pool", kg[gb][:, hh:hh + 4, :], kv[:, hh:hh + 4, cgi * W:(cgi + 1) * W], writes=[("kg", gb, hh)])
                p.dma("pool", vg[gb][:, hh:hh + 4, :], vv[:, hh:hh + 4, cgi * W:(cgi + 1) * W], writes=[("vg", gb, hh)])
        cs = slice(cc * 128, (cc + 1) * 128)
        for h in range(H):
            b = u % 2
            u += 1
            hk = (2 * h) // 4 * 4
            for dc in range(2):
                p.op("pe", lambda e: e.transpose(pT[b][:, dc, :], kg[gb][:, 2 * h + dc, cs], ident[:]), reads=[("kg", gb, hk), "ident"], writes=[("pT", b)])
            for dc in range(2):
                p.op("pe", lambda e: e.transpose(pT[b][:, 2 + dc, :], vg[gb][:, 2 * h + dc, cs], ident[:]), reads=[("vg", gb, hk), "ident"], writes=[("pT", b)])
            p.op("act", lambda e: e.copy(out=KTk[b][:, :, :], in_=pT[b][:, 0:2, :]), reads=[("pT", b)], writes=[("KT", b)])
            p.op("pool", lambda e: e.tensor_scalar(out=KDb[b][:], in0=KTk[b][:, :, :].rearrange("p a b -> p (a b)"), scalar1=kds[:, h:h + 1], scalar2=None, op0=ALU.mult),
                 reads=[("KT", b), "kds"], writes=[("KD", b)])
            p.op("act", lambda e: e.copy(out=VTk[b][:, :, :], in_=pT[b][:, 2:4, :]), reads=[("pT", b)], writes=[("VT", b)])
            for dc in range(2):
                p.op("pe", lambda e: e.matmul(pA[b][:], lhsT=kg[gb][:, 2 * h + dc, cs], rhs=qg[gb][:, 2 * h + dc, cs], start=(dc == 0), stop=(dc == 1)),
                     reads=[("kg", gb, hk), ("qg", gb, hk)], writes=[("pA", b)])
            p.op("dve", lambda e: e.tensor_tensor(out=PT[b][:], in0=pA[b][:], in1=dmts[:, h, :], op=ALU.mult), reads=[("pA", b), "dmts"], writes=[("PT", b)])
            if c > 0:
                for dc in range(2):
                    p.op("dve", lambda e: e.tensor_tensor(out=QD[b][:, dc, :], in0=qg[gb][:, 2 * h + dc, cs], in1=qds[:, h, :], op=ALU.mult),
                         reads=[("qg", gb, hk), "qds"], writes=[("QD", b, dc)])
            p.op("pe", lambda e: e.matmul(pY[b][:], lhsT=PT[b][:], rhs=VTk[b][:, :, :].rearrange("p a b -> p (a b)"), start=True, stop=(c == 0)), reads=[("PT", b), ("VT", b)], writes=[("pY", b)])
            if c > 0:
                for dc in range(2):
                    p.op("pe", lambda e: e.matmul(pY[b][:], lhsT=QD[b][:, dc, :], rhs=Sb[h][:, dc, :], start=False, stop=(dc == 1)),
                         reads=[("QD", b, dc), ("Sb", h)], writes=[("pY", b)])
            p.op("dve", lambda e: e.bn_stats(out=st6[b][:], in_=pY[b][:]), reads=[("pY", b)], writes=[("st6", b)])
            p.op("dve", lambda e: e.bn_aggr(out=mv[b][:], in_=st6[b][:]), reads=[("st6", b)], writes=[("mv", b)])
            p.op("act", lambda e: e.activation(out=rs[b][:], in_=mv[b][:, 1:2], func=AF.Sqrt, bias=epsb[:, 0:1]), reads=[("mv", b), "epsb"], writes=[("rs", b)])
            p.op("dve", lambda e: e.reciprocal(out=rs[b][:], in_=rs[b][:]), reads=[("rs", b)], writes=[("rs", b)])
            p.op("dve", lambda e: e.tensor_scalar(out=Yb[b][:], in0=pY[b][:], scalar1=mv[b][:, 0:1], scalar2=rs[b][:, 0:1], op0=ALU.subtract, op1=ALU.mult),
                 reads=[("pY", b), ("mv", b), ("rs", b)], writes=[("Yb", b)])
            if c < NCK - 1:
                for dc in range(2):
                    p.op("pe", lambda e: e.matmul(pS[b][:, dc, :], lhsT=KDb[b][:, dc * 128:(dc + 1) * 128], rhs=VTk[b][:, :, :].rearrange("p a b -> p (a b)"), start=True, stop=True),
                         reads=[("KD", b), ("VT", b)], writes=[("pS", b)])
                if c == 0:
                    p.op("dve", lambda e: e.tensor_copy(out=St[h][:], in_=pS[b][:]), reads=[("pS", b)], writes=[("S", h)])
                else:
                    p.op("dve", lambda e: e.scalar_tensor_tensor(out=St[h][:], in0=St[h][:], scalar=cds[:, h:h + 1], in1=pS[b][:], op0=ALU.mult, op1=ALU.add),
                         reads=[("pS", b), ("S", h), "cds"], writes=[("S", h)])
                p.op("act", lambda e: e.copy(out=Sb[h][:], in_=St[h][:]), reads=[("S", h)], writes=[("Sb", h)])
            for ec in range(2):
                p.op("pe", lambda e: e.transpose(pT[b][:, ec, :], Yb[b][:, ec * 128:(ec + 1) * 128], ident[:]), reads=[("Yb", b), "ident"], writes=[("pT", b)])
            p.op("act", lambda e: e.copy(out=YT[gb][:, 2 * h:2 * h + 2, cs], in_=pT[b][:, 0:2, :]), reads=[("pT", b)], writes=[("YT", gb, h)])
        if cc == CG - 1:
            p.dma("sp", yv[:, :, cgi * W:(cgi + 1) * W], YT[gb][:], reads=[("YT", gb, h) for h in range(H)])
    p.end_phase()


def nsa_consts(slopes):
    pp = np.arange(128, dtype=np.float64)[:, None]
    c = np.arange(512, dtype=np.float64)[None, :]
    dt = np.zeros((128, 6, 512), np.float32)
    dt[:, 0, :] = -(pp - c)
    for k in range(4):
        dist = pp - c + 128 * k
        dt[:, 1 + k, :] = np.where(dist >= 0, -dist, NEG)
    dist = pp - c + 512
    dt[:, 5, :] = np.where(dist < 512, -dist, NEG)
    u = np.arange(255, dtype=np.float64)[None, :]
    dist = pp - 16 * (u - 248) - 31
    dc = np.where(dist >= 0, -dist, NEG).astype(np.float32)
    jp = np.arange(126)[None, :] - 62
    hi = (np.arange(128)[:, None] >= 64).astype(np.int64)
    valid = jp <= hi
    forced = (jp == hi) | (jp == hi - 1)
    mm = (valid & ~forced).astype(np.float32)
    ma = np.where(forced, 1e9, np.where(valid, 0.0, -1e9)).astype(np.float32)
    slsc = np.tile((slopes / SCALE)[None, :], (128, 1)).astype(np.float32)
    deltas = 512 + 128 * np.arange(28)
    tab = np.tile((-slopes[:, None] * deltas[None, :])[None], (128, 1, 1)).astype(np.float32)
    return dict(dt=dt, dc=dc, mm=mm, ma=ma, slsc=slsc, tab=tab)


def ph_nsa(p, qpT, kvpT, attnT, w1, w2, posT, cst, ident_d, G, HG, S):
    p.begin_phase()
    NH = G * HG
    NT = S // 128
    NCMP = (S - 32) // 16 + 1
    NG3 = NH * 3

    def const(name, shape, src, dtype=F32, q="sp"):
        t = p.sb(name, shape, dtype)
        p.dma(q, t[:], src, writes=[name])
        return t
    Dt = const("Dt", [128, 6, 512], cst["dt"])
    Dc = const("Dc", [128, 255], cst["dc"])
    MM = const("MM", [128, 126], cst["mm"])
    MA = const("MA", [128, 126], cst["ma"])
    slsc = const("slsc", [128, NH], cst["slsc"])
    tab = const("tab", [128, NH, 28], cst["tab"])
    ident = const("ident", [128, 128], ident_d, BF16, "pool")
    w1s = const("w1s", [128, 2, 32, 128], w1.rearrange("y (l d) m -> d y l m", d=128), BF16, "pool")
    w2s = const("w2s", [128, 2, 128], w2.rearrange("y m n -> m y n"), BF16, "pool")
    poss = const("poss", [128, 2, 32], posT.rearrange("y d l -> d y l"), BF16, "pool")
    qg = p.sb("qg", [128, HG, S], BF16)
    ksT = p.sb("ksT", [128, S], BF16)
    kwT = p.sb("kwT", [128, S], BF16)
    vs = p.sb("vs", [128, NT, 132], BF16)
    vw = p.sb("vw", [128, NT, 132], BF16)
    tokT = p.sb("tokT", [128, S], BF16)
    kcT = p.sb("kcT", [128, 256], BF16)
    vc = p.sb("vc", [128, 2, 128], BF16)
    s1T = p.sb("s1T", [128, 256], BF16)
    bc = p.sb("bc", [128, 1], F32)
    tmp = [p.sb("tmp%d" % i, [128, 512], F32) for i in range(2)]
    eb = [p.sb("eb%d" % i, [128, 512], BF16) for i in range(2)]
    eT = [p.sb("eT%d" % i, [128, 4, 128], BF16) for i in range(2)]
    ef = p.sb("ef", [128, 256], F32)
    pnb = p.sb("pnb", [128, 256], BF16)
    Pacc = p.sb("Pacc", [128, 264], F32)
    imp = p.sb("imp", [128, 64], F32)
    sc2 = p.sb("sc2", [128, 64], F32)
    t8 = p.sb("t8", [128, 8], F32)
    t8b = p.sb("t8b", [128, 8], F32)
    selm = p.sb("selm", [128, 64], BF16)
    sm = [p.sb("sm%d" % i, [128, 4], F32) for i in range(2)]
    acc = [p.sb("acc%d" % i, [128, HG, 128], F32) for i in range(2)]
    accb = [p.sb("accb%d" % i, [128, HG, 128], BF16) for i in range(2)]
    AT = [p.sb("AT%d" % i, [128, HG, 128], F32) for i in range(2)]
    gt = p.sb("gt", [128, NT, NG3], F32)
    pS = [p.ps("pS%d" % i, [128, 512]) for i in range(2)]
    pT = [p.ps("pT%d" % i, [128, 4, 128], BF16) for i in range(2)]
    pO = [p.ps("pO%d" % i, [128, 132]) for i in range(2)]
    ctr = {"u": 0, "o": 0, "s": 0}
    p.op("dve", lambda e: e.memset(vs[:], 1.0), writes=["ksv"])
    p.op("dve", lambda e: e.memset(vw[:], 1.0), writes=["kwv"])
    p.dma("pool", tokT[:NG3, :], qpT[NH * 128:NH * 128 + NG3, :], writes=["tokT"])
    for ti in range(NT):
        b = ti % 2
        p.op("pe", lambda e: e.transpose(pT[b][:, 0, 0:NG3], tokT[:NG3, ti * 128:(ti + 1) * 128], ident[:NG3, :NG3]), reads=["tokT", "ident"], writes=[("pT", b)])
        p.op("act", lambda e: e.activation(out=gt[:, ti, :], in_=pT[b][:, 0, 0:NG3], func=AF.Sigmoid), reads=[("pT", b)], writes=["gt"])

    def unit(h, hh, ti, kT, k0, c_lo, ncol, Dap, bias, mask, Vt, vch0, po, first, last, rk):
        u = ctr["u"]; ctr["u"] += 1
        b = u % 2
        ts = slice(ti * 128, (ti + 1) * 128)
        p.op("pe", lambda e: e.matmul(pS[b][:, c_lo:ncol], lhsT=qg[:, h, ts], rhs=kT[:, k0 + c_lo:k0 + ncol], start=True, stop=True),
             reads=["qg", rk], writes=[("pS", b)])
        p.op("dve", lambda e: e.scalar_tensor_tensor(out=tmp[b][:, c_lo:ncol], in0=Dap[:, c_lo:ncol], scalar=slsc[:, hh:hh + 1], in1=pS[b][:, c_lo:ncol], op0=ALU.mult, op1=ALU.add),
             reads=[("pS", b), "Dt", "slsc"], writes=[("tmp", b)])
        if bias is None:
            p.op("act", lambda e: e.activation(out=eb[b][:, c_lo:ncol], in_=tmp[b][:, c_lo:ncol], func=AF.Exp, scale=SCALE), reads=[("tmp", b)], writes=[("eb", b)])
        else:
            p.op("act", lambda e: e.activation(out=eb[b][:, c_lo:ncol], in_=tmp[b][:, c_lo:ncol], func=AF.Exp, scale=SCALE, bias=bias),
                 reads=[("tmp", b), "tab"], writes=[("eb", b)])
        if mask is not None:
            p.op("pool", lambda e: e.tensor_tensor(out=eb[b][:].rearrange("p (j l) -> p j l", l=64), in0=eb[b][:].rearrange("p (j l) -> p j l", l=64),
                                                   in1=mask.unsqueeze(2).to_broadcast([128, 8, 64]), op=ALU.mult),
                 reads=[("eb", b), "selm"], writes=[("eb", b)])
        kc0 = c_lo // 128
        nkc = ncol // 128
        for kc in range(kc0, nkc):
            p.op("pe", lambda e: e.transpose(pT[b][:, kc, :], eb[b][:, kc * 128:(kc + 1) * 128], ident[:]), reads=[("eb", b), "ident"], writes=[("pT", b)])
        p.op("act", lambda e: e.copy(out=eT[b][:, kc0:nkc, :], in_=pT[b][:, kc0:nkc, :]), reads=[("pT", b)], writes=[("eT", b)])
        for kc in range(kc0, nkc):
            p.op("pe", lambda e: e.matmul(po[0][:, 0:130], lhsT=eT[b][:, kc, :], rhs=Vt[:, vch0 + kc, 0:130], start=(first and kc == kc0), stop=(last and kc == nkc - 1)),
                 reads=[("eT", b), rk + "v"], writes=[po[1]])

    qv = qpT[0:NH * 128, :].rearrange("(x p) s -> p x s", p=128)
    for g in range(G):
        for hq in range(0, HG, 2):
            p.dma("pool", qg[:, hq:hq + 2, :], qv[:, g * HG + hq:g * HG + hq + 2, :], writes=["qg"])
        kvrow = lambda j: kvpT[(j * G + g) * 128:(j * G + g + 1) * 128, :]
        p.dma("pool", ksT[:], kvrow(2), writes=["ks"])
        p.dma("pool", kwT[:], kvrow(4), writes=["kw"])
        for (j, Vt, key) in ((3, vs, "ksv"), (5, vw, "kwv")):
            p.dma("pool", tokT[:], kvrow(j), writes=["tokT"])
            for ti in range(NT):
                b = ti % 2
                p.op("pe", lambda e: e.transpose(pT[b][:, 0, :], tokT[:, ti * 128:(ti + 1) * 128], ident[:]), reads=["tokT", "ident"], writes=[("pT", b)])
                p.op("act", lambda e: e.copy(out=Vt[:, ti, 0:128], in_=pT[b][:, 0, :]), reads=[("pT", b)], writes=[key])
        for ty in range(2):
            p.dma("pool", tokT[:], kvrow(ty), writes=["tokT"])
            for l in range(32):
                p.op("pe", lambda e: e.matmul(pS[0][:, 0:1], lhsT=w1s[:, ty, l, :], rhs=poss[:, ty, l:l + 1], start=(l == 0), stop=(l == 31)),
                     reads=["w1s", "poss"], writes=[("pS", 0)])
            p.op("dve", lambda e: e.tensor_copy(out=bc[:], in_=pS[0][:, 0:1]), reads=[("pS", 0)], writes=["bc"])
            for l in range(32):
                p.op("pe", lambda e: e.matmul(pS[1][:, 0:NCMP], lhsT=w1s[:, ty, l, :], rhs=tokT[:, l:l + 16 * (NCMP - 1) + 1:16], start=(l == 0), stop=(l == 31)),
                     reads=["w1s", "tokT"], writes=[("pS", 1)])
            p.op("act", lambda e: e.activation(out=s1T[:, 0:NCMP], in_=pS[1][:, 0:NCMP], func=AF.Silu, bias=bc[:, 0:1]), reads=[("pS", 1), "bc"], writes=["s1T"])
            if ty == 0:
                p.op("pe", lambda e: e.matmul(pS[0][:, 0:NCMP], lhsT=w2s[:, 0, :], rhs=s1T[:, 0:NCMP], start=True, stop=True), reads=["w2s", "s1T"], writes=[("pS", 0)])
                p.op("dve", lambda e: e.tensor_copy(out=kcT[:, 0:NCMP], in_=pS[0][:, 0:NCMP]), reads=[("pS", 0)], writes=["kcT"])
            else:
                p.op("dve", lambda e: e.memset(vc[:], 0.0), writes=["vc"])
                for ch in range((NCMP + 127) // 128):
                    n0 = ch * 128
                    nn = min(128, NCMP - n0)
                    p.op("pe", lambda e: e.matmul(pS[0][:nn, 0:128], lhsT=s1T[:, n0:n0 + nn], rhs=w2s[:, 1, :], start=True, stop=True), reads=["w2s", "s1T"], writes=[("pS", 0)])
                    p.op("dve", lambda e: e.tensor_copy(out=vc[:nn, ch, :], in_=pS[0][:nn, 0:128]), reads=[("pS", 0)], writes=["vc"])
        for ti in range(NT):
            ab = ctr["o"] % 2; ctr["o"] += 1
            A = acc[ab]
            ts = slice(ti * 128, (ti + 1) * 128)
            n_hi = min(NCMP, 8 * ti + 7)
            u0 = 248 - 8 * ti
            p.op("dve", lambda e: e.memset(Pacc[:], 0.0), writes=["Pacc"])
            for h in range(HG):
                hh = g * HG + h
                si = ctr["s"] % 2; ctr["s"] += 1
                s_ = sm[si]
                p.op("pe", lambda e: e.matmul(pS[0][:, 0:n_hi], lhsT=qg[:, h, ts], rhs=kcT[:, 0:n_hi], start=True, stop=True), reads=["qg", "kcT"], writes=[("pS", 0)])
                p.op("dve", lambda e: e.scalar_tensor_tensor(out=tmp[0][:, 0:n_hi], in0=Dc[:, u0:u0 + n_hi], scalar=slsc[:, hh:hh + 1], in1=pS[0][:, 0:n_hi], op0=ALU.mult, op1=ALU.add),
                     reads=[("pS", 0), "Dc", "slsc"], writes=[("tmp", 0)])
                p.op("dve", lambda e: e.reduce_max(out=s_[:, 0:1], in_=tmp[0][:, 0:n_hi], axis=AX.X), reads=[("tmp", 0)], writes=[("sm", si)])
                p.op("dve", lambda e: e.tensor_scalar(out=s_[:, 0:1], in0=s_[:, 0:1], scalar1=-1.0e6, scalar2=-SCALE, op0=ALU.max, op1=ALU.mult), reads=[("sm", si)], writes=[("sm", si)])
                p.op("act", lambda e: e.activation(out=ef[:, 0:n_hi], in_=tmp[0][:, 0:n_hi], func=AF.Exp, scale=SCALE, bias=s_[:, 0:1], accum_out=s_[:, 1:2]),
                     reads=[("tmp", 0), ("sm", si)], writes=["ef", ("sm", si)])
                p.op("dve", lambda e: e.tensor_scalar(out=s_[:, 1:2], in0=s_[:, 1:2], scalar1=1.0e-30, scalar2=None, op0=ALU.max), reads=[("sm", si)], writes=[("sm", si)])
                p.op("dve", lambda e: e.reciprocal(out=s_[:, 1:2], in_=s_[:, 1:2]), reads=[("sm", si)], writes=[("sm", si)])
                p.op("dve", lambda e: e.scalar_tensor_tensor(out=Pacc[:, 1:1 + n_hi], in0=ef[:, 0:n_hi], scalar=s_[:, 1:2], in1=Pacc[:, 1:1 + n_hi], op0=ALU.mult, op1=ALU.add),
                     reads=["ef", ("sm", si), "Pacc"], writes=["Pacc"])
                p.op("dve", lambda e: e.memset(pnb[:], 0.0), writes=["pnb"])
                p.op("dve", lambda e: e.tensor_scalar(out=pnb[:, 0:n_hi], in0=ef[:, 0:n_hi], scalar1=s_[:, 1:2], scalar2=None, op0=ALU.mult), reads=["ef", ("sm", si)], writes=["pnb"])
                nch = (n_hi + 127) // 128
                for ch in range(nch):
                    p.op("pe", lambda e: e.transpose(pT[0][:, ch, :], pnb[:, ch * 128:(ch + 1) * 128], ident[:]), reads=["pnb", "ident"], writes=[("pT", 0)])
                p.op("act", lambda e: e.copy(out=eT[0][:, 0:nch, :], in_=pT[0][:, 0:nch, :]), reads=[("pT", 0)], writes=[("eT", 0)])
                for ch in range(nch):
                    p.op("pe", lambda e: e.matmul(pO[0][:, 0:128], lhsT=eT[0][:, ch, :], rhs=vc[:, ch, :], start=(ch == 0), stop=(ch == nch - 1)), reads=[("eT", 0), "vc"], writes=[("pO", 0)])
                p.op("dve", lambda e: e.tensor_scalar(out=A[:, h, :], in0=pO[0][:, 0:128], scalar1=gt[:, ti, hh * 3:hh * 3 + 1], scalar2=None, op0=ALU.mult),
                     reads=[("pO", 0), "gt"], writes=[("acc", ab, h)])
            Pv = Pacc[:, 0:256].rearrange("p (j k) -> p j k", k=4)
            Pv4 = Pacc[:, 4:260].rearrange("p (j k) -> p j k", k=4)
            p.op("dve", lambda e: e.scalar_tensor_tensor(out=imp[:], in0=Pv[:, :, 1], scalar=2.0, in1=Pv[:, :, 0], op0=ALU.mult, op1=ALU.add), reads=["Pacc"], writes=["imp"])
            p.op("dve", lambda e: e.scalar_tensor_tensor(out=imp[:], in0=Pv[:, :, 2], scalar=2.0, in1=imp[:], op0=ALU.mult, op1=ALU.add), reads=["Pacc", "imp"], writes=["imp"])
            p.op("dve", lambda e: e.scalar_tensor_tensor(out=imp[:], in0=Pv[:, :, 3], scalar=2.0, in1=imp[:], op0=ALU.mult, op1=ALU.add), reads=["Pacc", "imp"], writes=["imp"])
            p.op("dve", lambda e: e.tensor_tensor(out=imp[:], in0=imp[:], in1=Pv4[:, :, 0], op=ALU.add), reads=["Pacc", "imp"], writes=["imp"])
            m0 = 62 - 2 * ti
            p.op("dve", lambda e: e.tensor_tensor(out=imp[:], in0=imp[:], in1=MM[:, m0:m0 + 64], op=ALU.mult), reads=["imp", "MM"], writes=["imp"])
            p.op("dve", lambda e: e.tensor_tensor(out=imp[:], in0=imp[:], in1=MA[:, m0:m0 + 64], op=ALU.add), reads=["imp", "MA"], writes=["imp"])
            p.op("dve", lambda e: e.memset(imp[:, 0:1], 1.0e9), reads=["imp"], writes=["imp"])
            p.op("dve", lambda e: e.max(out=t8[:], in_=imp[:]), reads=["imp"], writes=["t8"])
            p.op("dve", lambda e: e.match_replace(out=sc2[:], in_to_replace=t8[:], in_values=imp[:], imm_value=-3.0e9), reads=["imp", "t8"], writes=["sc2"])
            p.op("dve", lambda e: e.max(out=t8b[:], in_=sc2[:]), reads=["sc2"], writes=["t8b"])
            p.op("dve", lambda e: e.tensor_scalar(out=selm[:], in0=imp[:], scalar1=t8b[:, 7:8], scalar2=None, op0=ALU.is_ge), reads=["imp", "t8b"], writes=["selm"])
            for h in range(HG):
                hh = g * HG + h
                for br in (1, 2):
                    ob = ctr["s"] % 2; ctr["s"] += 1
                    po = (pO[ob], ("pO", ob))
                    units = []
                    if br == 1:
                        nk = ti // 4
                        for kt in range(nk + 1):
                            if kt == nk:
                                units.append(dict(kT=ksT, k0=512 * kt, c_lo=0, ncol=128 * (ti % 4) + 128, Dap=Dt[:, 1 + ti % 4, :], bias=None,
                                                  mask=selm[:, 8 * kt:8 * kt + 8], Vt=vs, vch0=4 * kt, rk="ks"))
                            else:
                                di = (128 * ti - 512 * kt - 512) // 128
                                units.append(dict(kT=ksT, k0=512 * kt, c_lo=0, ncol=512, Dap=Dt[:, 0, :], bias=tab[:, hh, di:di + 1],
                                                  mask=selm[:, 8 * kt:8 * kt + 8], Vt=vs, vch0=4 * kt, rk="ks"))
                    else:
                        if ti >= 1:
                            c_lo = max(0, 512 - 128 * ti)
                            units.append(dict(kT=kwT, k0=128 * ti - 512, c_lo=c_lo, ncol=512, Dap=Dt[:, 5, :], bias=None, mask=None, Vt=vw, vch0=ti - 4, rk="kw"))
                        units.append(dict(kT=kwT, k0=128 * ti, c_lo=0, ncol=128, Dap=Dt[:, 1, :], bias=None, mask=None, Vt=vw, vch0=ti, rk="kw"))
                    for ui, ud in enumerate(units):
                        unit(h, hh, ti, po=po, first=(ui == 0), last=(ui == len(units) - 1), **ud)
                    s_ = sm[ob]
                    p.op("dve", lambda e: e.reciprocal(out=s_[:, 2:3], in_=pO[ob][:, 128:129]), reads=[("pO", ob)], writes=[("sm2", ob)])
                    p.op("dve", lambda e: e.tensor_tensor(out=s_[:, 3:4], in0=s_[:, 2:3], in1=gt[:, ti, hh * 3 + br:hh * 3 + br + 1], op=ALU.mult), reads=[("sm2", ob), "gt"], writes=[("sm3", ob)])
                    p.op("dve", lambda e: e.scalar_tensor_tensor(out=A[:, h, :], in0=pO[ob][:, 0:128], scalar=s_[:, 3:4], in1=A[:, h, :], op0=ALU.mult, op1=ALU.add),
                         reads=[("pO", ob), ("sm3", ob), ("acc", ab, h)], writes=[("acc", ab, h)])
            Ab_ = accb[ab]
            p.op("pool", lambda e: e.tensor_copy(out=Ab_[:], in_=A[:]), reads=[("acc", ab, h) for h in range(HG)], writes=[("accb", ab)])
            for h4 in range(0, HG, 4):
                b = (h4 // 4) % 2
                nk = min(4, HG - h4)
                for k in range(nk):
                    p.op("pe", lambda e: e.transpose(pT[b][:, k, :], Ab_[:, h4 + k, :], ident[:]), reads=[("accb", ab), "ident"], writes=[("pT", b)])
                p.op("act", lambda e: e.copy(out=AT[ab][:, h4:h4 + nk, :], in_=pT[b][:, 0:nk, :]), reads=[("pT", b)], writes=[("AT", ab, h4)])
            p.dma("sp", attnT.rearrange("(x p) s -> p x s", p=128)[:, g * HG:(g + 1) * HG, ts], AT[ab][:], reads=[("AT", ab, h4) for h4 in range(0, HG, 4)])
    p.end_phase()


from concourse.bass_utils import run_bass_kernel_spmd

D_MODEL = 4096
SEQ = 4096
BATCH = 4
D_FF = 11008
NCORES = 4


def build_fused():
    p = Prog()
    D, S, F = D_MODEL, SEQ, D_FF
    I = lambda name, shape, dt=F32: p.dram(name, shape, dt, "ExternalInput")
    T_ = lambda name, shape, dt=F32: p.dram(name, shape, dt, "Internal")
    xT = I("xT", [D, S])
    w_ret_in = I("w_ret_in", [D, 4 * D]); w_ret_out = I("w_ret_out", [D, D])
    w_kv = I("w_kv", [D, 3072]); w_q = I("w_q", [D, D + 96]); w_nsa_out = I("w_nsa_out", [D, D])
    w_fi = [I("w_fi%d" % l, [D, 2 * F]) for l in range(2)]
    w_fo = [I("w_fo%d" % l, [F, D]) for l in range(2)]
    cwl = [I("cw%d" % l, [128, F // 128, 4]) for l in range(2)]
    vec = {k: I(k, [128, D // 128]) for k in ("an0", "an1", "fn0", "fn1", "kvn", "finn", "gng")}
    w1 = I("w1", [2, 4096, 128]); w2 = I("w2", [2, 128, 128]); posT = I("posT", [2, 128, 32])
    ident = I("ident", [128, 128])
    rc = [dict(dmt=I("dmt%d" % i, [128, 8, 128]), qd=I("qd%d" % i, [128, 8, 128]), kd=I("kd%d" % i, [128, 8]), cd=I("cd%d" % i, [128, 8])) for i in range(2)]
    cst = dict(dt=I("dt", [128, 6, 512]), dc=I("dc", [128, 255]), mm=I("mm", [128, 126]), ma=I("ma", [128, 126]),
               slsc=I("slsc", [128, 32]), tab=I("tab", [128, 32, 28]))
    oT = p.dram("oT", [D, S], F32, "ExternalOutput")
    projT = T_("projT", [4 * D, S]); ynT = T_("ynT", [D, S])
    x1T = T_("x1T", [D, S]); x2T = T_("x2T", [D, S]); x3T = T_("x3T", [D, S]); x4T = T_("x4T", [D, S])
    gatedT = T_("gatedT", [F, S]); kvpT = T_("kvpT", [3072, S]); qpT = T_("qpT", [D + 96, S]); attnT = T_("attnT", [D, S])

    ph_gemm(p, w_ret_in, xT, projT, D, 4 * D, S, 2048, "norm", "rstd", g=vec["an0"])
    for hg in range(2):
        r = hg * 2048
        ph_retention(p, projT[r:r + 2048, :], projT[D + r:D + r + 2048, :], projT[2 * D + r:2 * D + r + 2048, :], ynT[r:r + 2048, :],
                     rc[hg]["dmt"], rc[hg]["qd"], rc[hg]["kd"], rc[hg]["cd"], ident, 8, S, CG=2)
    ph_gemm(p, w_ret_out, ynT, x1T, D, D, S, 2048, "gate", "resid", g=vec["gng"], gT=projT[3 * D:4 * D, :], rT=xT)
    ph_ffn_in(p, w_fi[0], x1T, gatedT, vec["fn0"], cwl[0], D, F, S, 1024)
    ph_gemm(p, w_fo[0], gatedT, x2T, F, D, S, 512, "plain", "resid", rT=x1T, WSPLIT=8)
    ph_gemm(p, w_kv, x2T, kvpT, D, 3072, S, 2048, "norm", "rstd", g=vec["kvn"])
    ph_gemm(p, w_q, x2T, qpT, D, D + 96, S, 2048, "norm", "rstd", g=vec["an1"])
    ph_nsa(p, qpT, kvpT, attnT, w1, w2, posT, cst, ident, 4, 8, S)
    ph_gemm(p, w_nsa_out, attnT, x3T, D, D, S, 2048, "plain", "resid", rT=x2T)
    ph_ffn_in(p, w_fi[1], x3T, gatedT, vec["fn1"], cwl[1], D, F, S, 1024)
    ph_gemm(p, w_fo[1], gatedT, x4T, F, D, S, 512, "plain", "resid", rT=x3T, WSPLIT=8)
    ph_final_norm(p, x4T, oT, vec["finn"], D, S)
    return p.finish()


def _gl(v):
    v = np.asarray(v, np.float32)
    return np.ascontiguousarray(v.reshape(-1, 128).T)


def _ret_consts(heads):
    Dh = 256
    lg = np.log1p(-np.exp2(-5.0 - np.asarray(heads, dtype=np.float64)))
    i = np.arange(128, dtype=np.float64)
    scale = Dh ** -0.5
    Hn = len(heads)
    dmt = np.zeros((128, Hn, 128), np.float32)
    qd = np.zeros((128, Hn, 128), np.float32)
    kd = np.zeros((128, Hn), np.float32)
    cd = np.zeros((128, Hn), np.float32)
    for a, l in enumerate(lg):
        diff = i[None, :] - i[:, None]
        dmt[:, a, :] = np.where(diff >= 0, np.exp(np.maximum(diff, 0) * l), 0.0) * scale
        qd[:, a, :] = np.exp((i + 1.0) * l)[None, :]
        kd[:, a] = np.exp((127.0 - i) * l) * scale
        cd[:, a] = np.exp(128 * l)
    return dmt, qd, kd, cd


def kernel(x, attn_norm, ffn_norm, w_ret_in, ret_gn_gain, w_ret_out, kv_norm, w_kv,
           cmp_pos, cmp_w1, cmp_w2, w_nsa_q, w_nsa_out, w_ffn_in, conv_w, conv_b,
           w_ffn_out, final_norm):
    f32 = lambda a: np.ascontiguousarray(np.asarray(a, np.float32))
    x = f32(x)
    common = dict(w_ret_in=f32(w_ret_in[0]), w_ret_out=f32(w_ret_out[0]), w_kv=f32(w_kv), w_q=f32(w_nsa_q[0]), w_nsa_out=f32(w_nsa_out[0]),
                  w1=f32(cmp_w1), w2=f32(cmp_w2), posT=f32(np.asarray(cmp_pos).transpose(0, 2, 1)), ident=np.eye(128, dtype=np.float32),
                  an0=_gl(attn_norm[0]), an1=_gl(attn_norm[1]), fn0=_gl(ffn_norm[0]), fn1=_gl(ffn_norm[1]), kvn=_gl(kv_norm),
                  finn=_gl(final_norm), gng=_gl(ret_gn_gain[0]))
    for l in range(2):
        common["w_fi%d" % l] = f32(w_ffn_in[l])
        common["w_fo%d" % l] = f32(w_ffn_out[l])
        cw = np.concatenate([np.asarray(conv_w[l], np.float32), np.asarray(conv_b[l], np.float32)[None]], 0)
        common["cw%d" % l] = np.ascontiguousarray(cw.reshape(4, D_FF // 128, 128).transpose(2, 1, 0))
    for i in range(2):
        dmt, qd, kd, cd = _ret_consts(list(range(i * 8, i * 8 + 8)))
        common.update({"dmt%d" % i: dmt, "qd%d" % i: qd, "kd%d" % i: kd, "cd%d" % i: cd})
    slopes = np.exp2(-8.0 * (np.arange(32, dtype=np.float64) + 1.0) / 32.0)
    common.update(nsa_consts(slopes))
    ins = []
    for b in range(NCORES):
        d = dict(common)
        d["xT"] = np.ascontiguousarray(x[b].T)
        ins.append(d)
    nc = build_fused()
    res = run_bass_kernel_spmd(nc, ins, core_ids=list(range(NCORES)))
    out = np.empty((BATCH, SEQ, D_MODEL), np.float32)
    for b in range(NCORES):
        out[b] = np.asarray(res.results[b]["oT"]).T
    return out
```

```python
import numpy as np
import concourse.bass as bass
import concourse.mybir as mybir
from contextlib import ExitStack

F32 = mybir.dt.float32
F32R = mybir.dt.float32r
BF16 = mybir.dt.bfloat16
AF = mybir.ActivationFunctionType
ALU = mybir.AluOpType
AX = mybir.AxisListType

CE = ("pe", "act", "dve", "pool")
NDMA = 48


class Prog:
    def __init__(self, name="k"):
        self.nc = bass.Bass("TRN2", target_bir_lowering=False)
        nc = self.nc
        self.stack = ExitStack()
        self.eng = {"pe": nc.tensor, "act": nc.scalar, "dve": nc.vector,
                    "pool": nc.gpsimd, "sp": nc.sync}
        self.sem = {e: self.stack.enter_context(nc.semaphore("s_" + e)) for e in CE}
        self.cnt = {e: 0 for e in CE}
        self.dsem = [self.stack.enter_context(nc.semaphore("d%d" % i)) for i in range(NDMA)]
        self.dcnt = [0] * NDMA
        self.dnext = 0
        self.know = {e: {} for e in self.eng}
        self.lastw = {}
        self.rd = {}
        self.nops = 0
        self.out_events = []
        self.pstack = None
        self.pid = 0
        self.sw_out = []
        self.sw_budget = 10000

    def dram(self, name, shape, dtype, kind, addr_space=None):
        if addr_space is not None:
            return self.nc.dram_tensor(name, list(shape), dtype, kind=kind, addr_space=addr_space).ap()
        return self.nc.dram_tensor(name, list(shape), dtype, kind=kind).ap()

    def sb(self, name, shape, dtype):
        st = self.pstack if self.pstack is not None else self.stack
        return st.enter_context(self.nc.sbuf_tensor("p%d_%s" % (self.pid, name), list(shape), dtype))

    def ps(self, name, shape, dtype=F32):
        st = self.pstack if self.pstack is not None else self.stack
        return st.enter_context(self.nc.psum_tensor("p%d_%s" % (self.pid, name), list(shape), dtype))

    def begin_phase(self):
        self.pid += 1
        self.pstack = ExitStack()

    def end_phase(self):
        self.barrier()
        self.pstack.close()
        self.pstack = None

    def barrier(self):
        targets = [(e, self.cnt[e]) for e in CE if self.cnt[e] > 0]
        targets += [(i, self.dcnt[i]) for i in range(NDMA) if self.dcnt[i] > 0]
        for e in self.eng:
            kn = self.know[e]
            for sk, v in targets:
                if kn.get(sk, 0) < v:
                    self.eng[e].wait_ge(self._semh(sk), v)
                    kn[sk] = v
        self.lastw = {}
        self.rd = {}
        self.sw_out = []

    def _semh(self, k):
        return self.sem[k] if isinstance(k, str) else self.dsem[k]

    def _need(self, e, reads, writes):
        ev = []
        for k in reads:
            w = self.lastw.get(k)
            if w is not None:
                ev.append(w)
        for k in writes:
            w = self.lastw.get(k)
            if w is not None:
                ev.append(w)
            ev.extend(self.rd.get(k, ()))
        need = {}
        kn = self.know[e]
        for (sk, v, clk) in ev:
            if e == "pe" and sk == "pe":
                continue
            if kn.get(sk, 0) >= v:
                continue
            if need.get(sk, 0) < v:
                need[sk] = v
        return need, ev

    def _wait(self, e, need, ev):
        eng = self.eng[e]
        kn = self.know[e]
        for sk, v in need.items():
            eng.wait_ge(self._semh(sk), v)
            kn[sk] = v
        for (sk, v, clk) in ev:
            if clk:
                for ck, cv in clk.items():
                    if kn.get(ck, 0) < cv:
                        kn[ck] = cv

    def _record(self, event, reads, writes):
        for k in writes:
            self.lastw[k] = event
            self.rd[k] = []
        for k in reads:
            if k in writes:
                continue
            self.rd.setdefault(k, []).append(event)

    def op(self, e, fn, reads=(), writes=()):
        need, ev = self._need(e, reads, writes)
        self._wait(e, need, ev)
        ins = fn(self.eng[e])
        self.cnt[e] += 1
        ins.then_inc(self.sem[e], 1)
        kn = self.know[e]
        clk = {k: v for k, v in kn.items() if isinstance(k, str)}
        clk[e] = self.cnt[e]
        if e != "pe":
            pass
        event = (e, self.cnt[e], clk)
        self._record(event, reads, writes)
        self.nops += 1
        return event

    def dma(self, q, out, in_, reads=(), writes=(), is_output=False):
        need, ev = self._need(q, reads, writes)
        nd = 0
        if q == "pool":
            nd = 1
            for d_ in list(out.shape)[:-1]:
                nd *= int(d_)
            tot = sum(n for (_, n) in self.sw_out) + nd
            while self.sw_out and tot > self.sw_budget:
                (osk, ov, _), on = self.sw_out.pop(0)
                tot -= on
                if self.know[q].get(osk, 0) < ov:
                    need[osk] = max(need.get(osk, 0), ov)
        s = self.dnext
        self.dnext = (self.dnext + 1) % NDMA
        prev = self.dcnt[s]
        if prev > 0 and self.know[q].get(s, 0) < prev:
            need[s] = max(need.get(s, 0), prev)
        self._wait(q, need, ev)
        ins = self.eng[q].dma_start(out=out, in_=in_)
        self.dcnt[s] = prev + 16
        ins.then_inc(self.dsem[s], 16)
        event = (s, self.dcnt[s], None)
        if q == "pool":
            self.sw_out.append((event, nd))
        self._record(event, reads, writes)
        if is_output:
            self.out_events.append(event)
        self.nops += 1
        return event

    def coll(self, kind, in_ap, out_ap, groups, reads=(), writes=()):
        q = "pool"
        need, ev = self._need(q, reads, writes)
        s = self.dnext
        self.dnext = (self.dnext + 1) % NDMA
        prev = self.dcnt[s]
        if prev > 0 and self.know[q].get(s, 0) < prev:
            need[s] = max(need.get(s, 0), prev)
        self._wait(q, need, ev)
        ins = self.nc.gpsimd.collective_compute(kind, mybir.AluOpType.bypass, replica_groups=groups, ins=[in_ap], outs=[out_ap])
        self.dcnt[s] = prev + 16
        ins.then_inc(self.dsem[s], 16)
        event = (s, self.dcnt[s], None)
        self._record(event, reads, writes)
        return event

    def finish(self):
        e = "sp"
        for (sk, v, _) in self.out_events:
            if self.know[e].get(sk, 0) < v:
                self.eng[e].wait_ge(self._semh(sk), v)
                self.know[e][sk] = v
        self.stack.close()
        return self.nc

NORM_EPS = 1e-6
GN_EPS = 1e-5
SCALE = 128 ** -0.5
NEG = -1.0e9


def ph_gemm(p, w, src, out, K, N, T, TT, pro, epi, g=None, gT=None, rT=None, src_dt=F32, WSPLIT=4, out_is_output=False):
    p.begin_phase()
    KC = K // 128
    NCH = (N + 127) // 128
    if pro in ("norm", "gate"):
        gs = p.sb("gs", [128, KC], F32)
        p.dma("sp", gs[:], g, writes=["gs"])
    hT = p.sb("hT", [128, KC, TT], BF16)
    NW = 2
    wb = [p.sb("wb%d" % i, [128, KC, 128], BF16) for i in range(NW)]
    NO = 3
    ob = [p.sb("ob%d" % i, [128, 512], F32) for i in range(NO)]
    NP = 3
    pt = [p.ps("pt%d" % i, [128, 512]) for i in range(NP)]
    NTS = TT // 512
    if pro == "norm":
        ones = p.sb("ones", [128, 128], BF16)
        p.op("dve", lambda e: e.memset(ones[:], 1.0), writes=["ones"])
        epsb = p.sb("epsb", [128, 1], F32)
        p.op("dve", lambda e: e.memset(epsb[:], NORM_EPS), writes=["epsb"])
        rstd = p.sb("rstd", [128, TT], F32)
        ss = [p.ps("ss%d" % i, [128, 512]) for i in range(NTS)]
        xs = [p.sb("xs%d" % i, [128, 512], F32) for i in range(3)]
        sq = [p.sb("sq%d" % i, [128, 512], BF16) for i in range(2)]
    if pro == "gate":
        xs = [p.sb("xs%d" % i, [128, 512], src_dt) for i in range(2)]
        gx = [p.sb("gx%d" % i, [128, 512], F32) for i in range(2)]
    if epi == "resid":
        rb = [p.sb("rb%d" % i, [128, 512], F32) for i in range(NO)]
    wv = w.rearrange("(c p) n -> p c n", p=128)
    xv = src.rearrange("(c p) t -> p c t", p=128)
    ksplit = [(i * KC // WSPLIT, (i + 1) * KC // WSPLIT) for i in range(WSPLIT)]
    ctr = {"x": 0, "o": 0, "p": 0, "w": 0}
    for tp in range(T // TT):
        t0 = tp * TT
        if pro == "plain":
            for (k0, k1) in ksplit:
                p.dma("pool", hT[:, k0:k1, :], xv[:, k0:k1, t0:t0 + TT], writes=[("hT", k) for k in range(k0, k1)])
        elif pro == "norm":
            for kc in range(KC):
                for ts in range(NTS):
                    i = ctr["x"]; ctr["x"] += 1
                    xb = xs[i % 3]; sb_ = sq[i % 2]
                    c0 = ts * 512
                    p.dma("sp", xb[:], xv[:, kc, t0 + c0:t0 + c0 + 512], writes=[("xs", i % 3)])
                    p.op("act", lambda e: e.activation(out=sb_[:], in_=xb[:], func=AF.Square), reads=[("xs", i % 3)], writes=[("sq", i % 2)])
                    p.op("pe", lambda e: e.matmul(ss[ts][:], lhsT=ones[:], rhs=sb_[:], start=(kc == 0), stop=(kc == KC - 1)),
                         reads=[("sq", i % 2), "ones"], writes=[("ss", ts)])
                    p.op("dve", lambda e: e.tensor_scalar(out=hT[:, kc, c0:c0 + 512], in0=xb[:], scalar1=gs[:, kc:kc + 1], scalar2=None, op0=ALU.mult),
                         reads=[("xs", i % 3), "gs"], writes=[("hT", kc)])
            for ts in range(NTS):
                c0 = ts * 512
                p.op("act", lambda e: e.activation(out=rstd[:, c0:c0 + 512], in_=ss[ts][:], func=AF.Sqrt, scale=1.0 / K, bias=epsb[:, 0:1]),
                     reads=[("ss", ts), "epsb"], writes=[("rstd", ts)])
                p.op("dve", lambda e: e.reciprocal(out=rstd[:, c0:c0 + 512], in_=rstd[:, c0:c0 + 512]), reads=[("rstd", ts)], writes=[("rstd", ts)])
        elif pro == "gate":
            gv = gT.rearrange("(c p) t -> p c t", p=128)
            for kc in range(KC):
                for ts in range(NTS):
                    i = ctr["x"]; ctr["x"] += 1
                    xb = xs[i % 2]; gb = gx[i % 2]
                    c0 = ts * 512
                    p.dma("sp", xb[:], xv[:, kc, t0 + c0:t0 + c0 + 512], writes=[("xs", i % 2)])
                    p.dma("sp", gb[:], gv[:, kc, t0 + c0:t0 + c0 + 512], writes=[("gx", i % 2)])
                    p.op("act", lambda e: e.activation(out=gb[:], in_=gb[:], func=AF.Silu), reads=[("gx", i % 2)], writes=[("gx", i % 2)])
                    p.op("dve", lambda e: e.scalar_tensor_tensor(out=hT[:, kc, c0:c0 + 512], in0=xb[:], scalar=gs[:, kc:kc + 1], in1=gb[:], op0=ALU.mult, op1=ALU.mult),
                         reads=[("xs", i % 2), ("gx", i % 2), "gs"], writes=[("hT", kc)])
        for n in range(NCH):
            nw = min(128, N - n * 128)
            wi = ctr["w"]; ctr["w"] += 1
            ws = wi % NW
            for si, (k0, k1) in enumerate(ksplit):
                p.dma("pool", wb[ws][:, k0:k1, :nw], wv[:, k0:k1, n * 128:n * 128 + nw], writes=[("wb", ws, si)])
            for ts in range(NTS):
                c0 = ts * 512
                pi = ctr["p"]; ctr["p"] += 1
                pb = pt[pi % NP]
                for si, (k0, k1) in enumerate(ksplit):
                    for kc in range(k0, k1):
                        p.op("pe", lambda e: e.matmul(pb[:nw, :], lhsT=wb[ws][:, kc, :nw], rhs=hT[:, kc, c0:c0 + 512], start=(kc == 0), stop=(kc == KC - 1)),
                             reads=[("wb", ws, si), ("hT", kc)], writes=[("pt", pi % NP)])
                oi = ctr["o"]; ctr["o"] += 1
                o_ = ob[oi % NO]
                if epi == "rstd":
                    p.op("dve", lambda e: e.tensor_tensor(out=o_[:nw, :], in0=pb[:nw, :], in1=rstd[:nw, c0:c0 + 512], op=ALU.mult),
                         reads=[("pt", pi % NP), ("rstd", ts)], writes=[("ob", oi % NO)])
                elif epi == "resid":
                    r_ = rb[oi % NO]
                    p.dma("sp", r_[:nw, :], rT[n * 128:n * 128 + nw, t0 + c0:t0 + c0 + 512], writes=[("rb", oi % NO)])
                    p.op("dve", lambda e: e.tensor_tensor(out=o_[:nw, :], in0=pb[:nw, :], in1=r_[:nw, :], op=ALU.add),
                         reads=[("pt", pi % NP), ("rb", oi % NO)], writes=[("ob", oi % NO)])
                p.dma("sp", out[n * 128:n * 128 + nw, t0 + c0:t0 + c0 + 512], o_[:nw, :], reads=[("ob", oi % NO)], is_output=out_is_output)
    p.end_phase()


def ph_ffn_in(p, w, src, out, g, cw, K, F, T, TT, WSPLIT=4):
    p.begin_phase()
    KC = K // 128
    FC = F // 128
    NPASS = T // TT
    NTS = TT // 512
    TW = TT + 2
    gs = p.sb("gs", [128, KC], F32)
    p.dma("sp", gs[:], g, writes=["gs"])
    cws = p.sb("cws", [128, FC, 4], F32)
    p.dma("sp", cws[:], cw, writes=["cws"])
    ones = p.sb("ones", [128, 128], BF16)
    p.op("dve", lambda e: e.memset(ones[:], 1.0), writes=["ones"])
    epsb = p.sb("epsb", [128, 1], F32)
    p.op("dve", lambda e: e.memset(epsb[:], NORM_EPS), writes=["epsb"])
    hT = p.sb("hT", [128, KC, TW], BF16)
    rstd = p.sb("rstd", [128, TW], F32)
    xs = [p.sb("xs%d" % i, [128, 512], F32) for i in range(2)]
    sq = [p.sb("sq%d" % i, [128, 512], BF16) for i in range(2)]
    NWH = 3
    wb = [p.sb("wb%d" % i, [128, KC, 128], BF16) for i in range(NWH)]
    NA = 3
    Ab = [p.sb("A%d" % i, [128, 514], F32) for i in range(NA)]
    Ub = [p.sb("U%d" % i, [128, 512], F32) for i in range(2)]
    Cb = [p.sb("C%d" % i, [128, 512], F32) for i in range(2)]
    Ob = [p.sb("O%d" % i, [128, 512], F32) for i in range(2)]
    NP = 4
    pt = [p.ps("pt%d" % i, [128, 512]) for i in range(NP)]
    ss = [p.ps("ss%d" % i, [128, 512]) for i in range(min(NTS, 3) + 1)]
    xv = src.rearrange("(c p) t -> p c t", p=128)
    wv = w.rearrange("(c p) n -> p c n", p=128)
    ksplit = [(i * KC // WSPLIT, (i + 1) * KC // WSPLIT) for i in range(WSPLIT)]
    ctr = {"x": 0, "p": 0, "w": 0, "a": 0, "e": 0}
    NSS = len(ss) - 1
    for tp in range(NPASS):
        pieces = [(2 + 512 * ts, 512, tp * TT + 512 * ts) for ts in range(NTS)]
        if tp > 0:
            pieces = [(0, 2, tp * TT - 2)] + pieces
        for (c0, cn, d0) in pieces:
            pi_ = 0 if cn == 2 else 1 + ((c0 - 2) // 512) % NSS
            for kc in range(KC):
                i = ctr["x"]; ctr["x"] += 1
                xb = xs[i % 2]; sb_ = sq[i % 2]
                p.dma("sp", xb[:, :cn], xv[:, kc, d0:d0 + cn], writes=[("xs", i % 2)])
                p.op("act", lambda e: e.activation(out=sb_[:, :cn], in_=xb[:, :cn], func=AF.Square), reads=[("xs", i % 2)], writes=[("sq", i % 2)])
                p.op("pe", lambda e: e.matmul(ss[pi_][:, :cn], lhsT=ones[:], rhs=sb_[:, :cn], start=(kc == 0), stop=(kc == KC - 1)),
                     reads=[("sq", i % 2), "ones"], writes=[("ss", pi_)])
                p.op("dve", lambda e: e.tensor_scalar(out=hT[:, kc, c0:c0 + cn], in0=xb[:, :cn], scalar1=gs[:, kc:kc + 1], scalar2=None, op0=ALU.mult),
                     reads=[("xs", i % 2), "gs"], writes=[("hT", kc)])
            p.op("act", lambda e: e.activation(out=rstd[:, c0:c0 + cn], in_=ss[pi_][:, :cn], func=AF.Sqrt, scale=1.0 / K, bias=epsb[:, 0:1]),
                 reads=[("ss", pi_), "epsb"], writes=[("rstd", c0)])
            p.op("dve", lambda e: e.reciprocal(out=rstd[:, c0:c0 + cn], in_=rstd[:, c0:c0 + cn]), reads=[("rstd", c0)], writes=[("rstd", c0)])
        for fc in range(FC):
            wsl = []
            for half, col0 in ((0, fc * 128), (1, F + fc * 128)):
                wi = ctr["w"]; ctr["w"] += 1
                ws = wi % NWH
                wsl.append(ws)
                for si, (k0, k1) in enumerate(ksplit):
                    p.dma("pool", wb[ws][:, k0:k1, :], wv[:, k0:k1, col0:col0 + 128], writes=[("wb", ws, si)])

            def mm(half, c0, cn, pb, pk):
                ws = wsl[half]
                for si, (k0, k1) in enumerate(ksplit):
                    for kc in range(k0, k1):
                        p.op("pe", lambda e: e.matmul(pb[:, :cn], lhsT=wb[ws][:, kc, :], rhs=hT[:, kc, c0:c0 + cn], start=(kc == 0), stop=(kc == KC - 1)),
                             reads=[("wb", ws, si), ("hT", kc)], writes=[pk])
            ai = ctr["a"]; ctr["a"] += 1
            A = Ab[ai % NA]
            if tp == 0:
                p.op("dve", lambda e: e.memset(A[:, 0:2], 0.0), writes=[("A", ai % NA)])
            else:
                qi = ctr["p"]; ctr["p"] += 1
                pb = pt[qi % NP]
                mm(0, 0, 2, pb, ("pt", qi % NP))
                p.op("dve", lambda e: e.tensor_tensor(out=A[:, 0:2], in0=pb[:, 0:2], in1=rstd[:, 0:2], op=ALU.mult),
                     reads=[("pt", qi % NP), ("rstd", 0)], writes=[("A", ai % NA)])
            for ts in range(NTS):
                c0 = 2 + 512 * ts
                ei = ctr["e"]; ctr["e"] += 1
                U, C, O = Ub[ei % 2], Cb[ei % 2], Ob[ei % 2]
                qa = ctr["p"]; ctr["p"] += 1
                pa = pt[qa % NP]
                mm(0, c0, 512, pa, ("pt", qa % NP))
                p.op("dve", lambda e: e.tensor_tensor(out=A[:, 2:514], in0=pa[:, :], in1=rstd[:, c0:c0 + 512], op=ALU.mult),
                     reads=[("pt", qa % NP), ("rstd", c0), ("A", ai % NA)], writes=[("A", ai % NA)])
                qu = ctr["p"]; ctr["p"] += 1
                pu = pt[qu % NP]
                mm(1, c0, 512, pu, ("pt", qu % NP))
                p.op("dve", lambda e: e.tensor_tensor(out=U[:], in0=pu[:, :], in1=rstd[:, c0:c0 + 512], op=ALU.mult),
                     reads=[("pt", qu % NP), ("rstd", c0)], writes=[("U", ei % 2)])
                if ts < NTS - 1:
                    an = ctr["a"]; ctr["a"] += 1
                    A2 = Ab[an % NA]
                    p.op("pool", lambda e: e.tensor_copy(out=A2[:, 0:2], in_=A[:, 512:514]), reads=[("A", ai % NA)], writes=[("A", an % NA)])
                p.op("dve", lambda e: e.tensor_scalar(out=C[:], in0=A[:, 0:512], scalar1=cws[:, fc, 0:1], scalar2=cws[:, fc, 3:4], op0=ALU.mult, op1=ALU.add),
                     reads=[("A", ai % NA), "cws"], writes=[("C", ei % 2)])
                p.op("dve", lambda e: e.scalar_tensor_tensor(out=C[:], in0=A[:, 1:513], scalar=cws[:, fc, 1:2], in1=C[:], op0=ALU.mult, op1=ALU.add),
                     reads=[("A", ai % NA), "cws", ("C", ei % 2)], writes=[("C", ei % 2)])
                p.op("dve", lambda e: e.scalar_tensor_tensor(out=C[:], in0=A[:, 2:514], scalar=cws[:, fc, 2:3], in1=C[:], op0=ALU.mult, op1=ALU.add),
                     reads=[("A", ai % NA), "cws", ("C", ei % 2)], writes=[("C", ei % 2)])
                p.op("act", lambda e: e.activation(out=C[:], in_=C[:], func=AF.Silu), reads=[("C", ei % 2)], writes=[("C", ei % 2)])
                p.op("pool", lambda e: e.tensor_tensor(out=O[:], in0=C[:], in1=U[:], op=ALU.mult), reads=[("C", ei % 2), ("U", ei % 2)], writes=[("O", ei % 2)])
                p.dma("sp", out[fc * 128:(fc + 1) * 128, tp * TT + 512 * ts:tp * TT + 512 * ts + 512], O[:], reads=[("O", ei % 2)])
                if ts < NTS - 1:
                    A = A2
                    ai = an
    p.end_phase()


def ph_final_norm(p, src, out, g, K, T):
    p.begin_phase()
    KC = K // 128
    gs = p.sb("gs", [128, KC], F32)
    p.dma("sp", gs[:], g, writes=["gs"])
    ones = p.sb("ones", [128, 128], BF16)
    p.op("dve", lambda e: e.memset(ones[:], 1.0), writes=["ones"])
    epsb = p.sb("epsb", [128, 1], F32)
    p.op("dve", lambda e: e.memset(epsb[:], NORM_EPS), writes=["epsb"])
    xa = [p.sb("xa%d" % i, [128, KC, 512], F32) for i in range(2)]
    sq = [p.sb("sq%d" % i, [128, 512], BF16) for i in range(2)]
    ob = [p.sb("ob%d" % i, [128, 512], F32) for i in range(3)]
    rstd = [p.sb("rstd%d" % i, [128, 512], F32) for i in range(2)]
    ss = [p.ps("ss%d" % i, [128, 512]) for i in range(2)]
    xv = src.rearrange("(c p) t -> p c t", p=128)
    i = 0
    oi = 0
    for ts in range(T // 512):
        b = ts % 2
        c0 = ts * 512
        for kq in range(4):
            k0, k1 = kq * KC // 4, (kq + 1) * KC // 4
            p.dma("sp", xa[b][:, k0:k1, :], xv[:, k0:k1, c0:c0 + 512], writes=[("xa", b, kq)])
        for kc in range(KC):
            kq = kc * 4 // KC
            sb_ = sq[i % 2]
            p.op("act", lambda e: e.activation(out=sb_[:], in_=xa[b][:, kc, :], func=AF.Square), reads=[("xa", b, kq)], writes=[("sq", i % 2)])
            p.op("pe", lambda e: e.matmul(ss[b][:], lhsT=ones[:], rhs=sb_[:], start=(kc == 0), stop=(kc == KC - 1)),
                 reads=[("sq", i % 2), "ones"], writes=[("ss", b)])
            i += 1
        p.op("act", lambda e: e.activation(out=rstd[b][:], in_=ss[b][:], func=AF.Sqrt, scale=1.0 / K, bias=epsb[:, 0:1]),
             reads=[("ss", b), "epsb"], writes=[("rstd", b)])
        p.op("dve", lambda e: e.reciprocal(out=rstd[b][:], in_=rstd[b][:]), reads=[("rstd", b)], writes=[("rstd", b)])
        for kc in range(KC):
            kq = kc * 4 // KC
            o_ = ob[oi % 3]
            p.op("dve", lambda e: e.scalar_tensor_tensor(out=o_[:], in0=xa[b][:, kc, :], scalar=gs[:, kc:kc + 1], in1=rstd[b][:], op0=ALU.mult, op1=ALU.mult),
                 reads=[("xa", b, kq), "gs", ("rstd", b)], writes=[("ob", oi % 3)])
            p.dma("sp", out[kc * 128:(kc + 1) * 128, c0:c0 + 512], o_[:], reads=[("ob", oi % 3)], is_output=True)
            oi += 1
    p.end_phase()


def ph_retention(p, qT, kT, vT, ynT, dmt, qd, kd, cd, ident_d, H, S, CG=4):
    p.begin_phase()
    NCK = S // 128
    D = 256
    dmts = p.sb("dmts", [128, H, 128], F32)
    qds = p.sb("qds", [128, H, 128], F32)
    kds = p.sb("kds", [128, H], F32)
    cds = p.sb("cds", [128, H], F32)
    ident = p.sb("ident", [128, 128], BF16)
    p.dma("sp", dmts[:], dmt, writes=["dmts"])
    p.dma("sp", qds[:], qd, writes=["qds"])
    p.dma("sp", kds[:], kd, writes=["kds"])
    p.dma("sp", cds[:], cd, writes=["cds"])
    p.dma("pool", ident[:], ident_d, writes=["ident"])
    epsb = p.sb("epsb", [128, 1], F32)
    p.op("dve", lambda e: e.memset(epsb[:], GN_EPS), writes=["epsb"])
    W = CG * 128
    qg = [p.sb("qg%d" % i, [128, 2 * H, W], BF16) for i in range(2)]
    kg = [p.sb("kg%d" % i, [128, 2 * H, W], BF16) for i in range(2)]
    vg = [p.sb("vg%d" % i, [128, 2 * H, W], BF16) for i in range(2)]
    YT = [p.sb("YT%d" % i, [128, 2 * H, W], F32) for i in range(2)]
    St = [p.sb("S%d" % h, [128, 2, D], F32) for h in range(H)]
    Sb = [p.sb("Sb%d" % h, [128, 2, D], BF16) for h in range(H)]
    PT = [p.sb("PT%d" % i, [128, 128], BF16) for i in range(2)]
    QD = [p.sb("QD%d" % i, [128, 2, 128], BF16) for i in range(2)]
    KDb = [p.sb("KD%d" % i, [128, D], BF16) for i in range(2)]
    VTk = [p.sb("VT%d" % i, [128, 2, 128], BF16) for i in range(2)]
    KTk = [p.sb("KT%d" % i, [128, 2, 128], BF16) for i in range(2)]
    Yb = [p.sb("Yb%d" % i, [128, D], BF16) for i in range(2)]
    st6 = [p.sb("st6_%d" % i, [128, 6], F32) for i in range(2)]
    mv = [p.sb("mv%d" % i, [128, 2], F32) for i in range(2)]
    rs = [p.sb("rs%d" % i, [128, 1], F32) for i in range(2)]
    pA = [p.ps("pA%d" % i, [128, 128]) for i in range(2)]
    pY = [p.ps("pY%d" % i, [128, D]) for i in range(2)]
    pS = [p.ps("pS%d" % i, [128, 2, D]) for i in range(2)]
    pT = [p.ps("pT%d" % i, [128, 4, 128], BF16) for i in range(2)]
    qv = qT.rearrange("(x p) s -> p x s", p=128)
    kv = kT.rearrange("(x p) s -> p x s", p=128)
    vv = vT.rearrange("(x p) s -> p x s", p=128)
    yv = ynT.rearrange("(x p) s -> p x s", p=128)
    u = 0
    for c in range(NCK):
        cgi = c // CG
        gb = cgi % 2
        cc = c % CG
        if cc == 0:
            for hh in range(0, 2 * H, 4):
                p.dma("pool", qg[gb][:, hh:hh + 4, :], qv[:, hh:hh + 4, cgi * W:(cgi + 1) * W], writes=[("qg", gb, hh)])
                p.dma("pool", kg[gb][:, hh:hh + 4, :], kv[:, hh:hh + 4, cgi * W:(cgi + 1) * W], writes=[("kg", gb, hh)])
                p.dma("pool", vg[gb][:, hh:hh + 4, :], vv[:, hh:hh + 4, cgi * W:(cgi + 1) * W], writes=[("vg", gb, hh)])
        cs = slice(cc * 128, (cc + 1) * 128)
        for h in range(H):
            b = u % 2
            u += 1
            hk = (2 * h) // 4 * 4
            for dc in range(2):
                p.op("pe", lambda e: e.transpose(pT[b][:, dc, :], kg[gb][:, 2 * h + dc, cs], ident[:]), reads=[("kg", gb, hk), "ident"], writes=[("pT", b)])
            for dc in range(2):
                p.op("pe", lambda e: e.transpose(pT[b][:, 2 + dc, :], vg[gb][:, 2 * h + dc, cs], ident[:]), reads=[("vg", gb, hk), "ident"], writes=[("pT", b)])
            p.op("act", lambda e: e.copy(out=KTk[b][:, :, :], in_=pT[b][:, 0:2, :]), reads=[("pT", b)], writes=[("KT", b)])
            p.op("pool", lambda e: e.tensor_scalar(out=KDb[b][:], in0=KTk[b][:, :, :].rearrange("p a b -> p (a b)"), scalar1=kds[:, h:h + 1], scalar2=None, op0=ALU.mult),
                 reads=[("KT", b), "kds"], writes=[("KD", b)])
            p.op("act", lambda e: e.copy(out=VTk[b][:, :, :], in_=pT[b][:, 2:4, :]), reads=[("pT", b)], writes=[("VT", b)])
            for dc in range(2):
                p.op("pe", lambda e: e.matmul(pA[b][:], lhsT=kg[gb][:, 2 * h + dc, cs], rhs=qg[gb][:, 2 * h + dc, cs], start=(dc == 0), stop=(dc == 1)),
                     reads=[("kg", gb, hk), ("qg", gb, hk)], writes=[("pA", b)])
            p.op("dve", lambda e: e.tensor_tensor(out=PT[b][:], in0=pA[b][:], in1=dmts[:, h, :], op=ALU.mult), reads=[("pA", b), "dmts"], writes=[("PT", b)])
            if c > 0:
                for dc in range(2):
                    p.op("dve", lambda e: e.tensor_tensor(out=QD[b][:, dc, :], in0=qg[gb][:, 2 * h + dc, cs], in1=qds[:, h, :], op=ALU.mult),
                         reads=[("qg", gb, hk), "qds"], writes=[("QD", b, dc)])
            p.op("pe", lambda e: e.matmul(pY[b][:], lhsT=PT[b][:], rhs=VTk[b][:, :, :].rearrange("p a b -> p (a b)"), start=True, stop=(c == 0)), reads=[("PT", b), ("VT", b)], writes=[("pY", b)])
            if c > 0:
                for dc in range(2):
                    p.op("pe", lambda e: e.matmul(pY[b][:], lhsT=QD[b][:, dc, :], rhs=Sb[h][:, dc, :], start=False, stop=(dc == 1)),
                         reads=[("QD", b, dc), ("Sb", h)], writes=[("pY", b)])
            p.op("dve", lambda e: e.bn_stats(out=st6[b][:], in_=pY[b][:]), reads=[("pY", b)], writes=[("st6", b)])
            p.op("dve", lambda e: e.bn_aggr(out=mv[b][:], in_=st6[b][:]), reads=[("st6", b)], writes=[("mv", b)])
            p.op("act", lambda e: e.activation(out=rs[b][:], in_=mv[b][:, 1:2], func=AF.Sqrt, bias=epsb[:, 0:1]), reads=[("mv", b), "epsb"], writes=[("rs", b)])
            p.op("dve", lambda e: e.reciprocal(out=rs[b][:], in_=rs[b][:]), reads=[("rs", b)], writes=[("rs", b)])
            p.op("dve", lambda e: e.tensor_scalar(out=Yb[b][:], in0=pY[b][:], scalar1=mv[b][:, 0:1], scalar2=rs[b][:, 0:1], op0=ALU.subtract, op1=ALU.mult),
                 reads=[("pY", b), ("mv", b), ("rs", b)], writes=[("Yb", b)])
            if c < NCK - 1:
                for dc in range(2):
                    p.op("pe", lambda e: e.matmul(pS[b][:, dc, :], lhsT=KDb[b][:, dc * 128:(dc + 1) * 128], rhs=VTk[b][:, :, :].rearrange("p a b -> p (a b)"), start=True, stop=True),
                         reads=[("KD", b), ("VT", b)], writes=[("pS", b)])
                if c == 0:
                    p.op("dve", lambda e: e.tensor_copy(out=St[h][:], in_=pS[b][:]), reads=[("pS", b)], writes=[("S", h)])
                else:
                    p.op("dve", lambda e: e.scalar_tensor_tensor(out=St[h][:], in0=St[h][:], scalar=cds[:, h:h + 1], in1=pS[b][:], op0=ALU.mult, op1=ALU.add),
                         reads=[("pS", b), ("S", h), "cds"], writes=[("S", h)])
                p.op("act", lambda e: e.copy(out=Sb[h][:], in_=St[h][:]), reads=[("S", h)], writes=[("Sb", h)])
            for ec in range(2):
                p.op("pe", lambda e: e.transpose(pT[b][:, ec, :], Yb[b][:, ec * 128:(ec + 1) * 128], ident[:]), reads=[("Yb", b), "ident"], writes=[("pT", b)])
            p.op("act", lambda e: e.copy(out=YT[gb][:, 2 * h:2 * h + 2, cs], in_=pT[b][:, 0:2, :]), reads=[("pT", b)], writes=[("YT", gb, h)])
        if cc == CG - 1:
            p.dma("sp", yv[:, :, cgi * W:(cgi + 1) * W], YT[gb][:], reads=[("YT", gb, h) for h in range(H)])
    p.end_phase()


def nsa_consts(slopes):
    pp = np.arange(128, dtype=np.float64)[:, None]
    c = np.arange(512, dtype=np.float64)[None, :]
    dt = np.zeros((128, 6, 512), np.float32)
    dt[:, 0, :] = -(pp - c)
    for k in range(4):
        dist = pp - c + 128 * k
        dt[:, 1 + k, :] = np.where(dist >= 0, -dist, NEG)
    dist = pp - c + 512
    dt[:, 5, :] = np.where(dist < 512, -dist, NEG)
    u = np.arange(255, dtype=np.float64)[None, :]
    dist = pp - 16 * (u - 248) - 31
    dc = np.where(dist >= 0, -dist, NEG).astype(np.float32)
    jp = np.arange(126)[None, :] - 62
    hi = (np.arange(128)[:, None] >= 64).astype(np.int64)
    valid = jp <= hi
    forced = (jp == hi) | (jp == hi - 1)
    mm = (valid & ~forced).astype(np.float32)
    ma = np.where(forced, 1e9, np.where(valid, 0.0, -1e9)).astype(np.float32)
    slsc = np.tile((slopes / SCALE)[None, :], (128, 1)).astype(np.float32)
    deltas = 512 + 128 * np.arange(28)
    tab = np.tile((-slopes[:, None] * deltas[None, :])[None], (128, 1, 1)).astype(np.float32)
    return dict(dt=dt, dc=dc, mm=mm, ma=ma, slsc=slsc, tab=tab)


def ph_nsa(p, qpT, kvpT, attnT, w1, w2, posT, cst, ident_d, G, HG, S):
    p.begin_phase()
    NH = G * HG
    NT = S // 128
    NCMP = (S - 32) // 16 + 1
    NG3 = NH * 3

    def const(name, shape, src, dtype=F32, q="sp"):
        t = p.sb(name, shape, dtype)
        p.dma(q, t[:], src, writes=[name])
        return t
    Dt = const("Dt", [128, 6, 512], cst["dt"])
    Dc = const("Dc", [128, 255], cst["dc"])
    MM = const("MM", [128, 126], cst["mm"])
    MA = const("MA", [128, 126], cst["ma"])
    slsc = const("slsc", [128, NH], cst["slsc"])
    tab = const("tab", [128, NH, 28], cst["tab"])
    ident = const("ident", [128, 128], ident_d, BF16, "pool")
    w1s = const("w1s", [128, 2, 32, 128], w1.rearrange("y (l d) m -> d y l m", d=128), BF16, "pool")
    w2s = const("w2s", [128, 2, 128], w2.rearrange("y m n -> m y n"), BF16, "pool")
    poss = const("poss", [128, 2, 32], posT.rearrange("y d l -> d y l"), BF16, "pool")
    qg = p.sb("qg", [128, HG, S], BF16)
    ksT = p.sb("ksT", [128, S], BF16)
    kwT = p.sb("kwT", [128, S], BF16)
    vs = p.sb("vs", [128, NT, 132], BF16)
    vw = p.sb("vw", [128, NT, 132], BF16)
    tokT = p.sb("tokT", [128, S], BF16)
    kcT = p.sb("kcT", [128, 256], BF16)
    vc = p.sb("vc", [128, 2, 128], BF16)
    s1T = p.sb("s1T", [128, 256], BF16)
    bc = p.sb("bc", [128, 1], F32)
    NB_ = 3
    tmp = [p.sb("tmp%d" % i, [128, 512], F32) for i in range(NB_)]
    eb = [p.sb("eb%d" % i, [128, 512], BF16) for i in range(NB_)]
    eT = [p.sb("eT%d" % i, [128, 4, 128], BF16) for i in range(NB_)]
    efs = [p.sb("ef%d" % i, [128, 256], F32) for i in range(2)]
    pnbs = [p.sb("pnb%d" % i, [128, 256], BF16) for i in range(2)]
    Pacc = p.sb("Pacc", [128, 264], F32)
    imp = p.sb("imp", [128, 64], F32)
    sc2 = p.sb("sc2", [128, 64], F32)
    t8 = p.sb("t8", [128, 8], F32)
    t8b = p.sb("t8b", [128, 8], F32)
    selm = p.sb("selm", [128, 64], BF16)
    sm = [p.sb("sm%d" % i, [128, 4], F32) for i in range(2)]
    acc = [p.sb("acc%d" % i, [128, HG, 128], F32) for i in range(2)]
    accb = [p.sb("accb%d" % i, [128, HG, 128], BF16) for i in range(2)]
    AT = [p.sb("AT%d" % i, [128, HG, 128], F32) for i in range(2)]
    gt = p.sb("gt", [128, NT, NG3], F32)
    pS = [p.ps("pS%d" % i, [128, 512]) for i in range(NB_)]
    pT = [p.ps("pT%d" % i, [128, 4, 128], BF16) for i in range(NB_)]
    pO = [p.ps("pO%d" % i, [128, 132]) for i in range(2)]
    ctr = {"u": 0, "o": 0, "s": 0}
    p.op("dve", lambda e: e.memset(vs[:], 1.0), writes=["ksv"])
    p.op("dve", lambda e: e.memset(vw[:], 1.0), writes=["kwv"])
    p.dma("pool", tokT[:NG3, :], qpT[NH * 128:NH * 128 + NG3, :], writes=["tokT"])
    for ti in range(NT):
        b = ti % 2
        p.op("pe", lambda e: e.transpose(pT[b][:, 0, 0:NG3], tokT[:NG3, ti * 128:(ti + 1) * 128], ident[:NG3, :NG3]), reads=["tokT", "ident"], writes=[("pT", b)])
        p.op("act", lambda e: e.activation(out=gt[:, ti, :], in_=pT[b][:, 0, 0:NG3], func=AF.Sigmoid), reads=[("pT", b)], writes=["gt"])

    def unit(h, hh, ti, kT, k0, c_lo, ncol, Dap, bias, mask, Vt, vch0, po, first, last, rk):
        u = ctr["u"]; ctr["u"] += 1
        b = u % NB_
        ts = slice(ti * 128, (ti + 1) * 128)
        p.op("pe", lambda e: e.matmul(pS[b][:, c_lo:ncol], lhsT=qg[:, h, ts], rhs=kT[:, k0 + c_lo:k0 + ncol], start=True, stop=True),
             reads=["qg", rk], writes=[("pS", b)])
        p.op("dve", lambda e: e.scalar_tensor_tensor(out=tmp[b][:, c_lo:ncol], in0=Dap[:, c_lo:ncol], scalar=slsc[:, hh:hh + 1], in1=pS[b][:, c_lo:ncol], op0=ALU.mult, op1=ALU.add),
             reads=[("pS", b), "Dt", "slsc"], writes=[("tmp", b)])
        if bias is None:
            p.op("act", lambda e: e.activation(out=eb[b][:, c_lo:ncol], in_=tmp[b][:, c_lo:ncol], func=AF.Exp, scale=SCALE), reads=[("tmp", b)], writes=[("eb", b)])
        else:
            p.op("act", lambda e: e.activation(out=eb[b][:, c_lo:ncol], in_=tmp[b][:, c_lo:ncol], func=AF.Exp, scale=SCALE, bias=bias),
                 reads=[("tmp", b), "tab"], writes=[("eb", b)])
        if mask is not None:
            p.op("pool", lambda e: e.tensor_tensor(out=eb[b][:].rearrange("p (j l) -> p j l", l=64), in0=eb[b][:].rearrange("p (j l) -> p j l", l=64),
                                                   in1=mask.unsqueeze(2).to_broadcast([128, 8, 64]), op=ALU.mult),
                 reads=[("eb", b), "selm"], writes=[("eb", b)])
        kc0 = c_lo // 128
        nkc = ncol // 128
        for kc in range(kc0, nkc):
            p.op("pe", lambda e: e.transpose(pT[b][:, kc, :], eb[b][:, kc * 128:(kc + 1) * 128], ident[:]), reads=[("eb", b), "ident"], writes=[("pT", b)])
        p.op("act", lambda e: e.copy(out=eT[b][:, kc0:nkc, :], in_=pT[b][:, kc0:nkc, :]), reads=[("pT", b)], writes=[("eT", b)])
        for kc in range(kc0, nkc):
            p.op("pe", lambda e: e.matmul(po[0][:, 0:130], lhsT=eT[b][:, kc, :], rhs=Vt[:, vch0 + kc, 0:130], start=(first and kc == kc0), stop=(last and kc == nkc - 1)),
                 reads=[("eT", b), rk + "v"], writes=[po[1]])

    qv = qpT[0:NH * 128, :].rearrange("(x p) s -> p x s", p=128)
    for g in range(G):
        for hq in range(0, HG, 2):
            p.dma("pool", qg[:, hq:hq + 2, :], qv[:, g * HG + hq:g * HG + hq + 2, :], writes=["qg"])
        kvrow = lambda j: kvpT[(j * G + g) * 128:(j * G + g + 1) * 128, :]
        p.dma("pool", ksT[:], kvrow(2), writes=["ks"])
        p.dma("pool", kwT[:], kvrow(4), writes=["kw"])
        for (j, Vt, key) in ((3, vs, "ksv"), (5, vw, "kwv")):
            p.dma("pool", tokT[:], kvrow(j), writes=["tokT"])
            for ti in range(NT):
                b = ti % 2
                p.op("pe", lambda e: e.transpose(pT[b][:, 0, :], tokT[:, ti * 128:(ti + 1) * 128], ident[:]), reads=["tokT", "ident"], writes=[("pT", b)])
                p.op("act", lambda e: e.copy(out=Vt[:, ti, 0:128], in_=pT[b][:, 0, :]), reads=[("pT", b)], writes=[key])
        for ty in range(2):
            p.dma("pool", tokT[:], kvrow(ty), writes=["tokT"])
            for l in range(32):
                p.op("pe", lambda e: e.matmul(pS[0][:, 0:1], lhsT=w1s[:, ty, l, :], rhs=poss[:, ty, l:l + 1], start=(l == 0), stop=(l == 31)),
                     reads=["w1s", "poss"], writes=[("pS", 0)])
            p.op("dve", lambda e: e.tensor_copy(out=bc[:], in_=pS[0][:, 0:1]), reads=[("pS", 0)], writes=["bc"])
            for l in range(32):
                p.op("pe", lambda e: e.matmul(pS[1][:, 0:NCMP], lhsT=w1s[:, ty, l, :], rhs=tokT[:, l:l + 16 * (NCMP - 1) + 1:16], start=(l == 0), stop=(l == 31)),
                     reads=["w1s", "tokT"], writes=[("pS", 1)])
            p.op("act", lambda e: e.activation(out=s1T[:, 0:NCMP], in_=pS[1][:, 0:NCMP], func=AF.Silu, bias=bc[:, 0:1]), reads=[("pS", 1), "bc"], writes=["s1T"])
            if ty == 0:
                p.op("pe", lambda e: e.matmul(pS[0][:, 0:NCMP], lhsT=w2s[:, 0, :], rhs=s1T[:, 0:NCMP], start=True, stop=True), reads=["w2s", "s1T"], writes=[("pS", 0)])
                p.op("dve", lambda e: e.tensor_copy(out=kcT[:, 0:NCMP], in_=pS[0][:, 0:NCMP]), reads=[("pS", 0)], writes=["kcT"])
            else:
                p.op("dve", lambda e: e.memset(vc[:], 0.0), writes=["vc"])
                for ch in range((NCMP + 127) // 128):
                    n0 = ch * 128
                    nn = min(128, NCMP - n0)
                    p.op("pe", lambda e: e.matmul(pS[0][:nn, 0:128], lhsT=s1T[:, n0:n0 + nn], rhs=w2s[:, 1, :], start=True, stop=True), reads=["w2s", "s1T"], writes=[("pS", 0)])
                    p.op("dve", lambda e: e.tensor_copy(out=vc[:nn, ch, :], in_=pS[0][:nn, 0:128]), reads=[("pS", 0)], writes=["vc"])
        for ti in range(NT):
            ab = ctr["o"] % 2; ctr["o"] += 1
            A = acc[ab]
            ts = slice(ti * 128, (ti + 1) * 128)
            n_hi = min(NCMP, 8 * ti + 7)
            u0 = 248 - 8 * ti
            p.op("dve", lambda e: e.memset(Pacc[:], 0.0), writes=["Pacc"])
            for h in range(HG):
                hh = g * HG + h
                si = ctr["s"] % 2; ctr["s"] += 1
                s_ = sm[si]
                cb_ = ctr["u"] % NB_; ctr["u"] += 1
                ef = efs[si]; pnb = pnbs[si]
                p.op("pe", lambda e: e.matmul(pS[cb_][:, 0:n_hi], lhsT=qg[:, h, ts], rhs=kcT[:, 0:n_hi], start=True, stop=True), reads=["qg", "kcT"], writes=[("pS", cb_)])
                p.op("dve", lambda e: e.scalar_tensor_tensor(out=tmp[cb_][:, 0:n_hi], in0=Dc[:, u0:u0 + n_hi], scalar=slsc[:, hh:hh + 1], in1=pS[cb_][:, 0:n_hi], op0=ALU.mult, op1=ALU.add),
                     reads=[("pS", cb_), "Dc", "slsc"], writes=[("tmp", cb_)])
                p.op("dve", lambda e: e.reduce_max(out=s_[:, 0:1], in_=tmp[cb_][:, 0:n_hi], axis=AX.X), reads=[("tmp", cb_)], writes=[("sm", si)])
                p.op("dve", lambda e: e.tensor_scalar(out=s_[:, 0:1], in0=s_[:, 0:1], scalar1=-1.0e6, scalar2=-SCALE, op0=ALU.max, op1=ALU.mult), reads=[("sm", si)], writes=[("sm", si)])
                p.op("act", lambda e: e.activation(out=ef[:, 0:n_hi], in_=tmp[cb_][:, 0:n_hi], func=AF.Exp, scale=SCALE, bias=s_[:, 0:1], accum_out=s_[:, 1:2]),
                     reads=[("tmp", cb_), ("sm", si)], writes=[("ef", si), ("sm", si)])
                p.op("dve", lambda e: e.tensor_scalar(out=s_[:, 1:2], in0=s_[:, 1:2], scalar1=1.0e-30, scalar2=None, op0=ALU.max), reads=[("sm", si)], writes=[("sm", si)])
                p.op("dve", lambda e: e.reciprocal(out=s_[:, 1:2], in_=s_[:, 1:2]), reads=[("sm", si)], writes=[("sm", si)])
                p.op("dve", lambda e: e.scalar_tensor_tensor(out=Pacc[:, 1:1 + n_hi], in0=ef[:, 0:n_hi], scalar=s_[:, 1:2], in1=Pacc[:, 1:1 + n_hi], op0=ALU.mult, op1=ALU.add),
                     reads=[("ef", si), ("sm", si), "Pacc"], writes=["Pacc"])
                p.op("dve", lambda e: e.memset(pnb[:], 0.0), writes=[("pnb", si)])
                p.op("dve", lambda e: e.tensor_scalar(out=pnb[:, 0:n_hi], in0=ef[:, 0:n_hi], scalar1=s_[:, 1:2], scalar2=None, op0=ALU.mult), reads=[("ef", si), ("sm", si)], writes=[("pnb", si)])
                nch = (n_hi + 127) // 128
                for ch in range(nch):
                    p.op("pe", lambda e: e.transpose(pT[cb_][:, ch, :], pnb[:, ch * 128:(ch + 1) * 128], ident[:]), reads=[("pnb", si), "ident"], writes=[("pT", cb_)])
                p.op("act", lambda e: e.copy(out=eT[cb_][:, 0:nch, :], in_=pT[cb_][:, 0:nch, :]), reads=[("pT", cb_)], writes=[("eT", cb_)])
                for ch in range(nch):
                    p.op("pe", lambda e: e.matmul(pO[si][:, 0:128], lhsT=eT[cb_][:, ch, :], rhs=vc[:, ch, :], start=(ch == 0), stop=(ch == nch - 1)), reads=[("eT", cb_), "vc"], writes=[("pO", si)])
                p.op("dve", lambda e: e.tensor_scalar(out=A[:, h, :], in0=pO[si][:, 0:128], scalar1=gt[:, ti, hh * 3:hh * 3 + 1], scalar2=None, op0=ALU.mult),
                     reads=[("pO", si), "gt"], writes=[("acc", ab, h)])
            Pv = Pacc[:, 0:256].rearrange("p (j k) -> p j k", k=4)
            Pv4 = Pacc[:, 4:260].rearrange("p (j k) -> p j k", k=4)
            p.op("dve", lambda e: e.scalar_tensor_tensor(out=imp[:], in0=Pv[:, :, 1], scalar=2.0, in1=Pv[:, :, 0], op0=ALU.mult, op1=ALU.add), reads=["Pacc"], writes=["imp"])
            p.op("dve", lambda e: e.scalar_tensor_tensor(out=imp[:], in0=Pv[:, :, 2], scalar=2.0, in1=imp[:], op0=ALU.mult, op1=ALU.add), reads=["Pacc", "imp"], writes=["imp"])
            p.op("dve", lambda e: e.scalar_tensor_tensor(out=imp[:], in0=Pv[:, :, 3], scalar=2.0, in1=imp[:], op0=ALU.mult, op1=ALU.add), reads=["Pacc", "imp"], writes=["imp"])
            p.op("dve", lambda e: e.tensor_tensor(out=imp[:], in0=imp[:], in1=Pv4[:, :, 0], op=ALU.add), reads=["Pacc", "imp"], writes=["imp"])
            m0 = 62 - 2 * ti
            p.op("dve", lambda e: e.tensor_tensor(out=imp[:], in0=imp[:], in1=MM[:, m0:m0 + 64], op=ALU.mult), reads=["imp", "MM"], writes=["imp"])
            p.op("dve", lambda e: e.tensor_tensor(out=imp[:], in0=imp[:], in1=MA[:, m0:m0 + 64], op=ALU.add), reads=["imp", "MA"], writes=["imp"])
            p.op("dve", lambda e: e.memset(imp[:, 0:1], 1.0e9), reads=["imp"], writes=["imp"])
            p.op("dve", lambda e: e.max(out=t8[:], in_=imp[:]), reads=["imp"], writes=["t8"])
            p.op("dve", lambda e: e.match_replace(out=sc2[:], in_to_replace=t8[:], in_values=imp[:], imm_value=-3.0e9), reads=["imp", "t8"], writes=["sc2"])
            p.op("dve", lambda e: e.max(out=t8b[:], in_=sc2[:]), reads=["sc2"], writes=["t8b"])
            p.op("dve", lambda e: e.tensor_scalar(out=selm[:], in0=imp[:], scalar1=t8b[:, 7:8], scalar2=None, op0=ALU.is_ge), reads=["imp", "t8b"], writes=["selm"])
            for h in range(HG):
                hh = g * HG + h
                for br in (1, 2):
                    ob = ctr["s"] % 2; ctr["s"] += 1
                    po = (pO[ob], ("pO", ob))
                    units = []
                    if br == 1:
                        nk = ti // 4
                        for kt in range(nk + 1):
                            if kt == nk:
                                units.append(dict(kT=ksT, k0=512 * kt, c_lo=0, ncol=128 * (ti % 4) + 128, Dap=Dt[:, 1 + ti % 4, :], bias=None,
                                                  mask=selm[:, 8 * kt:8 * kt + 8], Vt=vs, vch0=4 * kt, rk="ks"))
                            else:
                                di = (128 * ti - 512 * kt - 512) // 128
                                units.append(dict(kT=ksT, k0=512 * kt, c_lo=0, ncol=512, Dap=Dt[:, 0, :], bias=tab[:, hh, di:di + 1],
                                                  mask=selm[:, 8 * kt:8 * kt + 8], Vt=vs, vch0=4 * kt, rk="ks"))
                    else:
                        if ti >= 1:
                            c_lo = max(0, 512 - 128 * ti)
                            units.append(dict(kT=kwT, k0=128 * ti - 512, c_lo=c_lo, ncol=512, Dap=Dt[:, 5, :], bias=None, mask=None, Vt=vw, vch0=ti - 4, rk="kw"))
                        units.append(dict(kT=kwT, k0=128 * ti, c_lo=0, ncol=128, Dap=Dt[:, 1, :], bias=None, mask=None, Vt=vw, vch0=ti, rk="kw"))
                    for ui, ud in enumerate(units):
                        unit(h, hh, ti, po=po, first=(ui == 0), last=(ui == len(units) - 1), **ud)
                    s_ = sm[ob]
                    p.op("dve", lambda e: e.reciprocal(out=s_[:, 2:3], in_=pO[ob][:, 128:129]), reads=[("pO", ob)], writes=[("sm2", ob)])
                    p.op("dve", lambda e: e.tensor_tensor(out=s_[:, 3:4], in0=s_[:, 2:3], in1=gt[:, ti, hh * 3 + br:hh * 3 + br + 1], op=ALU.mult), reads=[("sm2", ob), "gt"], writes=[("sm3", ob)])
                    p.op("dve", lambda e: e.scalar_tensor_tensor(out=A[:, h, :], in0=pO[ob][:, 0:128], scalar=s_[:, 3:4], in1=A[:, h, :], op0=ALU.mult, op1=ALU.add),
                         reads=[("pO", ob), ("sm3", ob), ("acc", ab, h)], writes=[("acc", ab, h)])
            Ab_ = accb[ab]
            p.op("pool", lambda e: e.tensor_copy(out=Ab_[:], in_=A[:]), reads=[("acc", ab, h) for h in range(HG)], writes=[("accb", ab)])
            for h4 in range(0, HG, 4):
                b = (h4 // 4) % 2
                nk = min(4, HG - h4)
                for k in range(nk):
                    p.op("pe", lambda e: e.transpose(pT[b][:, k, :], Ab_[:, h4 + k, :], ident[:]), reads=[("accb", ab), "ident"], writes=[("pT", b)])
                p.op("act", lambda e: e.copy(out=AT[ab][:, h4:h4 + nk, :], in_=pT[b][:, 0:nk, :]), reads=[("pT", b)], writes=[("AT", ab, h4)])
            p.dma("sp", attnT.rearrange("(x p) s -> p x s", p=128)[:, g * HG:(g + 1) * HG, ts], AT[ab][:], reads=[("AT", ab, h4) for h4 in range(0, HG, 4)])
    p.end_phase()


from concourse.bass_utils import run_bass_kernel_spmd

D_MODEL = 4096
SEQ = 4096
BATCH = 4
D_FF = 11008
NCORES = 4


def build_fused():
    p = Prog()
    D, S, F = D_MODEL, SEQ, D_FF
    I = lambda name, shape, dt=F32: p.dram(name, shape, dt, "ExternalInput")
    T_ = lambda name, shape, dt=F32: p.dram(name, shape, dt, "Internal")
    xT = I("xT", [D, S])
    w_ret_in = I("w_ret_in", [D, 4 * D]); w_ret_out = I("w_ret_out", [D, D])
    w_kv = I("w_kv", [D, 3072]); w_q = I("w_q", [D, D + 96]); w_nsa_out = I("w_nsa_out", [D, D])
    w_fi = [I("w_fi%d" % l, [D, 2 * F]) for l in range(2)]
    w_fo = [I("w_fo%d" % l, [F, D]) for l in range(2)]
    cwl = [I("cw%d" % l, [128, F // 128, 4]) for l in range(2)]
    vec = {k: I(k, [128, D // 128]) for k in ("an0", "an1", "fn0", "fn1", "kvn", "finn", "gng")}
    w1 = I("w1", [2, 4096, 128]); w2 = I("w2", [2, 128, 128]); posT = I("posT", [2, 128, 32])
    ident = I("ident", [128, 128])
    rc = [dict(dmt=I("dmt%d" % i, [128, 8, 128]), qd=I("qd%d" % i, [128, 8, 128]), kd=I("kd%d" % i, [128, 8]), cd=I("cd%d" % i, [128, 8])) for i in range(2)]
    cst = dict(dt=I("dt", [128, 6, 512]), dc=I("dc", [128, 255]), mm=I("mm", [128, 126]), ma=I("ma", [128, 126]),
               slsc=I("slsc", [128, 32]), tab=I("tab", [128, 32, 28]))
    oT = p.dram("oT", [D, S], F32, "ExternalOutput")
    projT = T_("projT", [4 * D, S]); ynT = T_("ynT", [D, S])
    x1T = T_("x1T", [D, S]); x2T = T_("x2T", [D, S]); x3T = T_("x3T", [D, S]); x4T = T_("x4T", [D, S])
    gatedT = T_("gatedT", [F, S]); kvpT = T_("kvpT", [3072, S]); qpT = T_("qpT", [D + 96, S]); attnT = T_("attnT", [D, S])

    ph_gemm(p, w_ret_in, xT, projT, D, 4 * D, S, 2048, "norm", "rstd", g=vec["an0"])
    for hg in range(2):
        r = hg * 2048
        ph_retention(p, projT[r:r + 2048, :], projT[D + r:D + r + 2048, :], projT[2 * D + r:2 * D + r + 2048, :], ynT[r:r + 2048, :],
                     rc[hg]["dmt"], rc[hg]["qd"], rc[hg]["kd"], rc[hg]["cd"], ident, 8, S, CG=2)
    ph_gemm(p, w_ret_out, ynT, x1T, D, D, S, 2048, "gate", "resid", g=vec["gng"], gT=projT[3 * D:4 * D, :], rT=xT)
    ph_ffn_in(p, w_fi[0], x1T, gatedT, vec["fn0"], cwl[0], D, F, S, 2048)
    ph_gemm(p, w_fo[0], gatedT, x2T, F, D, S, 512, "plain", "resid", rT=x1T, WSPLIT=8)
    ph_gemm(p, w_kv, x2T, kvpT, D, 3072, S, 2048, "norm", "rstd", g=vec["kvn"])
    ph_gemm(p, w_q, x2T, qpT, D, D + 96, S, 2048, "norm", "rstd", g=vec["an1"])
    ph_nsa(p, qpT, kvpT, attnT, w1, w2, posT, cst, ident, 4, 8, S)
    ph_gemm(p, w_nsa_out, attnT, x3T, D, D, S, 2048, "plain", "resid", rT=x2T)
    ph_ffn_in(p, w_fi[1], x3T, gatedT, vec["fn1"], cwl[1], D, F, S, 2048)
    ph_gemm(p, w_fo[1], gatedT, x4T, F, D, S, 512, "plain", "resid", rT=x3T, WSPLIT=8)
    ph_final_norm(p, x4T, oT, vec["finn"], D, S)
    return p.finish()


def _gl(v):
    v = np.asarray(v, np.float32)
    return np.ascontiguousarray(v.reshape(-1, 128).T)


def _ret_consts(heads):
    Dh = 256
    lg = np.log1p(-np.exp2(-5.0 - np.asarray(heads, dtype=np.float64)))
    i = np.arange(128, dtype=np.float64)
    scale = Dh ** -0.5
    Hn = len(heads)
    dmt = np.zeros((128, Hn, 128), np.float32)
    qd = np.zeros((128, Hn, 128), np.float32)
    kd = np.zeros((128, Hn), np.float32)
    cd = np.zeros((128, Hn), np.float32)
    for a, l in enumerate(lg):
        diff = i[None, :] - i[:, None]
        dmt[:, a, :] = np.where(diff >= 0, np.exp(np.maximum(diff, 0) * l), 0.0) * scale
        qd[:, a, :] = np.exp((i + 1.0) * l)[None, :]
        kd[:, a] = np.exp((127.0 - i) * l) * scale
        cd[:, a] = np.exp(128 * l)
    return dmt, qd, kd, cd


def kernel(x, attn_norm, ffn_norm, w_ret_in, ret_gn_gain, w_ret_out, kv_norm, w_kv,
           cmp_pos, cmp_w1, cmp_w2, w_nsa_q, w_nsa_out, w_ffn_in, conv_w, conv_b,
           w_ffn_out, final_norm):
    f32 = lambda a: np.ascontiguousarray(np.asarray(a, np.float32))
    x = f32(x)
    common = dict(w_ret_in=f32(w_ret_in[0]), w_ret_out=f32(w_ret_out[0]), w_kv=f32(w_kv), w_q=f32(w_nsa_q[0]), w_nsa_out=f32(w_nsa_out[0]),
                  w1=f32(cmp_w1), w2=f32(cmp_w2), posT=f32(np.asarray(cmp_pos).transpose(0, 2, 1)), ident=np.eye(128, dtype=np.float32),
                  an0=_gl(attn_norm[0]), an1=_gl(attn_norm[1]), fn0=_gl(ffn_norm[0]), fn1=_gl(ffn_norm[1]), kvn=_gl(kv_norm),
                  finn=_gl(final_norm), gng=_gl(ret_gn_gain[0]))
    for l in range(2):
        common["w_fi%d" % l] = f32(w_ffn_in[l])
        common["w_fo%d" % l] = f32(w_ffn_out[l])
        cw = np.concatenate([np.asarray(conv_w[l], np.float32), np.asarray(conv_b[l], np.float32)[None]], 0)
        common["cw%d" % l] = np.ascontiguousarray(cw.reshape(4, D_FF // 128, 128).transpose(2, 1, 0))
    for i in range(2):
        dmt, qd, kd, cd = _ret_consts(list(range(i * 8, i * 8 + 8)))
        common.update({"dmt%d" % i: dmt, "qd%d" % i: qd, "kd%d" % i: kd, "cd%d" % i: cd})
    slopes = np.exp2(-8.0 * (np.arange(32, dtype=np.float64) + 1.0) / 32.0)
    common.update(nsa_consts(slopes))
    ins = []
    for b in range(NCORES):
        d = dict(common)
        d["xT"] = np.ascontiguousarray(x[b].T)
        ins.append(d)
    nc = build_fused()
    res = run_bass_kernel_spmd(nc, ins, core_ids=list(range(NCORES)))
    out = np.empty((BATCH, SEQ, D_MODEL), np.float32)
    for b in range(NCORES):
        out[b] = np.asarray(res.results[b]["oT"]).T
    return out
```

```python
import numpy as np
import concourse.bass as bass
import concourse.mybir as mybir
from contextlib import ExitStack

F32 = mybir.dt.float32
F32R = mybir.dt.float32r
BF16 = mybir.dt.bfloat16
AF = mybir.ActivationFunctionType
ALU = mybir.AluOpType
AX = mybir.AxisListType

CE = ("pe", "act", "dve", "pool")
NDMA = 48


class Prog:
    def __init__(self, name="k"):
        self.nc = bass.Bass("TRN2", target_bir_lowering=False)
        nc = self.nc
        self.stack = ExitStack()
        self.eng = {"pe": nc.tensor, "act": nc.scalar, "dve": nc.vector,
                    "pool": nc.gpsimd, "sp": nc.sync}
        self.sem = {e: self.stack.enter_context(nc.semaphore("s_" + e)) for e in CE}
        self.cnt = {e: 0 for e in CE}
        self.dsem = [self.stack.enter_context(nc.semaphore("d%d" % i)) for i in range(NDMA)]
        self.dcnt = [0] * NDMA
        self.dnext = 0
        self.know = {e: {} for e in self.eng}
        self.lastw = {}
        self.rd = {}
        self.nops = 0
        self.out_events = []
        self.pstack = None
        self.pid = 0
        self.sw_out = []
        self.sw_budget = 10000

    def dram(self, name, shape, dtype, kind, addr_space=None):
        if addr_space is not None:
            return self.nc.dram_tensor(name, list(shape), dtype, kind=kind, addr_space=addr_space).ap()
        return self.nc.dram_tensor(name, list(shape), dtype, kind=kind).ap()

    def sb(self, name, shape, dtype):
        st = self.pstack if self.pstack is not None else self.stack
        return st.enter_context(self.nc.sbuf_tensor("p%d_%s" % (self.pid, name), list(shape), dtype))

    def ps(self, name, shape, dtype=F32):
        st = self.pstack if self.pstack is not None else self.stack
        return st.enter_context(self.nc.psum_tensor("p%d_%s" % (self.pid, name), list(shape), dtype))

    def begin_phase(self):
        self.pid += 1
        self.pstack = ExitStack()

    def end_phase(self):
        self.barrier()
        self.pstack.close()
        self.pstack = None

    def barrier(self):
        targets = [(e, self.cnt[e]) for e in CE if self.cnt[e] > 0]
        targets += [(i, self.dcnt[i]) for i in range(NDMA) if self.dcnt[i] > 0]
        for e in self.eng:
            kn = self.know[e]
            for sk, v in targets:
                if kn.get(sk, 0) < v:
                    self.eng[e].wait_ge(self._semh(sk), v)
                    kn[sk] = v
        self.lastw = {}
        self.rd = {}
        self.sw_out = []

    def _semh(self, k):
        return self.sem[k] if isinstance(k, str) else self.dsem[k]

    def _need(self, e, reads, writes):
        ev = []
        for k in reads:
            w = self.lastw.get(k)
            if w is not None:
                ev.append(w)
        for k in writes:
            w = self.lastw.get(k)
            if w is not None:
                ev.append(w)
            ev.extend(self.rd.get(k, ()))
        need = {}
        kn = self.know[e]
        for (sk, v, clk) in ev:
            if e == "pe" and sk == "pe":
                continue
            if kn.get(sk, 0) >= v:
                continue
            if need.get(sk, 0) < v:
                need[sk] = v
        return need, ev

    def _wait(self, e, need, ev):
        eng = self.eng[e]
        kn = self.know[e]
        for sk, v in need.items():
            eng.wait_ge(self._semh(sk), v)
            kn[sk] = v
        for (sk, v, clk) in ev:
            if clk:
                for ck, cv in clk.items():
                    if kn.get(ck, 0) < cv:
                        kn[ck] = cv

    def _record(self, event, reads, writes):
        for k in writes:
            self.lastw[k] = event
            self.rd[k] = []
        for k in reads:
            if k in writes:
                continue
            self.rd.setdefault(k, []).append(event)

    def op(self, e, fn, reads=(), writes=()):
        need, ev = self._need(e, reads, writes)
        self._wait(e, need, ev)
        ins = fn(self.eng[e])
        self.cnt[e] += 1
        ins.then_inc(self.sem[e], 1)
        kn = self.know[e]
        clk = {k: v for k, v in kn.items() if isinstance(k, str)}
        clk[e] = self.cnt[e]
        if e != "pe":
            pass
        event = (e, self.cnt[e], clk)
        self._record(event, reads, writes)
        self.nops += 1
        return event

    def dma(self, q, out, in_, reads=(), writes=(), is_output=False):
        need, ev = self._need(q, reads, writes)
        nd = 0
        if q == "pool":
            nd = 1
            for d_ in list(out.shape)[:-1]:
                nd *= int(d_)
            tot = sum(n for (_, n) in self.sw_out) + nd
            while self.sw_out and tot > self.sw_budget:
                (osk, ov, _), on = self.sw_out.pop(0)
                tot -= on
                if self.know[q].get(osk, 0) < ov:
                    need[osk] = max(need.get(osk, 0), ov)
        s = self.dnext
        self.dnext = (self.dnext + 1) % NDMA
        prev = self.dcnt[s]
        if prev > 0 and self.know[q].get(s, 0) < prev:
            need[s] = max(need.get(s, 0), prev)
        self._wait(q, need, ev)
        ins = self.eng[q].dma_start(out=out, in_=in_)
        self.dcnt[s] = prev + 16
        ins.then_inc(self.dsem[s], 16)
        event = (s, self.dcnt[s], None)
        if q == "pool":
            self.sw_out.append((event, nd))
        self._record(event, reads, writes)
        if is_output:
            self.out_events.append(event)
        self.nops += 1
        return event

    def coll(self, kind, in_ap, out_ap, groups, reads=(), writes=()):
        q = "pool"
        need, ev = self._need(q, reads, writes)
        s = self.dnext
        self.dnext = (self.dnext + 1) % NDMA
        prev = self.dcnt[s]
        if prev > 0 and self.know[q].get(s, 0) < prev:
            need[s] = max(need.get(s, 0), prev)
        self._wait(q, need, ev)
        ins = self.nc.gpsimd.collective_compute(kind, mybir.AluOpType.bypass, replica_groups=groups, ins=[in_ap], outs=[out_ap])
        self.dcnt[s] = prev + 16
        ins.then_inc(self.dsem[s], 16)
        event = (s, self.dcnt[s], None)
        self._record(event, reads, writes)
        return event

    def finish(self):
        e = "sp"
        for (sk, v, _) in self.out_events:
            if self.know[e].get(sk, 0) < v:
                self.eng[e].wait_ge(self._semh(sk), v)
                self.know[e][sk] = v
        self.stack.close()
        return self.nc

NORM_EPS = 1e-6
GN_EPS = 1e-5
SCALE = 128 ** -0.5
NEG = -1.0e9


def ph_gemm(p, w, src, out, K, N, T, TT, pro, epi, g=None, gT=None, rT=None, src_dt=F32, WSPLIT=4, out_is_output=False):
    p.begin_phase()
    KC = K // 128
    NCH = (N + 127) // 128
    if pro in ("norm", "gate"):
        gs = p.sb("gs", [128, KC], F32)
        p.dma("sp", gs[:], g, writes=["gs"])
    hT = p.sb("hT", [128, KC, TT], BF16)
    NW = 2
    wb = [p.sb("wb%d" % i, [128, KC, 128], BF16) for i in range(NW)]
    NO = 3
    ob = [p.sb("ob%d" % i, [128, 512], F32) for i in range(NO)]
    NP = 3
    pt = [p.ps("pt%d" % i, [128, 512]) for i in range(NP)]
    NTS = TT // 512
    if pro == "norm":
        ones = p.sb("ones", [128, 128], BF16)
        p.op("dve", lambda e: e.memset(ones[:], 1.0), writes=["ones"])
        epsb = p.sb("epsb", [128, 1], F32)
        p.op("dve", lambda e: e.memset(epsb[:], NORM_EPS), writes=["epsb"])
        rstd = p.sb("rstd", [128, TT], F32)
        ss = [p.ps("ss%d" % i, [128, 512]) for i in range(NTS)]
        xs = [p.sb("xs%d" % i, [128, 512], F32) for i in range(3)]
        sq = [p.sb("sq%d" % i, [128, 512], BF16) for i in range(2)]
    if pro == "gate":
        xs = [p.sb("xs%d" % i, [128, 512], src_dt) for i in range(2)]
        gx = [p.sb("gx%d" % i, [128, 512], F32) for i in range(2)]
    if epi == "resid":
        rb = [p.sb("rb%d" % i, [128, 512], F32) for i in range(NO)]
    wv = w.rearrange("(c p) n -> p c n", p=128)
    xv = src.rearrange("(c p) t -> p c t", p=128)
    ksplit = [(i * KC // WSPLIT, (i + 1) * KC // WSPLIT) for i in range(WSPLIT)]
    ctr = {"x": 0, "o": 0, "p": 0, "w": 0}
    for tp in range(T // TT):
        t0 = tp * TT
        if pro == "plain":
            for (k0, k1) in ksplit:
                p.dma("pool", hT[:, k0:k1, :], xv[:, k0:k1, t0:t0 + TT], writes=[("hT", k) for k in range(k0, k1)])
        elif pro == "norm":
            for kc in range(KC):
                for ts in range(NTS):
                    i = ctr["x"]; ctr["x"] += 1
                    xb = xs[i % 3]; sb_ = sq[i % 2]
                    c0 = ts * 512
                    p.dma("sp", xb[:], xv[:, kc, t0 + c0:t0 + c0 + 512], writes=[("xs", i % 3)])
                    p.op("act", lambda e: e.activation(out=sb_[:], in_=xb[:], func=AF.Square), reads=[("xs", i % 3)], writes=[("sq", i % 2)])
                    p.op("pe", lambda e: e.matmul(ss[ts][:], lhsT=ones[:], rhs=sb_[:], start=(kc == 0), stop=(kc == KC - 1)),
                         reads=[("sq", i % 2), "ones"], writes=[("ss", ts)])
                    p.op("dve", lambda e: e.tensor_scalar(out=hT[:, kc, c0:c0 + 512], in0=xb[:], scalar1=gs[:, kc:kc + 1], scalar2=None, op0=ALU.mult),
                         reads=[("xs", i % 3), "gs"], writes=[("hT", kc)])
            for ts in range(NTS):
                c0 = ts * 512
                p.op("act", lambda e: e.activation(out=rstd[:, c0:c0 + 512], in_=ss[ts][:], func=AF.Sqrt, scale=1.0 / K, bias=epsb[:, 0:1]),
                     reads=[("ss", ts), "epsb"], writes=[("rstd", ts)])
                p.op("dve", lambda e: e.reciprocal(out=rstd[:, c0:c0 + 512], in_=rstd[:, c0:c0 + 512]), reads=[("rstd", ts)], writes=[("rstd", ts)])
        elif pro == "gate":
            gv = gT.rearrange("(c p) t -> p c t", p=128)
            for kc in range(KC):
                for ts in range(NTS):
                    i = ctr["x"]; ctr["x"] += 1
                    xb = xs[i % 2]; gb = gx[i % 2]
                    c0 = ts * 512
                    p.dma("sp", xb[:], xv[:, kc, t0 + c0:t0 + c0 + 512], writes=[("xs", i % 2)])
                    p.dma("sp", gb[:], gv[:, kc, t0 + c0:t0 + c0 + 512], writes=[("gx", i % 2)])
                    p.op("act", lambda e: e.activation(out=gb[:], in_=gb[:], func=AF.Silu), reads=[("gx", i % 2)], writes=[("gx", i % 2)])
                    p.op("dve", lambda e: e.scalar_tensor_tensor(out=hT[:, kc, c0:c0 + 512], in0=xb[:], scalar=gs[:, kc:kc + 1], in1=gb[:], op0=ALU.mult, op1=ALU.mult),
                         reads=[("xs", i % 2), ("gx", i % 2), "gs"], writes=[("hT", kc)])
        for n in range(NCH):
            nw = min(128, N - n * 128)
            wi = ctr["w"]; ctr["w"] += 1
            ws = wi % NW
            for si, (k0, k1) in enumerate(ksplit):
                p.dma("pool", wb[ws][:, k0:k1, :nw], wv[:, k0:k1, n * 128:n * 128 + nw], writes=[("wb", ws, si)])
            for ts in range(NTS):
                c0 = ts * 512
                pi = ctr["p"]; ctr["p"] += 1
                pb = pt[pi % NP]
                for si, (k0, k1) in enumerate(ksplit):
                    for kc in range(k0, k1):
                        p.op("pe", lambda e: e.matmul(pb[:nw, :], lhsT=wb[ws][:, kc, :nw], rhs=hT[:, kc, c0:c0 + 512], start=(kc == 0), stop=(kc == KC - 1)),
                             reads=[("wb", ws, si), ("hT", kc)], writes=[("pt", pi % NP)])
                oi = ctr["o"]; ctr["o"] += 1
                o_ = ob[oi % NO]
                if epi == "rstd":
                    p.op("dve", lambda e: e.tensor_tensor(out=o_[:nw, :], in0=pb[:nw, :], in1=rstd[:nw, c0:c0 + 512], op=ALU.mult),
                         reads=[("pt", pi % NP), ("rstd", ts)], writes=[("ob", oi % NO)])
                elif epi == "resid":
                    r_ = rb[oi % NO]
                    p.dma("sp", r_[:nw, :], rT[n * 128:n * 128 + nw, t0 + c0:t0 + c0 + 512], writes=[("rb", oi % NO)])
                    p.op("dve", lambda e: e.tensor_tensor(out=o_[:nw, :], in0=pb[:nw, :], in1=r_[:nw, :], op=ALU.add),
                         reads=[("pt", pi % NP), ("rb", oi % NO)], writes=[("ob", oi % NO)])
                p.dma("sp", out[n * 128:n * 128 + nw, t0 + c0:t0 + c0 + 512], o_[:nw, :], reads=[("ob", oi % NO)], is_output=out_is_output)
    p.end_phase()


def ph_ffn_in(p, w, src, out, g, cw, K, F, T, TT, WSPLIT=4):
    p.begin_phase()
    KC = K // 128
    FC = F // 128
    NPASS = T // TT
    NTS = TT // 512
    TW = TT + 2
    gs = p.sb("gs", [128, KC], F32)
    p.dma("sp", gs[:], g, writes=["gs"])
    cws = p.sb("cws", [128, FC, 4], F32)
    p.dma("sp", cws[:], cw, writes=["cws"])
    ones = p.sb("ones", [128, 128], BF16)
    p.op("dve", lambda e: e.memset(ones[:], 1.0), writes=["ones"])
    epsb = p.sb("epsb", [128, 1], F32)
    p.op("dve", lambda e: e.memset(epsb[:], NORM_EPS), writes=["epsb"])
    hT = p.sb("hT", [128, KC, TW], BF16)
    rstd = p.sb("rstd", [128, TW], F32)
    xs = [p.sb("xs%d" % i, [128, 512], F32) for i in range(2)]
    sq = [p.sb("sq%d" % i, [128, 512], BF16) for i in range(2)]
    NWH = 3
    wb = [p.sb("wb%d" % i, [128, KC, 128], BF16) for i in range(NWH)]
    NA = 3
    Ab = [p.sb("A%d" % i, [128, 514], F32) for i in range(NA)]
    Ub = [p.sb("U%d" % i, [128, 512], F32) for i in range(2)]
    Cb = [p.sb("C%d" % i, [128, 512], F32) for i in range(2)]
    Ob = [p.sb("O%d" % i, [128, 512], F32) for i in range(2)]
    NP = 4
    pt = [p.ps("pt%d" % i, [128, 512]) for i in range(NP)]
    ss = [p.ps("ss%d" % i, [128, 512]) for i in range(min(NTS, 3) + 1)]
    xv = src.rearrange("(c p) t -> p c t", p=128)
    wv = w.rearrange("(c p) n -> p c n", p=128)
    ksplit = [(i * KC // WSPLIT, (i + 1) * KC // WSPLIT) for i in range(WSPLIT)]
    ctr = {"x": 0, "p": 0, "w": 0, "a": 0, "e": 0}
    NSS = len(ss) - 1
    for tp in range(NPASS):
        pieces = [(2 + 512 * ts, 512, tp * TT + 512 * ts) for ts in range(NTS)]
        if tp > 0:
            pieces = [(0, 2, tp * TT - 2)] + pieces
        for (c0, cn, d0) in pieces:
            pi_ = 0 if cn == 2 else 1 + ((c0 - 2) // 512) % NSS
            for kc in range(KC):
                i = ctr["x"]; ctr["x"] += 1
                xb = xs[i % 2]; sb_ = sq[i % 2]
                p.dma("sp", xb[:, :cn], xv[:, kc, d0:d0 + cn], writes=[("xs", i % 2)])
                p.op("act", lambda e: e.activation(out=sb_[:, :cn], in_=xb[:, :cn], func=AF.Square), reads=[("xs", i % 2)], writes=[("sq", i % 2)])
                p.op("pe", lambda e: e.matmul(ss[pi_][:, :cn], lhsT=ones[:], rhs=sb_[:, :cn], start=(kc == 0), stop=(kc == KC - 1)),
                     reads=[("sq", i % 2), "ones"], writes=[("ss", pi_)])
                p.op("dve", lambda e: e.tensor_scalar(out=hT[:, kc, c0:c0 + cn], in0=xb[:, :cn], scalar1=gs[:, kc:kc + 1], scalar2=None, op0=ALU.mult),
                     reads=[("xs", i % 2), "gs"], writes=[("hT", kc)])
            p.op("act", lambda e: e.activation(out=rstd[:, c0:c0 + cn], in_=ss[pi_][:, :cn], func=AF.Sqrt, scale=1.0 / K, bias=epsb[:, 0:1]),
                 reads=[("ss", pi_), "epsb"], writes=[("rstd", c0)])
            p.op("dve", lambda e: e.reciprocal(out=rstd[:, c0:c0 + cn], in_=rstd[:, c0:c0 + cn]), reads=[("rstd", c0)], writes=[("rstd", c0)])
        for fc in range(FC):
            wsl = []
            for half, col0 in ((0, fc * 128), (1, F + fc * 128)):
                wi = ctr["w"]; ctr["w"] += 1
                ws = wi % NWH
                wsl.append(ws)
                for si, (k0, k1) in enumerate(ksplit):
                    p.dma("pool", wb[ws][:, k0:k1, :], wv[:, k0:k1, col0:col0 + 128], writes=[("wb", ws, si)])

            def mm(half, c0, cn, pb, pk):
                ws = wsl[half]
                for si, (k0, k1) in enumerate(ksplit):
                    for kc in range(k0, k1):
                        p.op("pe", lambda e: e.matmul(pb[:, :cn], lhsT=wb[ws][:, kc, :], rhs=hT[:, kc, c0:c0 + cn], start=(kc == 0), stop=(kc == KC - 1)),
                             reads=[("wb", ws, si), ("hT", kc)], writes=[pk])
            ai = ctr["a"]; ctr["a"] += 1
            A = Ab[ai % NA]
            if tp == 0:
                p.op("dve", lambda e: e.memset(A[:, 0:2], 0.0), writes=[("A", ai % NA)])
            else:
                qi = ctr["p"]; ctr["p"] += 1
                pb = pt[qi % NP]
                mm(0, 0, 2, pb, ("pt", qi % NP))
                p.op("dve", lambda e: e.tensor_tensor(out=A[:, 0:2], in0=pb[:, 0:2], in1=rstd[:, 0:2], op=ALU.mult),
                     reads=[("pt", qi % NP), ("rstd", 0)], writes=[("A", ai % NA)])
            for ts in range(NTS):
                c0 = 2 + 512 * ts
                ei = ctr["e"]; ctr["e"] += 1
                U, C, O = Ub[ei % 2], Cb[ei % 2], Ob[ei % 2]
                qa = ctr["p"]; ctr["p"] += 1
                pa = pt[qa % NP]
                mm(0, c0, 512, pa, ("pt", qa % NP))
                p.op("dve", lambda e: e.tensor_tensor(out=A[:, 2:514], in0=pa[:, :], in1=rstd[:, c0:c0 + 512], op=ALU.mult),
                     reads=[("pt", qa % NP), ("rstd", c0), ("A", ai % NA)], writes=[("A", ai % NA)])
                qu = ctr["p"]; ctr["p"] += 1
                pu = pt[qu % NP]
                mm(1, c0, 512, pu, ("pt", qu % NP))
                p.op("dve", lambda e: e.tensor_tensor(out=U[:], in0=pu[:, :], in1=rstd[:, c0:c0 + 512], op=ALU.mult),
                     reads=[("pt", qu % NP), ("rstd", c0)], writes=[("U", ei % 2)])
                if ts < NTS - 1:
                    an = ctr["a"]; ctr["a"] += 1
                    A2 = Ab[an % NA]
                    p.op("pool", lambda e: e.tensor_copy(out=A2[:, 0:2], in_=A[:, 512:514]), reads=[("A", ai % NA)], writes=[("A", an % NA)])
                p.op("dve", lambda e: e.tensor_scalar(out=C[:], in0=A[:, 0:512], scalar1=cws[:, fc, 0:1], scalar2=cws[:, fc, 3:4], op0=ALU.mult, op1=ALU.add),
                     reads=[("A", ai % NA), "cws"], writes=[("C", ei % 2)])
                p.op("dve", lambda e: e.scalar_tensor_tensor(out=C[:], in0=A[:, 1:513], scalar=cws[:, fc, 1:2], in1=C[:], op0=ALU.mult, op1=ALU.add),
                     reads=[("A", ai % NA), "cws", ("C", ei % 2)], writes=[("C", ei % 2)])
                p.op("dve", lambda e: e.scalar_tensor_tensor(out=C[:], in0=A[:, 2:514], scalar=cws[:, fc, 2:3], in1=C[:], op0=ALU.mult, op1=ALU.add),
                     reads=[("A", ai % NA), "cws", ("C", ei % 2)], writes=[("C", ei % 2)])
                p.op("act", lambda e: e.activation(out=C[:], in_=C[:], func=AF.Silu), reads=[("C", ei % 2)], writes=[("C", ei % 2)])
                p.op("pool", lambda e: e.tensor_tensor(out=O[:], in0=C[:], in1=U[:], op=ALU.mult), reads=[("C", ei % 2), ("U", ei % 2)], writes=[("O", ei % 2)])
                p.dma("sp", out[fc * 128:(fc + 1) * 128, tp * TT + 512 * ts:tp * TT + 512 * ts + 512], O[:], reads=[("O", ei % 2)])
                if ts < NTS - 1:
                    A = A2
                    ai = an
    p.end_phase()


def ph_final_norm(p, src, out, g, K, T):
    p.begin_phase()
    KC = K // 128
    gs = p.sb("gs", [128, KC], F32)
    p.dma("sp", gs[:], g, writes=["gs"])
    ones = p.sb("ones", [128, 128], BF16)
    p.op("dve", lambda e: e.memset(ones[:], 1.0), writes=["ones"])
    epsb = p.sb("epsb", [128, 1], F32)
    p.op("dve", lambda e: e.memset(epsb[:], NORM_EPS), writes=["epsb"])
    xa = [p.sb("xa%d" % i, [128, KC, 512], F32) for i in range(2)]
    sq = [p.sb("sq%d" % i, [128, 512], BF16) for i in range(2)]
    ob = [p.sb("ob%d" % i, [128, 512], F32) for i in range(3)]
    rstd = [p.sb("rstd%d" % i, [128, 512], F32) for i in range(2)]
    ss = [p.ps("ss%d" % i, [128, 512]) for i in range(2)]
    xv = src.rearrange("(c p) t -> p c t", p=128)
    i = 0
    oi = 0
    for ts in range(T // 512):
        b = ts % 2
        c0 = ts * 512
        for kq in range(4):
            k0, k1 = kq * KC // 4, (kq + 1) * KC // 4
            p.dma("sp", xa[b][:, k0:k1, :], xv[:, k0:k1, c0:c0 + 512], writes=[("xa", b, kq)])
        for kc in range(KC):
            kq = kc * 4 // KC
            sb_ = sq[i % 2]
            p.op("act", lambda e: e.activation(out=sb_[:], in_=xa[b][:, kc, :], func=AF.Square), reads=[("xa", b, kq)], writes=[("sq", i % 2)])
            p.op("pe", lambda e: e.matmul(ss[b][:], lhsT=ones[:], rhs=sb_[:], start=(kc == 0), stop=(kc == KC - 1)),
                 reads=[("sq", i % 2), "ones"], writes=[("ss", b)])
            i += 1
        p.op("act", lambda e: e.activation(out=rstd[b][:], in_=ss[b][:], func=AF.Sqrt, scale=1.0 / K, bias=epsb[:, 0:1]),
             reads=[("ss", b), "epsb"], writes=[("rstd", b)])
        p.op("dve", lambda e: e.reciprocal(out=rstd[b][:], in_=rstd[b][:]), reads=[("rstd", b)], writes=[("rstd", b)])
        for kc in range(KC):
            kq = kc * 4 // KC
            o_ = ob[oi % 3]
            p.op("dve", lambda e: e.scalar_tensor_tensor(out=o_[:], in0=xa[b][:, kc, :], scalar=gs[:, kc:kc + 1], in1=rstd[b][:], op0=ALU.mult, op1=ALU.mult),
                 reads=[("xa", b, kq), "gs", ("rstd", b)], writes=[("ob", oi % 3)])
            p.dma("sp", out[kc * 128:(kc + 1) * 128, c0:c0 + 512], o_[:], reads=[("ob", oi % 3)], is_output=True)
            oi += 1
    p.end_phase()


def ph_retention(p, qT, kT, vT, ynT, dmt, qd, kd, cd, ident_d, H, S, CG=4):
    p.begin_phase()
    NCK = S // 128
    D = 256
    dmts = p.sb("dmts", [128, H, 128], F32)
    qds = p.sb("qds", [128, H, 128], F32)
    kds = p.sb("kds", [128, H], F32)
    cds = p.sb("cds", [128, H], F32)
    ident = p.sb("ident", [128, 128], BF16)
    p.dma("sp", dmts[:], dmt, writes=["dmts"])
    p.dma("sp", qds[:], qd, writes=["qds"])
    p.dma("sp", kds[:], kd, writes=["kds"])
    p.dma("sp", cds[:], cd, writes=["cds"])
    p.dma("pool", ident[:], ident_d, writes=["ident"])
    epsb = p.sb("epsb", [128, 1], F32)
    p.op("dve", lambda e: e.memset(epsb[:], GN_EPS), writes=["epsb"])
    W = CG * 128
    qg = [p.sb("qg%d" % i, [128, 2 * H, W], BF16) for i in range(2)]
    kg = [p.sb("kg%d" % i, [128, 2 * H, W], BF16) for i in range(2)]
    vg = [p.sb("vg%d" % i, [128, 2 * H, W], BF16) for i in range(2)]
    YT = [p.sb("YT%d" % i, [128, 2 * H, W], F32) for i in range(2)]
    St = [p.sb("S%d" % h, [128, 2, D], F32) for h in range(H)]
    Sb = [p.sb("Sb%d" % h, [128, 2, D], BF16) for h in range(H)]
    PT = [p.sb("PT%d" % i, [128, 128], BF16) for i in range(2)]
    QD = [p.sb("QD%d" % i, [128, 2, 128], BF16) for i in range(2)]
    KDb = [p.sb("KD%d" % i, [128, D], BF16) for i in range(2)]
    VTk = [p.sb("VT%d" % i, [128, 2, 128], BF16) for i in range(2)]
    KTk = [p.sb("KT%d" % i, [128, 2, 128], BF16) for i in range(2)]
    Yb = [p.sb("Yb%d" % i, [128, D], BF16) for i in range(2)]
    st6 = [p.sb("st6_%d" % i, [128, 6], F32) for i in range(2)]
    mv = [p.sb("mv%d" % i, [128, 2], F32) for i in range(2)]
    rs = [p.sb("rs%d" % i, [128, 1], F32) for i in range(2)]
    pA = [p.ps("pA%d" % i, [128, 128]) for i in range(2)]
    pY = [p.ps("pY%d" % i, [128, D]) for i in range(2)]
    pS = [p.ps("pS%d" % i, [128, 2, D]) for i in range(2)]
    pT = [p.ps("pT%d" % i, [128, 4, 128], BF16) for i in range(2)]
    qv = qT.rearrange("(x p) s -> p x s", p=128)
    kv = kT.rearrange("(x p) s -> p x s", p=128)
    vv = vT.rearrange("(x p) s -> p x s", p=128)
    yv = ynT.rearrange("(x p) s -> p x s", p=128)
    u = 0
    for c in range(NCK):
        cgi = c // CG
        gb = cgi % 2
        cc = c % CG
        if cc == 0:
            for hh in range(0, 2 * H, 4):
                p.dma("pool", qg[gb][:, hh:hh + 4, :], qv[:, hh:hh + 4, cgi * W:(cgi + 1) * W], writes=[("qg", gb, hh)])
                p.dma("pool", kg[gb][:, hh:hh + 4, :], kv[:, hh:hh + 4, cgi * W:(cgi + 1) * W], writes=[("kg", gb, hh)])
                p.dma("pool", vg[gb][:, hh:hh + 4, :], vv[:, hh:hh + 4, cgi * W:(cgi + 1) * W], writes=[("vg", gb, hh)])
        cs = slice(cc * 128, (cc + 1) * 128)
        for h in range(H):
            b = u % 2
            u += 1
            hk = (2 * h) // 4 * 4
            for dc in range(2):
                p.op("pe", lambda e: e.transpose(pT[b][:, dc, :], kg[gb][:, 2 * h + dc, cs], ident[:]), reads=[("kg", gb, hk), "ident"], writes=[("pT", b)])
            for dc in range(2):
                p.op("pe", lambda e: e.transpose(pT[b][:, 2 + dc, :], vg[gb][:, 2 * h + dc, cs], ident[:]), reads=[("vg", gb, hk), "ident"], writes=[("pT", b)])
            p.op("act", lambda e: e.copy(out=KTk[b][:, :, :], in_=pT[b][:, 0:2, :]), reads=[("pT", b)], writes=[("KT", b)])
            p.op("pool", lambda e: e.tensor_scalar(out=KDb[b][:], in0=KTk[b][:, :, :].rearrange("p a b -> p (a b)"), scalar1=kds[:, h:h + 1], scalar2=None, op0=ALU.mult),
                 reads=[("KT", b), "kds"], writes=[("KD", b)])
            p.op("act", lambda e: e.copy(out=VTk[b][:, :, :], in_=pT[b][:, 2:4, :]), reads=[("pT", b)], writes=[("VT", b)])
            for dc in range(2):
                p.op("pe", lambda e: e.matmul(pA[b][:], lhsT=kg[gb][:, 2 * h + dc, cs], rhs=qg[gb][:, 2 * h + dc, cs], start=(dc == 0), stop=(dc == 1)),
                     reads=[("kg", gb, hk), ("qg", gb, hk)], writes=[("pA", b)])
            p.op("dve", lambda e: e.tensor_tensor(out=PT[b][:], in0=pA[b][:], in1=dmts[:, h, :], op=ALU.mult), reads=[("pA", b), "dmts"], writes=[("PT", b)])
            if c > 0:
                for dc in range(2):
                    p.op("dve", lambda e: e.tensor_tensor(out=QD[b][:, dc, :], in0=qg[gb][:, 2 * h + dc, cs], in1=qds[:, h, :], op=ALU.mult),
                         reads=[("qg", gb, hk), "qds"], writes=[("QD", b, dc)])
            p.op("pe", lambda e: e.matmul(pY[b][:], lhsT=PT[b][:], rhs=VTk[b][:, :, :].rearrange("p a b -> p (a b)"), start=True, stop=(c == 0)), reads=[("PT", b), ("VT", b)], writes=[("pY", b)])
            if c > 0:
                for dc in range(2):
                    p.op("pe", lambda e: e.matmul(pY[b][:], lhsT=QD[b][:, dc, :], rhs=Sb[h][:, dc, :], start=False, stop=(dc == 1)),
                         reads=[("QD", b, dc), ("Sb", h)], writes=[("pY", b)])
            p.op("dve", lambda e: e.bn_stats(out=st6[b][:], in_=pY[b][:]), reads=[("pY", b)], writes=[("st6", b)])
            p.op("dve", lambda e: e.bn_aggr(out=mv[b][:], in_=st6[b][:]), reads=[("st6", b)], writes=[("mv", b)])
            p.op("act", lambda e: e.activation(out=rs[b][:], in_=mv[b][:, 1:2], func=AF.Sqrt, bias=epsb[:, 0:1]), reads=[("mv", b), "epsb"], writes=[("rs", b)])
            p.op("dve", lambda e: e.reciprocal(out=rs[b][:], in_=rs[b][:]), reads=[("rs", b)], writes=[("rs", b)])
            p.op("dve", lambda e: e.tensor_scalar(out=Yb[b][:], in0=pY[b][:], scalar1=mv[b][:, 0:1], scalar2=rs[b][:, 0:1], op0=ALU.subtract, op1=ALU.mult),
                 reads=[("pY", b), ("mv", b), ("rs", b)], writes=[("Yb", b)])
            if c < NCK - 1:
                for dc in range(2):
                    p.op("pe", lambda e: e.matmul(pS[b][:, dc, :], lhsT=KDb[b][:, dc * 128:(dc + 1) * 128], rhs=VTk[b][:, :, :].rearrange("p a b -> p (a b)"), start=True, stop=True),
                         reads=[("KD", b), ("VT", b)], writes=[("pS", b)])
                if c == 0:
                    p.op("dve", lambda e: e.tensor_copy(out=St[h][:], in_=pS[b][:]), reads=[("pS", b)], writes=[("S", h)])
                else:
                    p.op("dve", lambda e: e.scalar_tensor_tensor(out=St[h][:], in0=St[h][:], scalar=cds[:, h:h + 1], in1=pS[b][:], op0=ALU.mult, op1=ALU.add),
                         reads=[("pS", b), ("S", h), "cds"], writes=[("S", h)])
                p.op("act", lambda e: e.copy(out=Sb[h][:], in_=St[h][:]), reads=[("S", h)], writes=[("Sb", h)])
            for ec in range(2):
                p.op("pe", lambda e: e.transpose(pT[b][:, ec, :], Yb[b][:, ec * 128:(ec + 1) * 128], ident[:]), reads=[("Yb", b), "ident"], writes=[("pT", b)])
            p.op("act", lambda e: e.copy(out=YT[gb][:, 2 * h:2 * h + 2, cs], in_=pT[b][:, 0:2, :]), reads=[("pT", b)], writes=[("YT", gb, h)])
        if cc == CG - 1:
            p.dma("sp", yv[:, :, cgi * W:(cgi + 1) * W], YT[gb][:], reads=[("YT", gb, h) for h in range(H)])
    p.end_phase()


def nsa_consts(slopes):
    pp = np.arange(128, dtype=np.float64)[:, None]
    c = np.arange(512, dtype=np.float64)[None, :]
    dt = np.zeros((128, 6, 512), np.float32)
    dt[:, 0, :] = -(pp - c)
    for k in range(4):
        dist = pp - c + 128 * k
        dt[:, 1 + k, :] = np.where(dist >= 0, -dist, NEG)
    dist = pp - c + 512
    dt[:, 5, :] = np.where(dist < 512, -dist, NEG)
    u = np.arange(255, dtype=np.float64)[None, :]
    dist = pp - 16 * (u - 248) - 31
    dc = np.where(dist >= 0, -dist, NEG).astype(np.float32)
    jp = np.arange(126)[None, :] - 62
    hi = (np.arange(128)[:, None] >= 64).astype(np.int64)
    valid = jp <= hi
    forced = (jp == hi) | (jp == hi - 1)
    mm = (valid & ~forced).astype(np.float32)
    ma = np.where(forced, 1e9, np.where(valid, 0.0, -1e9)).astype(np.float32)
    slsc = np.tile((slopes / SCALE)[None, :], (128, 1)).astype(np.float32)
    deltas = 512 + 128 * np.arange(28)
    tab = np.tile((-slopes[:, None] * deltas[None, :])[None], (128, 1, 1)).astype(np.float32)
    return dict(dt=dt, dc=dc, mm=mm, ma=ma, slsc=slsc, tab=tab)


def ph_nsa(p, qpT, kvpT, attnT, w1, w2, posT, cst, ident_d, G, HG, S):
    p.begin_phase()
    NH = G * HG
    NT = S // 128
    NCMP = (S - 32) // 16 + 1
    NG3 = NH * 3

    def const(name, shape, src, dtype=F32, q="sp"):
        t = p.sb(name, shape, dtype)
        p.dma(q, t[:], src, writes=[name])
        return t
    Dt = const("Dt", [128, 6, 512], cst["dt"])
    Dc = const("Dc", [128, 255], cst["dc"])
    MM = const("MM", [128, 126], cst["mm"])
    MA = const("MA", [128, 126], cst["ma"])
    slsc = const("slsc", [128, NH], cst["slsc"])
    tab = const("tab", [128, NH, 28], cst["tab"])
    ident = const("ident", [128, 128], ident_d, BF16, "pool")
    w1s = const("w1s", [128, 2, 32, 128], w1.rearrange("y (l d) m -> d y l m", d=128), BF16, "pool")
    w2s = const("w2s", [128, 2, 128], w2.rearrange("y m n -> m y n"), BF16, "pool")
    poss = const("poss", [128, 2, 32], posT.rearrange("y d l -> d y l"), BF16, "pool")
    qg = p.sb("qg", [128, HG, S], BF16)
    ksT = p.sb("ksT", [128, S], BF16)
    kwT = p.sb("kwT", [128, S], BF16)
    vs = p.sb("vs", [128, NT, 132], BF16)
    vw = p.sb("vw", [128, NT, 132], BF16)
    tokT = p.sb("tokT", [128, S], BF16)
    kcT = p.sb("kcT", [128, 256], BF16)
    vc = p.sb("vc", [128, 2, 128], BF16)
    s1T = p.sb("s1T", [128, 256], BF16)
    bc = p.sb("bc", [128, 1], F32)
    NB_ = 3
    tmp = [p.sb("tmp%d" % i, [128, 512], F32) for i in range(NB_)]
    eb = [p.sb("eb%d" % i, [128, 512], BF16) for i in range(NB_)]
    eT = [p.sb("eT%d" % i, [128, 4, 128], BF16) for i in range(NB_)]
    efs = [p.sb("ef%d" % i, [128, 256], F32) for i in range(2)]
    pnbs = [p.sb("pnb%d" % i, [128, 256], BF16) for i in range(2)]
    Pacc = p.sb("Pacc", [128, 264], F32)
    imp = p.sb("imp", [128, 64], F32)
    sc2 = p.sb("sc2", [128, 64], F32)
    t8 = p.sb("t8", [128, 8], F32)
    t8b = p.sb("t8b", [128, 8], F32)
    selm = p.sb("selm", [128, 64], BF16)
    sm = [p.sb("sm%d" % i, [128, 4], F32) for i in range(2)]
    acc = [p.sb("acc%d" % i, [128, HG, 128], F32) for i in range(2)]
    accb = [p.sb("accb%d" % i, [128, HG, 128], BF16) for i in range(2)]
    AT = [p.sb("AT%d" % i, [128, HG, 128], F32) for i in range(2)]
    gt = p.sb("gt", [128, NT, NG3], F32)
    pS = [p.ps("pS%d" % i, [128, 512]) for i in range(NB_)]
    pT = [p.ps("pT%d" % i, [128, 4, 128], BF16) for i in range(NB_)]
    pO = [p.ps("pO%d" % i, [128, 132]) for i in range(2)]
    ctr = {"u": 0, "o": 0, "s": 0}
    p.op("dve", lambda e: e.memset(vs[:], 1.0), writes=["ksv"])
    p.op("dve", lambda e: e.memset(vw[:], 1.0), writes=["kwv"])
    p.dma("pool", tokT[:NG3, :], qpT[NH * 128:NH * 128 + NG3, :], writes=["tokT"])
    for ti in range(NT):
        b = ti % 2
        p.op("pe", lambda e: e.transpose(pT[b][:, 0, 0:NG3], tokT[:NG3, ti * 128:(ti + 1) * 128], ident[:NG3, :NG3]), reads=["tokT", "ident"], writes=[("pT", b)])
        p.op("act", lambda e: e.activation(out=gt[:, ti, :], in_=pT[b][:, 0, 0:NG3], func=AF.Sigmoid), reads=[("pT", b)], writes=["gt"])

    def stA(ud):
        u = ctr["u"]; ctr["u"] += 1
        b = u % NB_
        ud["b"] = b
        h, hh, ti, kT, k0, c_lo, ncol, Dap, bias, mask, rk = (ud[k] for k in ("h", "hh", "ti", "kT", "k0", "c_lo", "ncol", "Dap", "bias", "mask", "rk"))
        ts = slice(ti * 128, (ti + 1) * 128)
        p.op("pe", lambda e: e.matmul(pS[b][:, c_lo:ncol], lhsT=qg[:, h, ts], rhs=kT[:, k0 + c_lo:k0 + ncol], start=True, stop=True),
             reads=["qg", rk], writes=[("pS", b)])
        p.op("dve", lambda e: e.scalar_tensor_tensor(out=tmp[b][:, c_lo:ncol], in0=Dap[:, c_lo:ncol], scalar=slsc[:, hh:hh + 1], in1=pS[b][:, c_lo:ncol], op0=ALU.mult, op1=ALU.add),
             reads=[("pS", b), "Dt", "slsc"], writes=[("tmp", b)])
        if bias is None:
            p.op("act", lambda e: e.activation(out=eb[b][:, c_lo:ncol], in_=tmp[b][:, c_lo:ncol], func=AF.Exp, scale=SCALE), reads=[("tmp", b)], writes=[("eb", b)])
        else:
            p.op("act", lambda e: e.activation(out=eb[b][:, c_lo:ncol], in_=tmp[b][:, c_lo:ncol], func=AF.Exp, scale=SCALE, bias=bias),
                 reads=[("tmp", b), "tab"], writes=[("eb", b)])
        if mask is not None:
            p.op("pool", lambda e: e.tensor_tensor(out=eb[b][:].rearrange("p (j l) -> p j l", l=64), in0=eb[b][:].rearrange("p (j l) -> p j l", l=64),
                                                   in1=mask.unsqueeze(2).to_broadcast([128, 8, 64]), op=ALU.mult),
                 reads=[("eb", b), "selm"], writes=[("eb", b)])

    def stB(ud):
        b = ud["b"]
        kc0 = ud["c_lo"] // 128
        nkc = ud["ncol"] // 128
        for kc in range(kc0, nkc):
            p.op("pe", lambda e: e.transpose(pT[b][:, kc, :], eb[b][:, kc * 128:(kc + 1) * 128], ident[:]), reads=[("eb", b), "ident"], writes=[("pT", b)])
        p.op("act", lambda e: e.copy(out=eT[b][:, kc0:nkc, :], in_=pT[b][:, kc0:nkc, :]), reads=[("pT", b)], writes=[("eT", b)])

    def stC(ud):
        b = ud["b"]
        kc0 = ud["c_lo"] // 128
        nkc = ud["ncol"] // 128
        po, first, last, Vt, vch0, rk = ud["po"], ud["first"], ud["last"], ud["Vt"], ud["vch0"], ud["rk"]
        for kc in range(kc0, nkc):
            p.op("pe", lambda e: e.matmul(po[0][:, 0:130], lhsT=eT[b][:, kc, :], rhs=Vt[:, vch0 + kc, 0:130], start=(first and kc == kc0), stop=(last and kc == nkc - 1)),
                 reads=[("eT", b), rk + "v"], writes=[po[1]])
        if last:
            ob, h, hh, br, ti, A, ab = ud["ob"], ud["h"], ud["hh"], ud["br"], ud["ti"], ud["A"], ud["ab"]
            s_ = sm[ob]
            p.op("dve", lambda e: e.reciprocal(out=s_[:, 2:3], in_=pO[ob][:, 128:129]), reads=[("pO", ob)], writes=[("sm2", ob)])
            p.op("dve", lambda e: e.tensor_tensor(out=s_[:, 3:4], in0=s_[:, 2:3], in1=gt[:, ti, hh * 3 + br:hh * 3 + br + 1], op=ALU.mult), reads=[("sm2", ob), "gt"], writes=[("sm3", ob)])
            p.op("dve", lambda e: e.scalar_tensor_tensor(out=A[:, h, :], in0=pO[ob][:, 0:128], scalar=s_[:, 3:4], in1=A[:, h, :], op0=ALU.mult, op1=ALU.add),
                 reads=[("pO", ob), ("sm3", ob), ("acc", ab, h)], writes=[("acc", ab, h)])

    qv = qpT[0:NH * 128, :].rearrange("(x p) s -> p x s", p=128)
    for g in range(G):
        for hq in range(0, HG, 2):
            p.dma("pool", qg[:, hq:hq + 2, :], qv[:, g * HG + hq:g * HG + hq + 2, :], writes=["qg"])
        kvrow = lambda j: kvpT[(j * G + g) * 128:(j * G + g + 1) * 128, :]
        p.dma("pool", ksT[:], kvrow(2), writes=["ks"])
        p.dma("pool", kwT[:], kvrow(4), writes=["kw"])
        for (j, Vt, key) in ((3, vs, "ksv"), (5, vw, "kwv")):
            p.dma("pool", tokT[:], kvrow(j), writes=["tokT"])
            for ti in range(NT):
                b = ti % 2
                p.op("pe", lambda e: e.transpose(pT[b][:, 0, :], tokT[:, ti * 128:(ti + 1) * 128], ident[:]), reads=["tokT", "ident"], writes=[("pT", b)])
                p.op("act", lambda e: e.copy(out=Vt[:, ti, 0:128], in_=pT[b][:, 0, :]), reads=[("pT", b)], writes=[key])
        for ty in range(2):
            p.dma("pool", tokT[:], kvrow(ty), writes=["tokT"])
            for l in range(32):
                p.op("pe", lambda e: e.matmul(pS[0][:, 0:1], lhsT=w1s[:, ty, l, :], rhs=poss[:, ty, l:l + 1], start=(l == 0), stop=(l == 31)),
                     reads=["w1s", "poss"], writes=[("pS", 0)])
            p.op("dve", lambda e: e.tensor_copy(out=bc[:], in_=pS[0][:, 0:1]), reads=[("pS", 0)], writes=["bc"])
            for l in range(32):
                p.op("pe", lambda e: e.matmul(pS[1][:, 0:NCMP], lhsT=w1s[:, ty, l, :], rhs=tokT[:, l:l + 16 * (NCMP - 1) + 1:16], start=(l == 0), stop=(l == 31)),
                     reads=["w1s", "tokT"], writes=[("pS", 1)])
            p.op("act", lambda e: e.activation(out=s1T[:, 0:NCMP], in_=pS[1][:, 0:NCMP], func=AF.Silu, bias=bc[:, 0:1]), reads=[("pS", 1), "bc"], writes=["s1T"])
            if ty == 0:
                p.op("pe", lambda e: e.matmul(pS[0][:, 0:NCMP], lhsT=w2s[:, 0, :], rhs=s1T[:, 0:NCMP], start=True, stop=True), reads=["w2s", "s1T"], writes=[("pS", 0)])
                p.op("dve", lambda e: e.tensor_copy(out=kcT[:, 0:NCMP], in_=pS[0][:, 0:NCMP]), reads=[("pS", 0)], writes=["kcT"])
            else:
                p.op("dve", lambda e: e.memset(vc[:], 0.0), writes=["vc"])
                for ch in range((NCMP + 127) // 128):
                    n0 = ch * 128
                    nn = min(128, NCMP - n0)
                    p.op("pe", lambda e: e.matmul(pS[0][:nn, 0:128], lhsT=s1T[:, n0:n0 + nn], rhs=w2s[:, 1, :], start=True, stop=True), reads=["w2s", "s1T"], writes=[("pS", 0)])
                    p.op("dve", lambda e: e.tensor_copy(out=vc[:nn, ch, :], in_=pS[0][:nn, 0:128]), reads=[("pS", 0)], writes=["vc"])
        for ti in range(NT):
            ab = ctr["o"] % 2; ctr["o"] += 1
            A = acc[ab]
            ts = slice(ti * 128, (ti + 1) * 128)
            n_hi = min(NCMP, 8 * ti + 7)
            u0 = 248 - 8 * ti
            p.op("dve", lambda e: e.memset(Pacc[:], 0.0), writes=["Pacc"])
            for h in range(HG):
                hh = g * HG + h
                si = ctr["s"] % 2; ctr["s"] += 1
                s_ = sm[si]
                cb_ = ctr["u"] % NB_; ctr["u"] += 1
                ef = efs[si]; pnb = pnbs[si]
                p.op("pe", lambda e: e.matmul(pS[cb_][:, 0:n_hi], lhsT=qg[:, h, ts], rhs=kcT[:, 0:n_hi], start=True, stop=True), reads=["qg", "kcT"], writes=[("pS", cb_)])
                p.op("dve", lambda e: e.scalar_tensor_tensor(out=tmp[cb_][:, 0:n_hi], in0=Dc[:, u0:u0 + n_hi], scalar=slsc[:, hh:hh + 1], in1=pS[cb_][:, 0:n_hi], op0=ALU.mult, op1=ALU.add),
                     reads=[("pS", cb_), "Dc", "slsc"], writes=[("tmp", cb_)])
                p.op("dve", lambda e: e.reduce_max(out=s_[:, 0:1], in_=tmp[cb_][:, 0:n_hi], axis=AX.X), reads=[("tmp", cb_)], writes=[("sm", si)])
                p.op("dve", lambda e: e.tensor_scalar(out=s_[:, 0:1], in0=s_[:, 0:1], scalar1=-1.0e6, scalar2=-SCALE, op0=ALU.max, op1=ALU.mult), reads=[("sm", si)], writes=[("sm", si)])
                p.op("act", lambda e: e.activation(out=ef[:, 0:n_hi], in_=tmp[cb_][:, 0:n_hi], func=AF.Exp, scale=SCALE, bias=s_[:, 0:1], accum_out=s_[:, 1:2]),
                     reads=[("tmp", cb_), ("sm", si)], writes=[("ef", si), ("sm", si)])
                p.op("dve", lambda e: e.tensor_scalar(out=s_[:, 1:2], in0=s_[:, 1:2], scalar1=1.0e-30, scalar2=None, op0=ALU.max), reads=[("sm", si)], writes=[("sm", si)])
                p.op("dve", lambda e: e.reciprocal(out=s_[:, 1:2], in_=s_[:, 1:2]), reads=[("sm", si)], writes=[("sm", si)])
                p.op("dve", lambda e: e.scalar_tensor_tensor(out=Pacc[:, 1:1 + n_hi], in0=ef[:, 0:n_hi], scalar=s_[:, 1:2], in1=Pacc[:, 1:1 + n_hi], op0=ALU.mult, op1=ALU.add),
                     reads=[("ef", si), ("sm", si), "Pacc"], writes=["Pacc"])
                p.op("dve", lambda e: e.memset(pnb[:], 0.0), writes=[("pnb", si)])
                p.op("dve", lambda e: e.tensor_scalar(out=pnb[:, 0:n_hi], in0=ef[:, 0:n_hi], scalar1=s_[:, 1:2], scalar2=None, op0=ALU.mult), reads=[("ef", si), ("sm", si)], writes=[("pnb", si)])
                nch = (n_hi + 127) // 128
                for ch in range(nch):
                    p.op("pe", lambda e: e.transpose(pT[cb_][:, ch, :], pnb[:, ch * 128:(ch + 1) * 128], ident[:]), reads=[("pnb", si), "ident"], writes=[("pT", cb_)])
                p.op("act", lambda e: e.copy(out=eT[cb_][:, 0:nch, :], in_=pT[cb_][:, 0:nch, :]), reads=[("pT", cb_)], writes=[("eT", cb_)])
                for ch in range(nch):
                    p.op("pe", lambda e: e.matmul(pO[si][:, 0:128], lhsT=eT[cb_][:, ch, :], rhs=vc[:, ch, :], start=(ch == 0), stop=(ch == nch - 1)), reads=[("eT", cb_), "vc"], writes=[("pO", si)])
                p.op("dve", lambda e: e.tensor_scalar(out=A[:, h, :], in0=pO[si][:, 0:128], scalar1=gt[:, ti, hh * 3:hh * 3 + 1], scalar2=None, op0=ALU.mult),
                     reads=[("pO", si), "gt"], writes=[("acc", ab, h)])
            Pv = Pacc[:, 0:256].rearrange("p (j k) -> p j k", k=4)
            Pv4 = Pacc[:, 4:260].rearrange("p (j k) -> p j k", k=4)
            p.op("dve", lambda e: e.scalar_tensor_tensor(out=imp[:], in0=Pv[:, :, 1], scalar=2.0, in1=Pv[:, :, 0], op0=ALU.mult, op1=ALU.add), reads=["Pacc"], writes=["imp"])
            p.op("dve", lambda e: e.scalar_tensor_tensor(out=imp[:], in0=Pv[:, :, 2], scalar=2.0, in1=imp[:], op0=ALU.mult, op1=ALU.add), reads=["Pacc", "imp"], writes=["imp"])
            p.op("dve", lambda e: e.scalar_tensor_tensor(out=imp[:], in0=Pv[:, :, 3], scalar=2.0, in1=imp[:], op0=ALU.mult, op1=ALU.add), reads=["Pacc", "imp"], writes=["imp"])
            p.op("dve", lambda e: e.tensor_tensor(out=imp[:], in0=imp[:], in1=Pv4[:, :, 0], op=ALU.add), reads=["Pacc", "imp"], writes=["imp"])
            m0 = 62 - 2 * ti
            p.op("dve", lambda e: e.tensor_tensor(out=imp[:], in0=imp[:], in1=MM[:, m0:m0 + 64], op=ALU.mult), reads=["imp", "MM"], writes=["imp"])
            p.op("dve", lambda e: e.tensor_tensor(out=imp[:], in0=imp[:], in1=MA[:, m0:m0 + 64], op=ALU.add), reads=["imp", "MA"], writes=["imp"])
            p.op("dve", lambda e: e.memset(imp[:, 0:1], 1.0e9), reads=["imp"], writes=["imp"])
            p.op("dve", lambda e: e.max(out=t8[:], in_=imp[:]), reads=["imp"], writes=["t8"])
            p.op("dve", lambda e: e.match_replace(out=sc2[:], in_to_replace=t8[:], in_values=imp[:], imm_value=-3.0e9), reads=["imp", "t8"], writes=["sc2"])
            p.op("dve", lambda e: e.max(out=t8b[:], in_=sc2[:]), reads=["sc2"], writes=["t8b"])
            p.op("dve", lambda e: e.tensor_scalar(out=selm[:], in0=imp[:], scalar1=t8b[:, 7:8], scalar2=None, op0=ALU.is_ge), reads=["imp", "t8b"], writes=["selm"])
            flat = []
            for h in range(HG):
                hh = g * HG + h
                for br in (1, 2):
                    ob = ctr["s"] % 2; ctr["s"] += 1
                    po = (pO[ob], ("pO", ob))
                    units = []
                    if br == 1:
                        nk = ti // 4
                        for kt in range(nk + 1):
                            if kt == nk:
                                units.append(dict(kT=ksT, k0=512 * kt, c_lo=0, ncol=128 * (ti % 4) + 128, Dap=Dt[:, 1 + ti % 4, :], bias=None,
                                                  mask=selm[:, 8 * kt:8 * kt + 8], Vt=vs, vch0=4 * kt, rk="ks"))
                            else:
                                di = (128 * ti - 512 * kt - 512) // 128
                                units.append(dict(kT=ksT, k0=512 * kt, c_lo=0, ncol=512, Dap=Dt[:, 0, :], bias=tab[:, hh, di:di + 1],
                                                  mask=selm[:, 8 * kt:8 * kt + 8], Vt=vs, vch0=4 * kt, rk="ks"))
                    else:
                        if ti >= 1:
                            c_lo = max(0, 512 - 128 * ti)
                            units.append(dict(kT=kwT, k0=128 * ti - 512, c_lo=c_lo, ncol=512, Dap=Dt[:, 5, :], bias=None, mask=None, Vt=vw, vch0=ti - 4, rk="kw"))
                        units.append(dict(kT=kwT, k0=128 * ti, c_lo=0, ncol=128, Dap=Dt[:, 1, :], bias=None, mask=None, Vt=vw, vch0=ti, rk="kw"))
                    for ui, ud in enumerate(units):
                        ud.update(h=h, hh=hh, ti=ti, br=br, po=po, ob=ob, first=(ui == 0), last=(ui == len(units) - 1), A=A, ab=ab)
                        flat.append(ud)
            nfl = len(flat)
            for i in range(nfl + 2):
                if i < nfl:
                    stA(flat[i])
                if 0 <= i - 1 < nfl:
                    stB(flat[i - 1])
                if 0 <= i - 2 < nfl:
                    stC(flat[i - 2])
            Ab_ = accb[ab]
            p.op("pool", lambda e: e.tensor_copy(out=Ab_[:], in_=A[:]), reads=[("acc", ab, h) for h in range(HG)], writes=[("accb", ab)])
            for h4 in range(0, HG, 4):
                b = (h4 // 4) % 2
                nk = min(4, HG - h4)
                for k in range(nk):
                    p.op("pe", lambda e: e.transpose(pT[b][:, k, :], Ab_[:, h4 + k, :], ident[:]), reads=[("accb", ab), "ident"], writes=[("pT", b)])
                p.op("act", lambda e: e.copy(out=AT[ab][:, h4:h4 + nk, :], in_=pT[b][:, 0:nk, :]), reads=[("pT", b)], writes=[("AT", ab, h4)])
            p.dma("sp", attnT.rearrange("(x p) s -> p x s", p=128)[:, g * HG:(g + 1) * HG, ts], AT[ab][:], reads=[("AT", ab, h4) for h4 in range(0, HG, 4)])
    p.end_phase()


from concourse.bass_utils import run_bass_kernel_spmd

D_MODEL = 4096
SEQ = 4096
BATCH = 4
D_FF = 11008
NCORES = 4


def build_fused():
    p = Prog()
    D, S, F = D_MODEL, SEQ, D_FF
    I = lambda name, shape, dt=F32: p.dram(name, shape, dt, "ExternalInput")
    T_ = lambda name, shape, dt=F32: p.dram(name, shape, dt, "Internal")
    xT = I("xT", [D, S])
    w_ret_in = I("w_ret_in", [D, 4 * D]); w_ret_out = I("w_ret_out", [D, D])
    w_kv = I("w_kv", [D, 3072]); w_q = I("w_q", [D, D + 96]); w_nsa_out = I("w_nsa_out", [D, D])
    w_fi = [I("w_fi%d" % l, [D, 2 * F]) for l in range(2)]
    w_fo = [I("w_fo%d" % l, [F, D]) for l in range(2)]
    cwl = [I("cw%d" % l, [128, F // 128, 4]) for l in range(2)]
    vec = {k: I(k, [128, D // 128]) for k in ("an0", "an1", "fn0", "fn1", "kvn", "finn", "gng")}
    w1 = I("w1", [2, 4096, 128]); w2 = I("w2", [2, 128, 128]); posT = I("posT", [2, 128, 32])
    ident = I("ident", [128, 128])
    rc = [dict(dmt=I("dmt%d" % i, [128, 8, 128]), qd=I("qd%d" % i, [128, 8, 128]), kd=I("kd%d" % i, [128, 8]), cd=I("cd%d" % i, [128, 8])) for i in range(2)]
    cst = dict(dt=I("dt", [128, 6, 512]), dc=I("dc", [128, 255]), mm=I("mm", [128, 126]), ma=I("ma", [128, 126]),
               slsc=I("slsc", [128, 32]), tab=I("tab", [128, 32, 28]))
    oT = p.dram("oT", [D, S], F32, "ExternalOutput")
    projT = T_("projT", [4 * D, S]); ynT = T_("ynT", [D, S])
    x1T = T_("x1T", [D, S]); x2T = T_("x2T", [D, S]); x3T = T_("x3T", [D, S]); x4T = T_("x4T", [D, S])
    gatedT = T_("gatedT", [F, S]); kvpT = T_("kvpT", [3072, S]); qpT = T_("qpT", [D + 96, S]); attnT = T_("attnT", [D, S])

    ph_gemm(p, w_ret_in, xT, projT, D, 4 * D, S, 2048, "norm", "rstd", g=vec["an0"])
    for hg in range(2):
        r = hg * 2048
        ph_retention(p, projT[r:r + 2048, :], projT[D + r:D + r + 2048, :], projT[2 * D + r:2 * D + r + 2048, :], ynT[r:r + 2048, :],
                     rc[hg]["dmt"], rc[hg]["qd"], rc[hg]["kd"], rc[hg]["cd"], ident, 8, S, CG=2)
    ph_gemm(p, w_ret_out, ynT, x1T, D, D, S, 2048, "gate", "resid", g=vec["gng"], gT=projT[3 * D:4 * D, :], rT=xT)
    ph_ffn_in(p, w_fi[0], x1T, gatedT, vec["fn0"], cwl[0], D, F, S, 2048)
    ph_gemm(p, w_fo[0], gatedT, x2T, F, D, S, 512, "plain", "resid", rT=x1T, WSPLIT=8)
    ph_gemm(p, w_kv, x2T, kvpT, D, 3072, S, 2048, "norm", "rstd", g=vec["kvn"])
    ph_gemm(p, w_q, x2T, qpT, D, D + 96, S, 2048, "norm", "rstd", g=vec["an1"])
    ph_nsa(p, qpT, kvpT, attnT, w1, w2, posT, cst, ident, 4, 8, S)
    ph_gemm(p, w_nsa_out, attnT, x3T, D, D, S, 2048, "plain", "resid", rT=x2T)
    ph_ffn_in(p, w_fi[1], x3T, gatedT, vec["fn1"], cwl[1], D, F, S, 2048)
    ph_gemm(p, w_fo[1], gatedT, x4T, F, D, S, 512, "plain", "resid", rT=x3T, WSPLIT=8)
    ph_final_norm(p, x4T, oT, vec["finn"], D, S)
    return p.finish()


def _gl(v):
    v = np.asarray(v, np.float32)
    return np.ascontiguousarray(v.reshape(-1, 128).T)


def _ret_consts(heads):
    Dh = 256
    lg = np.log1p(-np.exp2(-5.0 - np.asarray(heads, dtype=np.float64)))
    i = np.arange(128, dtype=np.float64)
    scale = Dh ** -0.5
    Hn = len(heads)
    dmt = np.zeros((128, Hn, 128), np.float32)
    qd = np.zeros((128, Hn, 128), np.float32)
    kd = np.zeros((128, Hn), np.float32)
    cd = np.zeros((128, Hn), np.float32)
    for a, l in enumerate(lg):
        diff = i[None, :] - i[:, None]
        dmt[:, a, :] = np.where(diff >= 0, np.exp(np.maximum(diff, 0) * l), 0.0) * scale
        qd[:, a, :] = np.exp((i + 1.0) * l)[None, :]
        kd[:, a] = np.exp((127.0 - i) * l) * scale
        cd[:, a] = np.exp(128 * l)
    return dmt, qd, kd, cd


def kernel(x, attn_norm, ffn_norm, w_ret_in, ret_gn_gain, w_ret_out, kv_norm, w_kv,
           cmp_pos, cmp_w1, cmp_w2, w_nsa_q, w_nsa_out, w_ffn_in, conv_w, conv_b,
           w_ffn_out, final_norm):
    f32 = lambda a: np.ascontiguousarray(np.asarray(a, np.float32))
    x = f32(x)
    common = dict(w_ret_in=f32(w_ret_in[0]), w_ret_out=f32(w_ret_out[0]), w_kv=f32(w_kv), w_q=f32(w_nsa_q[0]), w_nsa_out=f32(w_nsa_out[0]),
                  w1=f32(cmp_w1), w2=f32(cmp_w2), posT=f32(np.asarray(cmp_pos).transpose(0, 2, 1)), ident=np.eye(128, dtype=np.float32),
                  an0=_gl(attn_norm[0]), an1=_gl(attn_norm[1]), fn0=_gl(ffn_norm[0]), fn1=_gl(ffn_norm[1]), kvn=_gl(kv_norm),
                  finn=_gl(final_norm), gng=_gl(ret_gn_gain[0]))
    for l in range(2):
        common["w_fi%d" % l] = f32(w_ffn_in[l])
        common["w_fo%d" % l] = f32(w_ffn_out[l])
        cw = np.concatenate([np.asarray(conv_w[l], np.float32), np.asarray(conv_b[l], np.float32)[None]], 0)
        common["cw%d" % l] = np.ascontiguousarray(cw.reshape(4, D_FF // 128, 128).transpose(2, 1, 0))
    for i in range(2):
        dmt, qd, kd, cd = _ret_consts(list(range(i * 8, i * 8 + 8)))
        common.update({"dmt%d" % i: dmt, "qd%d" % i: qd, "kd%d" % i: kd, "cd%d" % i: cd})
    slopes = np.exp2(-8.0 * (np.arange(32, dtype=np.float64) + 1.0) / 32.0)
    common.update(nsa_consts(slopes))
    ins = []
    for b in range(NCORES):
        d = dict(common)
        d["xT"] = np.ascontiguousarray(x[b].T)
        ins.append(d)
    nc = build_fused()
    res = run_bass_kernel_spmd(nc, ins, core_ids=list(range(NCORES)))
    out = np.empty((BATCH, SEQ, D_MODEL), np.float32)
    for b in range(NCORES):
        out[b] = np.asarray(res.results[b]["oT"]).T
    return out
```
